# Optimizing a Trainium2 kernel written in Bass

```python
import jax, jax.numpy as jnp
from jax import lax
import numpy as np

D_MODEL = 1024
BATCH = 8
SEQ = 2048
DEPTH = 1

D_MIX = D_MODEL
D_A = D_MIX // 2
D_B = D_MIX - D_A
HEAD_CH = 64
N_HEADS_A = D_A // HEAD_CH
N_HEADS_B = D_B // HEAD_CH
CONV_A_WIDTH = 3
CONV_B_WIDTH = 31
D_IN = 3 * D_A + 2 * D_B
N_GROUPS = 4
EXPERTS_PER_GROUP = 8
N_EXPERTS = N_GROUPS * EXPERTS_PER_GROUP
TOP_K_IN_GROUP = 2
D_EXPERT = D_MODEL // 2
EPS = 1e-6

kernel_name = "hybrid_shortconv_conformer_hmoe_encoder"


def rmsnorm(x, g):
    xf = x.astype(jnp.float32)
    r = lax.rsqrt(jnp.mean(xf * xf, axis=-1, keepdims=True) + EPS)
    return (xf * r).astype(x.dtype) * g


def layernorm(x, g, b):
    xf = x.astype(jnp.float32)
    mu = jnp.mean(xf, axis=-1, keepdims=True)
    var = jnp.mean(jnp.square(xf - mu), axis=-1, keepdims=True)
    return ((xf - mu) * lax.rsqrt(var + EPS)).astype(x.dtype) * g + b


def head_rmsnorm(y, n_heads, gain):
    bsz, s, c = y.shape
    yh = y.reshape(bsz, s, n_heads, c // n_heads).astype(jnp.float32)
    yh = yh * lax.rsqrt(jnp.mean(yh * yh, axis=-1, keepdims=True) + EPS)
    return yh.reshape(bsz, s, c).astype(y.dtype) * gain


def depthwise_conv_centred(u, w):
    k = w.shape[0]
    pad = (k - 1) // 2
    return lax.conv_general_dilated(
        u, w[:, None, :].astype(u.dtype), window_strides=(1,), padding=[(pad, pad)],
        dimension_numbers=("NWC", "WIO", "NWC"), feature_group_count=u.shape[-1])


def mixer_block(h, w_in, conv_a_w, conv_b_w, conv_b_bias, ln_b_g, ln_b_b, beta_a, beta_b, w_out):
    proj = jnp.einsum("bsd,dc->bsc", h, w_in)
    a_in, a_b, a_c, b_val, b_gate = jnp.split(
        proj, [D_A, 2 * D_A, 3 * D_A, 3 * D_A + D_B], axis=-1)
    y_a = a_b * depthwise_conv_centred(a_c * a_in, conv_a_w)
    u = b_val * jax.nn.sigmoid(b_gate)
    u = depthwise_conv_centred(u, conv_b_w) + conv_b_bias
    y_b = jax.nn.silu(layernorm(u, ln_b_g, ln_b_b))
    y = jnp.concatenate([head_rmsnorm(y_a, N_HEADS_A, beta_a),
                         head_rmsnorm(y_b, N_HEADS_B, beta_b)], axis=-1)
    return jnp.einsum("bsc,cd->bsd", y, w_out)


def hierarchical_moe(h, w_route_group, b_route_group, w_route_expert, b_route_expert, w1, w3, w2):
    bsz, s, d = h.shape
    t = h.reshape(bsz * s, d)
    p_group = jax.nn.softmax((t @ w_route_group + b_route_group).astype(jnp.float32), axis=-1)
    g_idx = jnp.argmax(p_group, axis=-1)
    p_sel = jnp.take_along_axis(p_group, g_idx[:, None], axis=1)[:, 0]
    le = (t @ w_route_expert + b_route_expert).astype(jnp.float32)
    le = le.reshape(-1, N_GROUPS, EXPERTS_PER_GROUP)
    le_sel = jnp.take_along_axis(le, g_idx[:, None, None], axis=1)[:, 0]
    p_exp = jax.nn.softmax(le_sel, axis=-1)
    top_v, top_i = lax.top_k(p_exp, TOP_K_IN_GROUP)
    top_w = top_v / jnp.sum(top_v, axis=-1, keepdims=True)
    eid = g_idx[:, None] * EXPERTS_PER_GROUP + top_i
    combine = p_sel[:, None] * jnp.einsum(
        "tk,tke->te", top_w, jax.nn.one_hot(eid, N_EXPERTS, dtype=jnp.float32))
    combine = combine.astype(t.dtype)
    out = jnp.zeros_like(t)
    for e in range(N_EXPERTS):
        act = jax.nn.silu(t @ w1[e]) * (t @ w3[e])
        out = out + combine[:, e:e + 1] * (act @ w2[e])
    return out.reshape(bsz, s, d)


def setup_inputs(seed: int = 0) -> dict:
    key = jax.random.key(seed)
    ks = jax.random.split(key, 20)
    f32 = jnp.float32
    nrm = lambda k, shape, scale: jax.random.normal(k, shape, f32) * scale
    L = DEPTH
    return {
        "x": jax.random.normal(ks[0], (BATCH, SEQ, D_MODEL), f32),
        "norm_mix_g": 1.0 + nrm(ks[1], (L, D_MODEL), 0.02),
        "w_in": nrm(ks[2], (L, D_MODEL, D_IN), D_MODEL ** -0.5),
        "conv_a_w": nrm(ks[3], (L, CONV_A_WIDTH, D_A), CONV_A_WIDTH ** -0.5),
        "conv_b_w": nrm(ks[4], (L, CONV_B_WIDTH, D_B), CONV_B_WIDTH ** -0.5),
        "conv_b_bias": nrm(ks[5], (L, D_B), 0.02),
        "ln_b_g": 1.0 + nrm(ks[6], (L, D_B), 0.02),
        "ln_b_b": nrm(ks[7], (L, D_B), 0.02),
        "beta_a": 1.0 + nrm(ks[8], (L, D_A), 0.02),
        "beta_b": 1.0 + nrm(ks[9], (L, D_B), 0.02),
        "w_out": nrm(ks[10], (L, D_MIX, D_MODEL), D_MIX ** -0.5),
        "norm_ffn_g": 1.0 + nrm(ks[11], (L, D_MODEL), 0.02),
        "w_route_group": nrm(ks[12], (L, D_MODEL, N_GROUPS), D_MODEL ** -0.5),
        "b_route_group": nrm(ks[13], (L, N_GROUPS), 0.01),
        "w_route_expert": nrm(ks[14], (L, D_MODEL, N_EXPERTS), D_MODEL ** -0.5),
        "b_route_expert": nrm(ks[15], (L, N_EXPERTS), 0.01),
        "w1": nrm(ks[16], (L, N_EXPERTS, D_MODEL, D_EXPERT), D_MODEL ** -0.5),
        "w3": nrm(ks[17], (L, N_EXPERTS, D_MODEL, D_EXPERT), D_MODEL ** -0.5),
        "w2": nrm(ks[18], (L, N_EXPERTS, D_EXPERT, D_MODEL), D_EXPERT ** -0.5),
        "norm_final_g": 1.0 + nrm(ks[19], (D_MODEL,), 0.02),
    }


def reference(x, norm_mix_g, w_in, conv_a_w, conv_b_w, conv_b_bias, ln_b_g, ln_b_b,
              beta_a, beta_b, w_out, norm_ffn_g, w_route_group, b_route_group,
              w_route_expert, b_route_expert, w1, w3, w2, norm_final_g):
    for l in range(DEPTH):
        h = rmsnorm(x, norm_mix_g[l])
        x = x + mixer_block(h, w_in[l], conv_a_w[l], conv_b_w[l], conv_b_bias[l],
                            ln_b_g[l], ln_b_b[l], beta_a[l], beta_b[l], w_out[l])
        h = rmsnorm(x, norm_ffn_g[l])
        x = x + hierarchical_moe(h, w_route_group[l], b_route_group[l], w_route_expert[l],
                                 b_route_expert[l], w1[l], w3[l], w2[l])
    return rmsnorm(x, norm_final_g)
```

```python
import contextlib
import numpy as np
import concourse.bass as bass
import concourse.mybir as mybir
from concourse.bass_utils import run_bass_kernel_spmd
from concourse.alu_op_type import AluOpType as ALU

F32 = mybir.dt.float32
BF16 = mybir.dt.bfloat16
I32 = mybir.dt.int32
AF = mybir.ActivationFunctionType
AX = mybir.AxisListType

S = 2048
D = 1024
NT = 16
NQ = 4
QW = 512
NE = 32
CAP = 384
NST = CAP // 128
NPRE = 10
BIGI = 2 * S
EPS = 1e-6
ENGS = ("pe", "act", "dve", "pool", "sp")


class Op:
    __slots__ = ("eng", "fn", "reads", "writes", "deps", "sem", "count",
                 "is_dma", "has_cons", "idx", "name", "nofence")

    def __init__(self, eng, fn, reads, writes, is_dma, sem, name):
        self.eng = eng
        self.fn = fn
        self.reads = reads
        self.writes = writes
        self.deps = []
        self.sem = sem
        self.count = None
        self.is_dma = is_dma
        self.has_cons = False
        self.name = name
        self.nofence = False


class Prog:
    def __init__(self, nc):
        self.nc = nc
        self.ops = []
        self.last_writer = {}
        self.readers = {}
        self.fence = []

    def set_fence(self):
        f = set()
        for w in self.last_writer.values():
            f.add(w)
        for rs in self.readers.values():
            for r in rs:
                f.add(r)
        f = {o for o in f if not o.nofence}
        latest = {}
        for o in f:
            k = (o.eng, id(o.sem) if o.is_dma else 0, o.is_dma)
            if k not in latest or latest[k].idx < o.idx:
                latest[k] = o
        keep = [o for o in f if o.is_dma] + [o for o in latest.values() if not o.is_dma]
        self.fence = keep

    def _add(self, op, extra_deps):
        deps = set()
        for k in op.reads:
            w = self.last_writer.get(k)
            if w is not None:
                deps.add(w)
        for k in op.writes:
            w = self.last_writer.get(k)
            if w is not None:
                deps.add(w)
            for r in self.readers.get(k, ()):
                deps.add(r)
        for d in extra_deps:
            if d is not None:
                deps.add(d)
        for d in self.fence:
            deps.add(d)
        deps.discard(op)
        if op.eng == "pe" and not op.is_dma:
            deps = {d for d in deps if not (d.eng == "pe" and not d.is_dma)}
        op.deps = sorted(deps, key=lambda d: d.idx)
        for d in op.deps:
            d.has_cons = True
        for k in op.reads:
            self.readers.setdefault(k, []).append(op)
        for k in op.writes:
            self.last_writer[k] = op
            self.readers[k] = []
        return op

    def op(self, eng, fn, reads=(), writes=(), deps=(), name=""):
        o = Op(eng, fn, tuple(reads), tuple(writes), False, None, name)
        o.idx = len(self.ops)
        self.ops.append(o)
        return self._add(o, deps)

    def dma(self, eng, fn, sem, reads=(), writes=(), deps=(), name="", nofence=False):
        o = Op(eng, fn, tuple(reads), tuple(writes), True, sem, name)
        o.idx = len(self.ops)
        o.has_cons = True
        o.nofence = nofence
        self.ops.append(o)
        return self._add(o, deps)

    def emit(self, st, final_waits=()):
        nc = self.nc
        esem = {e: st.enter_context(nc.semaphore("es_" + e)) for e in ENGS if e != "sp"}
        ecount = {e: 0 for e in ENGS}
        dcount = {}
        for o in self.ops:
            if o.is_dma:
                k = id(o.sem)
                dcount[k] = dcount.get(k, 0) + 16
                o.count = dcount[k]
            elif o.has_cons:
                ecount[o.eng] += 1
                o.count = ecount[o.eng]
                o.sem = esem[o.eng]
        per_eng = {e: [o for o in self.ops if o.eng == e] for e in ENGS}
        block = st.enter_context(nc.Block())

        def run(e, eo):
            waited = {}
            for o in per_eng[e]:
                need = {}
                for d in o.deps:
                    k = id(d.sem)
                    if k not in need or need[k][1] < d.count:
                        need[k] = (d.sem, d.count)
                for k, (sm, cnt) in need.items():
                    if waited.get(k, 0) >= cnt:
                        continue
                    eo.wait_ge(sm, cnt)
                    waited[k] = cnt
                ins = o.fn(eo)
                if o.is_dma:
                    ins.then_inc(o.sem, 16)
                elif o.has_cons:
                    ins.then_inc(o.sem, 1)
            if e == "sp":
                for o in final_waits:
                    eo.wait_ge(o.sem, o.count)

        @block.tensor
        def _(eo):
            run("pe", eo)

        @block.scalar
        def _(eo):
            run("act", eo)

        @block.vector
        def _(eo):
            run("dve", eo)

        @block.gpsimd
        def _(eo):
            run("pool", eo)

        @block.sync
        def _(eo):
            run("sp", eo)


class Arena:
    def __init__(self, A, n):
        self.A = A
        self.n = n
        self.off = 0

    def mark(self):
        return self.off

    def reset(self, m):
        self.off = m

    def alloc(self, shape, dt):
        size = {F32: 4, BF16: 2, I32: 4}[dt]
        assert shape[0] == 128
        ne = 1
        for s_ in shape[1:]:
            ne *= s_
        nb = ne * size
        nb = (nb + 31) // 32 * 32
        n2 = nb // 2
        self.peak = max(getattr(self, "peak", 0), self.off + n2)
        assert self.off + n2 <= self.n, ("arena overflow", self.off, n2, self.n)
        v = self.A[:, self.off:self.off + n2]
        self.off += n2
        if dt != BF16:
            v = v.bitcast(dt)
        v = v[:, 0:ne]
        if len(shape) == 3:
            return v.rearrange("p (a b) -> p a b", b=shape[2])
        if len(shape) == 4:
            return v.rearrange("p (a b c) -> p a b c", b=shape[2], c=shape[3])
        return v


def build_nc(debug=False):
    nc = bass.Bass("TRN2", target_bir_lowering=False)

    def din(name, shape, dt=F32):
        return nc.dram_tensor(name, shape, dt, kind="ExternalInput").ap()

    x = din("x", [S, D])
    norm_mix_g = din("norm_mix_g", [1, D])
    w_in = din("w_in", [D, 2560])
    conv_a_w = din("conv_a_w", [3, 512])
    conv_b_w = din("conv_b_w", [31, 512])
    conv_b_bias = din("conv_b_bias", [1, 512])
    ln_b_g = din("ln_b_g", [1, 512])
    ln_b_b = din("ln_b_b", [1, 512])
    beta_a = din("beta_a", [1, 512])
    beta_b = din("beta_b", [1, 512])
    w_out = din("w_out", [D, D])
    norm_ffn_g = din("norm_ffn_g", [1, D])
    w_rg = din("w_route_group", [D, 4])
    b_rg = din("b_route_group", [1, 4])
    w_re = din("w_route_expert", [D, 32])
    b_re = din("b_route_expert", [1, 32])
    w1 = din("w1", [NE, D, 512])
    w3 = din("w3", [NE, D, 512])
    w2 = din("w2", [NE, 512, D])
    norm_final_g = din("norm_final_g", [1, D])
    out = nc.dram_tensor("out", [S, D], F32, kind="ExternalOutput").ap()
    x1_d = nc.dram_tensor("x1_d", [S, D], F32, kind="Internal").ap()
    xs_d = nc.dram_tensor("xs_d", [S, D], BF16, kind="Internal").ap()
    yt_d = nc.dram_tensor("yt_d", [2 * S + 128, D], BF16, kind="Internal").ap()
    inv_d = nc.dram_tensor("inv_d", [NE * CAP, 16], I32, kind="Internal").ap()
    w1p = nc.dram_tensor("w1p", [NPRE, 128, 8 * 512], BF16, kind="Internal").ap()
    w3p = nc.dram_tensor("w3p", [NPRE, 128, 8 * 512], BF16, kind="Internal").ap()
    w2p = nc.dram_tensor("w2p", [NPRE, 128, 4 * D], BF16, kind="Internal").ap()
    dbg = {}
    if debug:
        dbg["u"] = nc.dram_tensor("dbg_u", [128, 4, S + 30], BF16, kind="ExternalOutput").ap()
        dbg["v"] = nc.dram_tensor("dbg_v", [128, 4, S + 2], BF16, kind="ExternalOutput").ap()
        dbg["ab"] = nc.dram_tensor("dbg_ab", [128, 4, S], BF16, kind="ExternalOutput").ap()
        dbg["x1"] = x1_d
        dbg["yT"] = nc.dram_tensor("dbg_yT", [NQ, 128, 8, QW], BF16, kind="ExternalOutput").ap()
        dbg["lg"] = nc.dram_tensor("dbg_lg", [NT, 128, 36], F32, kind="ExternalOutput").ap()
        dbg["cw"] = nc.dram_tensor("dbg_cw", [128, NT, 2], F32, kind="ExternalOutput").ap()
        dbg["ridx"] = nc.dram_tensor("dbg_ridx", [128, NT, 2], I32, kind="ExternalOutput").ap()

    with contextlib.ExitStack() as st:
        ARN = 106400
        A_t = st.enter_context(nc.sbuf_tensor("arena", [128, ARN], BF16))
        ar = Arena(A_t, ARN)
        banks = [st.enter_context(nc.psum_tensor(f"bank{i}", [128, 512], F32)) for i in range(8)]
        nsem = [0]

        def sem(name):
            nsem[0] += 1
            return st.enter_context(nc.semaphore(name))

        P = Prog(nc)

        identf = ar.alloc([128, 128], F32)
        identb = ar.alloc([128, 128], BF16)
        io_i = ar.alloc([128, 128], I32)
        B64 = ar.alloc([128, 128], BF16)
        O512b = ar.alloc([128, 128], BF16)
        O512f = ar.alloc([128, 128], F32)
        Ltri = ar.alloc([128, 128], BF16)
        ones_b = ar.alloc([128, 128], BF16)
        ones_row = ar.alloc([128, 128], F32)
        epsc = ar.alloc([128, 1], F32)
        iota_e = ar.alloc([128, 32], F32)
        iota_ei = ar.alloc([128, 32], I32)
        prmT = ar.alloc([128, 8, 41], F32)
        gF_bc = ar.alloc([128, D], F32)
        wr = ar.alloc([128, 8, 36], F32)
        br = ar.alloc([128, 36], F32)
        br_bc = ar.alloc([128, 36], F32)
        mDg = ar.mark()
        Dg = ar.alloc([128, 4 * 34, 128], BF16)
        ss1 = ar.alloc([128, NT], F32)
        rs1 = ar.alloc([128, NT], F32)
        ss2 = ar.alloc([128, NT], F32)
        rs2 = ar.alloc([128, NT], F32)
        ssF = ar.alloc([128, NT], F32)
        rsF = ar.alloc([128, NT], F32)
        cw = ar.alloc([128, NT, 2], F32)
        ridx = ar.alloc([128, NT, 2], I32)
        Mb = ar.alloc([128, NT, 32], BF16)
        lgs = ar.alloc([128, 36], F32)
        r_mg = ar.alloc([128, 8], F32)
        ohg = ar.alloc([128, 4], F32)
        eg = ar.alloc([128, 4], F32)
        t48 = ar.alloc([128, 4, 8], F32)
        lsel = ar.alloc([128, 8], F32)
        oh1 = ar.alloc([128, 8], F32)
        l2 = ar.alloc([128, 8], F32)
        oh2 = ar.alloc([128, 8], F32)
        E1 = ar.alloc([128, 4, 8], F32)
        E2 = ar.alloc([128, 4, 8], F32)
        j32 = ar.alloc([128, 32], F32)
        rsc = ar.alloc([128, 8], F32)
        junkb = ar.alloc([128, D], BF16)

        mF = ar.mark()
        u = ar.alloc([128, 4, S + 30], BF16)
        v = ar.alloc([128, 4, S + 2], BF16)
        ab = ar.alloc([128, 4, S], BF16)
        mU = ar.mark()
        woutb = ar.alloc([128, 8, D], BF16)
        ar.reset(mU)
        xb = [ar.alloc([128, D], F32) for _ in range(3)]
        xnb = [ar.alloc([128, D], BF16) for _ in range(2)]
        assert ar.mark() - mU == 8 * D
        prm = ar.alloc([128, D], F32)
        gF_row = ar.alloc([128, D], F32)

        s_prm = sem("s_prm")
        s_gf = sem("s_gf")
        s_br = sem("s_br")
        s_wr = sem("s_wr")

        P.op("pool", lambda e: e.iota(io_i[:], pattern=[[1, 128]], base=0, channel_multiplier=-1),
             writes=["io_i"])
        P.op("dve", lambda e: e.tensor_single_scalar(out=identf[:], in_=io_i[:], scalar=0, op=ALU.is_equal),
             reads=["io_i"], writes=["identf"])
        P.op("dve", lambda e: e.tensor_copy(out=identb[:], in_=identf[:]), reads=["identf"], writes=["identb"])
        P.op("dve", lambda e: e.tensor_single_scalar(out=Ltri[:], in_=io_i[:], scalar=0, op=ALU.is_gt),
             reads=["io_i"], writes=["Ltri"])
        P.op("dve", lambda e: e.memset(ones_b[:], 1.0), writes=["ones_b"])
        P.op("dve", lambda e: e.memset(ones_row[:], 1.0), writes=["ones_row"])
        P.op("dve", lambda e: e.memset(O512b[:], 1.0 / 512), writes=["O512b"])
        P.op("dve", lambda e: e.memset(O512f[:], 1.0 / 512), writes=["O512f"])
        P.op("dve", lambda e: e.memset(epsc[:], EPS), writes=["epsc"])
        P.op("pool", lambda e: e.memset(B64[:], 0.0), writes=["B64"])
        P.op("pool", lambda e: e.memset(B64[0:64, 0:64], 1.0 / 64), writes=["B64"])
        P.op("pool", lambda e: e.memset(B64[64:128, 64:128], 1.0 / 64), writes=["B64"])
        P.op("pool", lambda e: e.iota(iota_ei[:], pattern=[[1, 32]], base=0, channel_multiplier=0),
             writes=["iota_ei"])
        P.op("dve", lambda e: e.tensor_copy(out=iota_e[:], in_=iota_ei[:]), reads=["iota_ei"], writes=["iota_e"])
        P.op("pool", lambda e: e.memset(prm[:], 0.0), writes=["prm"])
        P.op("pool", lambda e: e.memset(u[:, :, 0:15], 0.0), writes=["u_halo"])
        P.op("pool", lambda e: e.memset(u[:, :, S + 15:S + 30], 0.0), writes=["u_halo"])
        P.op("pool", lambda e: e.memset(v[:, :, 0:1], 0.0), writes=["v_halo"])
        P.op("pool", lambda e: e.memset(v[:, :, S + 1:S + 2], 0.0), writes=["v_halo"])
        prm_rows = [(conv_a_w, 0, 3, 512), (conv_b_w, 3, 31, 512), (conv_b_bias, 34, 1, 512),
                    (ln_b_g, 35, 1, 512), (ln_b_b, 36, 1, 512), (beta_a, 37, 1, 512),
                    (beta_b, 38, 1, 512), (norm_mix_g, 39, 1, D), (norm_ffn_g, 40, 1, D)]
        for (src, r0, nr, w_) in prm_rows:
            P.dma("sp", lambda e, src=src, r0=r0, nr=nr, w_=w_: e.dma_start(out=prm[r0:r0 + nr, 0:w_], in_=src),
                  s_prm, writes=[("prmrow", r0)], deps=[P.last_writer["prm"]])
        P.dma("sp", lambda e: e.dma_start(out=gF_row[0:1, :], in_=norm_final_g), s_gf, writes=["gF_row"])
        P.dma("sp", lambda e: e.dma_start(out=br[0:1, 0:4], in_=b_rg), s_br, writes=[("br", 0)])
        P.dma("sp", lambda e: e.dma_start(out=br[0:1, 4:36], in_=b_re), s_br, writes=[("br", 1)])
        P.dma("sp", lambda e: e.dma_start(out=wr[:, :, 0:4], in_=w_rg.rearrange("(k p) c -> p k c", p=128)),
              s_wr, writes=[("wr", 0)])
        P.dma("sp", lambda e: e.dma_start(out=wr[:, :, 4:36], in_=w_re.rearrange("(k p) c -> p k c", p=128)),
              s_wr, writes=[("wr", 1)])
        for k in range(8):
            P.op("pe", lambda e, k=k: e.transpose(out=banks[0][:, k * 41:(k + 1) * 41],
                                                 in_=prm[0:41, k * 128:(k + 1) * 128],
                                                 identity=identf[0:41, 0:41]),
                 reads=["prm", "identf"] + [("prmrow", r[1]) for r in prm_rows], writes=[("bank", 0)])
        P.op("dve", lambda e: e.tensor_copy(out=prmT[:], in_=banks[0][:, 0:8 * 41].rearrange("p (k c) -> p k c", c=41)),
             reads=[("bank", 0)], writes=["prmT"])
        for h in range(2):
            P.op("pe", lambda e, h=h: e.matmul(banks[1 + h][:, :], lhsT=ones_row[0:1, :],
                                              rhs=gF_row[0:1, h * 512:(h + 1) * 512], start=True, stop=True),
                 reads=["ones_row", "gF_row"], writes=[("bank", 1 + h)])
            P.op("dve", lambda e, h=h: e.tensor_copy(out=gF_bc[:, h * 512:(h + 1) * 512], in_=banks[1 + h][:, :]),
                 reads=[("bank", 1 + h)], writes=["gF_bc"])
        P.op("pe", lambda e: e.matmul(banks[3][:, 0:36], lhsT=ones_row[0:1, :], rhs=br[0:1, :], start=True, stop=True),
             reads=["ones_row", ("br", 0), ("br", 1)], writes=[("bank", 3)])
        P.op("dve", lambda e: e.tensor_copy(out=br_bc[:], in_=banks[3][:, 0:36]), reads=[("bank", 3)], writes=["br_bc"])
        P.op("dve", lambda e: e.tensor_tensor(out=wr[:], in0=wr[:],
                                              in1=prmT[:, :, 40:41].broadcast_to([128, 8, 36]), op=ALU.mult),
             reads=[("wr", 0), ("wr", 1), "prmT"], writes=["wr"])
        hT = ar.alloc([128, 8, S], BF16)
        winb = ar.alloc([128, 8, 2560], BF16)
        tmpA = [ar.alloc([128, QW], F32) for _ in range(2)]
        s_xb = [sem(f"s_xb{i}") for i in range(3)]
        zt = ar.alloc([128, 2048], BF16)
        bigt = ar.alloc([128, NE * CAP * 16 // 128], I32)
        s_init = sem("s_init")
        P.op("pool", lambda e: e.memset(zt[:], 0.0), writes=["zt"])
        P.op("pool", lambda e: e.memset(bigt[:], BIGI), writes=["bigt"])
        init_ops = []
        init_list = [("inv", 0)] + [("yt", r0) for r0 in range(0, 2 * S, 2048)]
        init_pos = [0]

        def init_next(dep):
            if init_pos[0] >= len(init_list):
                return
            kind, r0 = init_list[init_pos[0]]
            init_pos[0] += 1
            if kind == "inv":
                init_ops.append(P.dma("sp", lambda e: e.dma_start(
                    out=inv_d.rearrange("(p a) c -> p (a c)", p=128), in_=bigt[:]), s_init, reads=["bigt"], deps=[dep]))
                return
            dst = yt_d
            init_ops.append(P.dma("sp", lambda e, r0=r0, dst=dst: e.dma_start(
                out=dst[r0:r0 + 2048, :].rearrange("(p a2 a1) d -> p a2 (a1 d)", p=128, a2=8, a1=2),
                in_=zt[:, None, :].broadcast_to([128, 8, 2048])), s_init, reads=["zt"], deps=[dep]))
        s_pre = [sem(f"s_pre{i}") for i in range(NPRE)]
        pre_list = [(e_, m) for e_ in range(NPRE) for m in range(3)]
        pre_pos = [0]

        def precast_next(dep):
            if pre_pos[0] >= len(pre_list):
                return
            e_, m = pre_list[pre_pos[0]]
            pre_pos[0] += 1
            src = (w1, w3, w2)[m][e_].rearrange("(k p) f -> p k f", p=128)
            dst = (w1p, w3p, w2p)[m][e_].rearrange("p (k f) -> p k f", k=(8, 8, 4)[m])
            P.dma("pool", lambda e, src=src, dst=dst: e.dma_start(out=dst, in_=src), s_pre[e_],
                  writes=[("wp", e_, m)], deps=[dep], nofence=True)
        s_win = [sem(f"s_win{i}") for i in range(5)]
        def load_win(blk, deps=()):
            P.dma("pool", lambda e, blk=blk: e.dma_start(
                out=winb[:, :, blk * 512:(blk + 1) * 512],
                in_=w_in[:, blk * 512:(blk + 1) * 512].rearrange("(k p) c -> p k c", p=128)),
                s_win[blk], writes=[("winb", blk)], deps=deps)
        load_win(3)
        load_win(4)
        win_after = {7: 0, 11: 2, 15: 1}
        def ph0_prep(i):
            b = i % 3
            n = i % 2
            xl = P.dma("sp", lambda e, i=i, b=b: e.dma_start(out=xb[b][:], in_=x[i * 128:(i + 1) * 128, :]),
                       s_xb[b], writes=[("xb", b)])
            if i in win_after:
                load_win(win_after[i], deps=[xl])
            P.op("act", lambda e, i=i, b=b: e.activation(out=junkb[:], in_=xb[b][:], func=AF.Square,
                                                         accum_out=ss1[:, i:i + 1]),
                 reads=[("xb", b)], writes=["junkb", ("ss1", i)])
            P.op("act", lambda e, i=i: e.activation(out=rs1[:, i:i + 1], in_=ss1[:, i:i + 1], func=AF.Ln,
                                                    bias=epsc[:, 0:1], scale=1.0 / D),
                 reads=[("ss1", i), "epsc"], writes=[("rs1", i)])
            P.op("act", lambda e, i=i: e.activation(out=rs1[:, i:i + 1], in_=rs1[:, i:i + 1], func=AF.Exp,
                                                    scale=-0.5),
                 reads=[("rs1", i)], writes=[("rs1", i)])
            P.op("dve", lambda e, i=i, b=b, n=n: e.tensor_scalar(out=xnb[n][:], in0=xb[b][:],
                                                                scalar1=rs1[:, i:i + 1], scalar2=None, op0=ALU.mult),
                 reads=[("xb", b), ("rs1", i)], writes=[("xnb", n)])

        def ph0_tr(i):
            n = i % 2
            pb = banks[i % 2]
            pT = pb[:, :].bitcast(BF16).rearrange("p (k t) -> p k t", t=128)
            for k in range(8):
                P.op("pe", lambda e, k=k, n=n, pT=pT: e.transpose(out=pT[:, k, :], in_=xnb[n][:, k * 128:(k + 1) * 128],
                                                                 identity=identb[:]),
                     reads=[("xnb", n), "identb"], writes=[("bank", i % 2)])
            P.op("dve", lambda e, i=i, pT=pT: e.tensor_tensor(
                out=hT[:, :, i * 128:(i + 1) * 128], in0=pT,
                in1=prmT[:, :, 39:40].broadcast_to([128, 8, 128]), op=ALU.mult),
                reads=[("bank", i % 2), "prmT"], writes=[("hT", i)])

        def proj(bank, col0, q):
            for k in range(8):
                P.op("pe", lambda e, k=k, bank=bank, col0=col0, q=q: e.matmul(
                    banks[bank][:, :], lhsT=winb[:, k, col0:col0 + 128], rhs=hT[:, k, q * QW:(q + 1) * QW],
                    start=(k == 0), stop=(k == 7)),
                    reads=[("winb", col0 // 512)] + [("hT", 4 * q + t_) for t_ in range(4)], writes=[("bank", bank)])

        step_c = [0]

        def stepA(j, q):
            step = step_c[0]
            b0 = 2 + 2 * (step % 3)
            step += 1
            step_c[0] = step
            proj(b0, 1536 + j * 128, q)
            proj(b0 + 1, 2048 + j * 128, q)
            t = tmpA[step % 2]
            tk = ("tmpA", step % 2)
            P.op("act", lambda e, t=t, b0=b0: e.activation(out=t[:], in_=banks[b0 + 1][:, :], func=AF.Exp,
                                                           scale=-1.0),
                 reads=[("bank", b0 + 1)], writes=[tk])
            P.op("act", lambda e, t=t: e.activation(out=t[:], in_=t[:], func=AF.Ln, bias=ones_row[:, 0:1], scale=1.0),
                 reads=[tk, "ones_row"], writes=[tk])
            P.op("act", lambda e, t=t: e.activation(out=t[:], in_=t[:], func=AF.Exp, scale=-1.0),
                 reads=[tk], writes=[tk])
            P.op("dve", lambda e, t=t, b0=b0, j=j, q=q: e.tensor_tensor(
                out=u[:, j, 15 + q * QW:15 + (q + 1) * QW], in0=banks[b0][:, :], in1=t[:], op=ALU.mult),
                reads=[tk, ("bank", b0)], writes=[("u", j, q)])
            if q == NQ - 1 and j >= 1:
                precast_next(P.ops[-1])

        PRO = 5
        ph0_prep(0)
        ph0_prep(1)
        for i in range(PRO):
            ph0_tr(i)
            if i + 2 <= PRO:
                ph0_prep(i + 2)
        sA = 0
        for q in range(NQ):
            for j in range(4):
                t_ = PRO + sA
                sA += 1
                if t_ + 1 < NT:
                    ph0_prep(t_ + 1)
                stepA(j, q)
                if t_ < NT:
                    ph0_tr(t_)
        step = step_c[0]
        for j in range(4):
            for tp in range(34):
                eng = "pool" if (tp % 2 == 0) else "dve"
                P.op(eng, lambda e, j=j, tp=tp: e.tensor_scalar(
                    out=Dg[:, j * 34 + tp, :], in0=identf[:], scalar1=prmT[:, j, tp:tp + 1],
                    scalar2=1.0, op0=ALU.mult, op1=ALU.mult),
                    reads=["identf", "prmT"], writes=[("Dg", j, tp)])

        s_wout = [sem(f"s_wout{i}") for i in range(2)]
        for h in range(2):
            P.dma("pool", lambda e, h=h: e.dma_start(
                out=woutb[:, :, h * 512:(h + 1) * 512],
                in_=w_out[:, h * 512:(h + 1) * 512].rearrange("(k p) c -> p k c", p=128)),
                s_wout[h], writes=[("woutb", h)] + [("xb", b_) for b_ in range(3)] + [("xnb", n_) for n_ in range(2)])
        for j in range(4):
            for q in range(NQ):
                b0 = 2 + 2 * (step % 3)
                step += 1
                proj(b0, 0 + j * 128, q)
                proj(b0 + 1, 1024 + j * 128, q)
                t = tmpA[step % 2]
                P.op("act", lambda e, t=t, b0=b0: e.activation(out=t[:], in_=banks[b0][:, :], func=AF.Copy),
                     reads=[("bank", b0)], writes=[("tmpA", step % 2)])
                P.op("dve", lambda e, t=t, b0=b0, j=j, q=q: e.tensor_tensor(
                    out=v[:, j, 1 + q * QW:1 + (q + 1) * QW], in0=banks[b0 + 1][:, :], in1=t[:], op=ALU.mult),
                    reads=[("tmpA", step % 2), ("bank", b0 + 1)], writes=[("v", j, q)])
                if step % 3 == 0:
                    precast_next(P.ops[-1])
                if step % 3 == 1:
                    init_next(P.ops[-1])
        for j in range(4):
            for q in range(NQ):
                b0 = 2 + (step % 6)
                step += 1
                proj(b0, 512 + j * 128, q)
                P.op("act", lambda e, b0=b0, j=j, q=q: e.activation(
                    out=ab[:, j, q * QW:(q + 1) * QW], in_=banks[b0][:, :], func=AF.Copy),
                    reads=[("bank", b0)], writes=[("ab", j, q)])
                if step % 3 == 0:
                    precast_next(P.ops[-1])
                if step % 3 == 1:
                    init_next(P.ops[-1])

        while init_pos[0] < len(init_list):
            init_next(P.ops[-1])
        if debug:
            s_dbg = sem("s_dbg")
            dbg_ops = []
            dbg_ops.append(P.dma("sp", lambda e: e.dma_start(out=dbg["u"], in_=u[:]), s_dbg,
                                 reads=[("u", j, q) for j in range(4) for q in range(4)] + ["u_halo"]))
            dbg_ops.append(P.dma("sp", lambda e: e.dma_start(out=dbg["v"], in_=v[:]), s_dbg,
                                 reads=[("v", j, q) for j in range(4) for q in range(4)] + ["v_halo"]))
            dbg_ops.append(P.dma("sp", lambda e: e.dma_start(out=dbg["ab"], in_=ab[:]), s_dbg,
                                 reads=[("ab", j, q) for j in range(4) for q in range(4)]))

        P.set_fence()
        ar.reset(mU)
        ar.alloc([128, 8, D], BF16)
        cbuf = [ar.alloc([128, 4, QW], F32) for _ in range(2)]
        csq = [ar.alloc([128, 4, QW], BF16) for _ in range(2)]
        mean_sb = ar.alloc([128, QW], F32)
        var_sb = ar.alloc([128, QW], F32)
        t_sb = [ar.alloc([128, QW], F32) for _ in range(4)]
        yab = [ar.alloc([128, QW], F32) for _ in range(4)]
        ysq = [ar.alloc([128, QW], BF16) for _ in range(4)]
        rsH = [ar.alloc([128, QW], F32) for _ in range(2)] * 2
        yT = [ar.alloc([128, 8, QW], BF16) for _ in range(2)]
        xr = [ar.alloc([128, D], F32) for _ in range(2)]
        xsb = [ar.alloc([128, D], BF16) for _ in range(4)]
        lgs4 = ar.alloc([128, 4, 36], F32)
        rq = ar.alloc([128, 8, 4], F32)
        ohg4 = ar.alloc([128, 4, 4], F32)
        d4 = ar.alloc([128, 4, 4], F32)
        t48_4 = ar.alloc([128, 4, 4, 8], F32)
        lsel4 = ar.alloc([128, 4, 8], F32)
        oh1_4 = ar.alloc([128, 4, 8], F32)
        l2_4 = ar.alloc([128, 4, 8], F32)
        oh2_4 = ar.alloc([128, 4, 8], F32)
        E1_4 = ar.alloc([128, 4, 4, 8], F32)
        E2_4 = ar.alloc([128, 4, 4, 8], F32)
        pr4 = ar.alloc([128, 4, 32], F32)
        s4 = ar.alloc([128, 4, 2], F32)
        eid4 = ar.alloc([128, 4, 2], F32)
        rf4 = ar.alloc([128, 4, 2], F32)
        xsT = ar.alloc([128, 8, 128], F32)
        s_xr = [sem(f"s_xr{i}") for i in range(2)]
        s_x1 = [sem(f"s_x1{i}") for i in range(2)]
        s_sc = [sem(f"s_sc{i}") for i in range(4)]
        s_si = [sem(f"s_si{i}") for i in range(4)]
        invsrc = ar.alloc([128, NT * 2, 16], I32)
        P.op("pool", lambda e: e.iota(invsrc[:].rearrange("p (i k) c -> p i k c", k=2),
                                      pattern=[[256, NT], [1, 2], [0, 16]], base=0, channel_multiplier=2),
             writes=["invsrc"])
        P.op("pool", lambda e: e.iota(invsrc[:].rearrange("p (i k) c -> p i k c", k=2)[:, :, :, 8:16],
                                      pattern=[[128, NT], [0, 2], [0, 8]], base=0, channel_multiplier=1),
             writes=["invsrc"])
        scat_ops = []
        xs_ops = []
        x1_ops = {}

        def convB(q, j):
            bk = j % 2
            cb = cbuf[q % 2]
            cs = csq[q % 2]
            for tp in range(31):
                P.op("pe", lambda e, j=j, tp=tp, bk=bk, q=q: e.matmul(
                    banks[bk][:, :], lhsT=Dg[:, j * 34 + 3 + tp, :],
                    rhs=u[:, j, q * QW + tp:q * QW + tp + QW], start=(tp == 0), stop=(tp == 30)),
                    reads=[("Dg", j, 3 + tp)] + [("u", j, qq) for qq in (q - 1, q, q + 1) if 0 <= qq < NQ] + ["u_halo"],
                    writes=[("bank", bk)])
            P.op("act", lambda e, j=j, bk=bk, cb=cb: e.activation(
                out=cb[:, j, :], in_=banks[bk][:, :], func=AF.Identity, bias=prmT[:, j, 34:35], scale=1.0),
                reads=[("bank", bk), "prmT"], writes=[("cb", q % 2, j)])
            P.op("act", lambda e, j=j, bk=bk, cs=cs: e.activation(
                out=cs[:, j, :], in_=banks[bk][:, :], func=AF.Square, bias=prmT[:, j, 34:35], scale=1.0),
                reads=[("bank", bk), "prmT"], writes=[("cs", q % 2, j)])
            precast_next(P.ops[-1])

        def convA(q, j):
            bk = j % 2
            for tp in range(3):
                P.op("pe", lambda e, j=j, tp=tp, bk=bk, q=q: e.matmul(
                    banks[bk][:, :], lhsT=Dg[:, j * 34 + tp, :],
                    rhs=v[:, j, q * QW + tp:q * QW + tp + QW], start=(tp == 0), stop=(tp == 2)),
                    reads=[("Dg", j, tp)] + [("v", j, qq) for qq in (q - 1, q, q + 1) if 0 <= qq < NQ] + ["v_halo"],
                    writes=[("bank", bk)])
            P.op("dve", lambda e, j=j, bk=bk, q=q: e.tensor_tensor(
                out=yab[j][:], in0=banks[bk][:, :], in1=ab[:, j, q * QW:(q + 1) * QW], op=ALU.mult),
                reads=[("bank", bk), ("ab", j, q)], writes=[("yab", j)])
            P.op("pool", lambda e, j=j: e.tensor_tensor(out=ysq[j][:], in0=yab[j][:], in1=yab[j][:], op=ALU.mult),
                 reads=[("yab", j)], writes=[("ysq", j)])

        def head_stats(q, j, beta_col, ych):
            sbk = 2 + (j % 2)
            yq = yT[q % 2]
            P.op("pe", lambda e, j=j, sbk=sbk: e.matmul(banks[sbk][:, :], lhsT=B64[:], rhs=ysq[j][:],
                                                       start=True, stop=True),
                 reads=["B64", ("ysq", j)], writes=[("bank", sbk)])
            P.op("act", lambda e, j=j, sbk=sbk: e.activation(out=rsH[j][:], in_=banks[sbk][:, :], func=AF.Ln,
                                                             bias=epsc[:, 0:1], scale=1.0),
                 reads=[("bank", sbk), "epsc"], writes=[("rsH", j % 2)])
            P.op("act", lambda e, j=j: e.activation(out=rsH[j][:], in_=rsH[j][:], func=AF.Exp, scale=-0.5),
                 reads=[("rsH", j % 2)], writes=[("rsH", j % 2)])
            P.op("dve", lambda e, j=j, yq=yq, ych=ych, beta_col=beta_col: e.scalar_tensor_tensor(
                out=yq[:, ych, :], in0=yab[j][:], scalar=prmT[:, j, beta_col:beta_col + 1], in1=rsH[j][:],
                op0=ALU.mult, op1=ALU.mult),
                reads=[("yab", j), ("rsH", j % 2), "prmT"], writes=[("yT", q % 2, ych)])

        def S2(q):
            cb = cbuf[q % 2]
            cs = csq[q % 2]
            for j in range(4):
                P.op("pe", lambda e, j=j, cb=cb: e.matmul(banks[2][:, :], lhsT=O512f[:], rhs=cb[:, j, :],
                                                         start=(j == 0), stop=(j == 3)),
                     reads=["O512f", ("cb", q % 2, j)], writes=[("bank", 2)])
            for j in range(4):
                P.op("pe", lambda e, j=j, cs=cs: e.matmul(banks[3][:, :], lhsT=O512b[:], rhs=cs[:, j, :],
                                                         start=(j == 0), stop=(j == 3)),
                     reads=["O512b", ("cs", q % 2, j)], writes=[("bank", 3)])
            P.op("act", lambda e: e.activation(out=mean_sb[:], in_=banks[2][:, :], func=AF.Copy),
                 reads=[("bank", 2)], writes=["mean_sb"])
            P.op("dve", lambda e: e.tensor_tensor(out=var_sb[:], in0=mean_sb[:], in1=mean_sb[:], op=ALU.mult),
                 reads=["mean_sb"], writes=["var_sb"])
            P.op("dve", lambda e: e.tensor_tensor(out=var_sb[:], in0=banks[3][:, :], in1=var_sb[:], op=ALU.subtract),
                 reads=[("bank", 3), "var_sb"], writes=["var_sb"])
            P.op("act", lambda e: e.activation(out=var_sb[:], in_=var_sb[:], func=AF.Ln, bias=epsc[:, 0:1], scale=1.0),
                 reads=["var_sb", "epsc"], writes=["var_sb"])
            P.op("act", lambda e: e.activation(out=var_sb[:], in_=var_sb[:], func=AF.Exp, scale=-0.5),
                 reads=["var_sb"], writes=["var_sb"])
            precast_next(P.ops[-1])
            for j in range(4):
                head_stats(q, j, 37, j)

        def S3(q):
            cb = cbuf[q % 2]
            for j in range(4):
                P.op("dve", lambda e, j=j, cb=cb: e.tensor_tensor(out=t_sb[j][:], in0=cb[:, j, :], in1=mean_sb[:],
                                                                 op=ALU.subtract),
                     reads=[("cb", q % 2, j), "mean_sb"], writes=[("t_sb", j)])
            for j in range(4):
                P.op("dve", lambda e, j=j: e.tensor_tensor(out=t_sb[j][:], in0=t_sb[j][:], in1=var_sb[:], op=ALU.mult),
                     reads=[("t_sb", j), "var_sb"], writes=[("t_sb", j)])
            for j in range(4):
                P.op("act", lambda e, j=j: e.activation(
                    out=yab[j][:], in_=t_sb[j][:], func=AF.Silu, bias=prmT[:, j, 36:37], scale=prmT[:, j, 35:36]),
                    reads=[("t_sb", j), "prmT"], writes=[("yab", j)])
            for j in range(4):
                P.op("pool", lambda e, j=j: e.tensor_tensor(out=ysq[j][:], in0=yab[j][:], in1=yab[j][:], op=ALU.mult),
                     reads=[("yab", j)], writes=[("ysq", j)])

        def S4(q):
            for j in range(4):
                head_stats(q, j, 38, 4 + j)

        wpre = []

        def S5a(q, tl):
            i = q * 4 + tl
            p2 = i % 2
            yq = yT[q % 2]
            P.dma("sp", lambda e, i=i, p2=p2: e.dma_start(out=xr[p2][:], in_=x[i * 128:(i + 1) * 128, :]),
                  s_xr[p2], writes=[("xr", p2, 0), ("xr", p2, 1)])
            if wpre:
                e0_, m0_ = wpre.pop(0)
                load_w(e0_, extra_writes=U_NAMES, parts=(m0_,))
            for h in range(2):
                for c in range(8):
                    P.op("pe", lambda e, h=h, c=c, tl=tl, yq=yq: e.matmul(
                        banks[4 + h][:, :], lhsT=yq[:, c, tl * 128:(tl + 1) * 128],
                        rhs=woutb[:, c, h * 512:(h + 1) * 512], start=(c == 0), stop=(c == 7)),
                        reads=[("yT", q % 2, c), ("woutb", h)], writes=[("bank", 4 + h)])
                P.op("dve", lambda e, h=h, p2=p2: e.tensor_tensor(
                    out=xr[p2][:, h * 512:(h + 1) * 512], in0=banks[4 + h][:, :],
                    in1=xr[p2][:, h * 512:(h + 1) * 512], op=ALU.add),
                    reads=[("bank", 4 + h), ("xr", p2, h)], writes=[("xr", p2, h)])
            xk = [("xr", p2, 0), ("xr", p2, 1)]
            x1_ops[i] = P.dma("sp", lambda e, i=i, p2=p2: e.dma_start(out=x1_d[i * 128:(i + 1) * 128, :], in_=xr[p2][:]),
                              s_x1[p2], reads=xk, writes=[("x1_d", i)])
            P.op("act", lambda e, i=i, p2=p2: e.activation(out=junkb[:], in_=xr[p2][:], func=AF.Square,
                                                           accum_out=ss2[:, i:i + 1]),
                 reads=xk, writes=["junkb", ("ss2", i)])
            P.op("act", lambda e, i=i: e.activation(out=rs2[:, i:i + 1], in_=ss2[:, i:i + 1], func=AF.Ln,
                                                    bias=epsc[:, 0:1], scale=1.0 / D),
                 reads=[("ss2", i), "epsc"], writes=[("rs2", i)])
            P.op("act", lambda e, i=i: e.activation(out=rs2[:, i:i + 1], in_=rs2[:, i:i + 1], func=AF.Exp,
                                                    scale=-0.5),
                 reads=[("rs2", i)], writes=[("rs2", i)])
            P.op("pool", lambda e, i=i, p2=p2, tl=tl: e.tensor_scalar(out=xsb[tl][:], in0=xr[p2][:],
                                                                     scalar1=rs2[:, i:i + 1], scalar2=1.0,
                                                                     op0=ALU.mult, op1=ALU.mult),
                 reads=xk + [("rs2", i)], writes=[("xsb", tl)])
            xs_ops.append(P.dma("pool", lambda e, i=i, tl=tl: e.dma_start(out=xs_d[i * 128:(i + 1) * 128, :], in_=xsb[tl][:]),
                                s_sc[tl], reads=[("xsb", tl)], writes=[("xs_d", i)]))
            precast_next(P.ops[-1])

        def S5b(q, tl):
            i = q * 4 + tl
            p2 = i % 2
            xk = [("xr", p2, 0), ("xr", p2, 1)]
            for hh in range(2):
                tbk = (6, 2, 3)[(2 * i + hh) % 3]
                pT6 = banks[tbk][:, :].rearrange("p (k t) -> p k t", t=128)
                for k4 in range(4):
                    k = hh * 4 + k4
                    P.op("pe", lambda e, k=k, k4=k4, p2=p2, pT6=pT6: e.transpose(
                        out=pT6[:, k4, :], in_=xr[p2][:, k * 128:(k + 1) * 128], identity=identf[:]),
                        reads=xk + ["identf"], writes=[("bank", tbk)])
                P.op("dve", lambda e, hh=hh, pT6=pT6: e.tensor_copy(out=xsT[:, hh * 4:(hh + 1) * 4, :], in_=pT6),
                     reads=[("bank", tbk)], writes=[("xsT", hh)])
            lgp = banks[7][:, tl * 36:(tl + 1) * 36]
            for k in range(8):
                P.op("pe", lambda e, k=k, lgp=lgp: e.matmul(lgp, lhsT=xsT[:, k, :], rhs=wr[:, k, :],
                                                           start=(k == 0), stop=(k == 7)),
                     reads=[("xsT", k // 4), "wr"], writes=[("b7lg", tl)])
            P.op("dve", lambda e, i=i, tl=tl, lgp=lgp: e.scalar_tensor_tensor(
                out=lgs4[:, tl, :], in0=lgp, scalar=rs2[:, i:i + 1], in1=br_bc[:], op0=ALU.mult, op1=ALU.add),
                reads=[("b7lg", tl), ("rs2", i), "br_bc"], writes=[("lgs4", tl)])
            if debug:
                dbg_ops.append(P.dma("sp", lambda e, i=i, tl=tl: e.dma_start(out=dbg["lg"][i], in_=lgs4[:, tl, :]), s_dbg,
                                     reads=[("lgs4", tl)]))

        def bc(ap2, shape):
            return ap2.broadcast_to(shape)

        def RT(q):
            R = "rt"
            T0 = q * 4
            lg_g = lgs4[:, :, 0:4]
            P.op("dve", lambda e: e.tensor_reduce(out=rq[:, 0, :], in_=lg_g, axis=AX.X, op=ALU.max),
                 reads=[("lgs4", t_) for t_ in range(4)], writes=[R])
            P.op("dve", lambda e: e.tensor_tensor(out=ohg4[:], in0=lg_g, in1=bc(rq[:, 0, :, None], [128, 4, 4]),
                                                  op=ALU.is_ge), reads=[R], writes=[R])
            P.op("dve", lambda e: e.tensor_tensor(out=d4[:], in0=lg_g, in1=bc(rq[:, 0, :, None], [128, 4, 4]),
                                                  op=ALU.subtract), reads=[R], writes=[R])
            P.op("act", lambda e: e.activation(out=d4[:], in_=d4[:], func=AF.Exp), reads=[R], writes=[R])
            P.op("dve", lambda e: e.tensor_reduce(out=rq[:, 1, :], in_=d4[:], axis=AX.X, op=ALU.add),
                 reads=[R], writes=[R])
            P.op("dve", lambda e: e.reciprocal(out=rq[:, 2, :], in_=rq[:, 1, :]), reads=[R], writes=[R])
            le4 = lgs4[:, :, 4:36].rearrange("p t (g x) -> p t g x", x=8)
            P.op("dve", lambda e: e.tensor_tensor(out=t48_4[:], in0=le4, in1=bc(ohg4[:, :, :, None], [128, 4, 4, 8]),
                                                  op=ALU.mult), reads=[R], writes=[R])
            P.op("dve", lambda e: e.tensor_reduce(out=lsel4[:], in_=t48_4[:].rearrange("p t g x -> p t x g"),
                                                  axis=AX.X, op=ALU.add), reads=[R], writes=[R])
            P.op("dve", lambda e: e.tensor_reduce(out=rq[:, 3, :], in_=lsel4[:], axis=AX.X, op=ALU.max),
                 reads=[R], writes=[R])
            P.op("dve", lambda e: e.tensor_tensor(out=oh1_4[:], in0=lsel4[:], in1=bc(rq[:, 3, :, None], [128, 4, 8]),
                                                  op=ALU.is_ge), reads=[R], writes=[R])
            fl = lambda t_: t_[:].rearrange("p t x -> p (t x)")
            P.op("dve", lambda e: e.scalar_tensor_tensor(out=fl(l2_4), in0=fl(oh1_4), scalar=-1e30, in1=fl(lsel4),
                                                         op0=ALU.mult, op1=ALU.add), reads=[R], writes=[R])
            P.op("dve", lambda e: e.tensor_reduce(out=rq[:, 4, :], in_=l2_4[:], axis=AX.X, op=ALU.max),
                 reads=[R], writes=[R])
            P.op("dve", lambda e: e.tensor_tensor(out=oh2_4[:], in0=l2_4[:], in1=bc(rq[:, 4, :, None], [128, 4, 8]),
                                                  op=ALU.is_ge), reads=[R], writes=[R])
            P.op("dve", lambda e: e.tensor_tensor(out=rq[:, 5, :], in0=rq[:, 4, :], in1=rq[:, 3, :],
                                                  op=ALU.subtract), reads=[R], writes=[R])
            P.op("act", lambda e: e.activation(out=rq[:, 5, :], in_=rq[:, 5, :], func=AF.Exp), reads=[R], writes=[R])
            P.op("dve", lambda e: e.tensor_scalar(out=rq[:, 5, :], in0=rq[:, 5, :], scalar1=1.0, scalar2=None,
                                                  op0=ALU.add), reads=[R], writes=[R])
            P.op("dve", lambda e: e.reciprocal(out=rq[:, 6, :], in_=rq[:, 5, :]), reads=[R], writes=[R])
            P.op("dve", lambda e: e.tensor_tensor(out=cw[:, T0:T0 + 4, 0], in0=rq[:, 6, :], in1=rq[:, 2, :],
                                                  op=ALU.mult), reads=[R], writes=[R, ("cw", q)])
            P.op("dve", lambda e: e.tensor_tensor(out=cw[:, T0:T0 + 4, 1], in0=rq[:, 2, :], in1=cw[:, T0:T0 + 4, 0],
                                                  op=ALU.subtract), reads=[R], writes=[R, ("cw", q)])
            P.op("dve", lambda e: e.tensor_tensor(out=E1_4[:], in0=bc(ohg4[:, :, :, None], [128, 4, 4, 8]),
                                                  in1=bc(oh1_4[:, :, None, :], [128, 4, 4, 8]), op=ALU.mult),
                 reads=[R], writes=[R, "E4"])
            P.op("dve", lambda e: e.tensor_tensor(out=E2_4[:], in0=bc(ohg4[:, :, :, None], [128, 4, 4, 8]),
                                                  in1=bc(oh2_4[:, :, None, :], [128, 4, 4, 8]), op=ALU.mult),
                 reads=[R], writes=[R, "E4"])
            P.op("dve", lambda e: e.tensor_tensor(
                out=Mb[:, T0:T0 + 4, :].rearrange("p t (g x) -> p t g x", x=8), in0=E1_4[:], in1=E2_4[:], op=ALU.add),
                reads=[R], writes=[("Mb", q)])

        def S6(q):
            T0 = q * 4
            posq = banks[7][:, 256:384].rearrange("p (t x) -> p t x", x=32)
            for tl in range(4):
                i = T0 + tl
                P.op("pe", lambda e, i=i, tl=tl: e.matmul(posq[:, tl, :], lhsT=Ltri[:], rhs=Mb[:, i, :], start=True,
                                                          stop=(i == 0)),
                     reads=["Ltri", ("Mb", q)], writes=["b7pos"])
                for jj in range(i):
                    P.op("pe", lambda e, jj=jj, i=i, tl=tl: e.matmul(posq[:, tl, :], lhsT=ones_b[:], rhs=Mb[:, jj, :],
                                                                    start=False, stop=(jj == i - 1)),
                         reads=["ones_b", ("Mb", jj // 4)], writes=["b7pos"])
            Ef = lambda E_: E_[:].rearrange("p t g x -> p t (g x)")
            S = "s6"
            for kk, E_ in enumerate((E1_4, E2_4)):
                P.op("dve", lambda e, E_=E_: e.tensor_tensor(out=pr4[:], in0=Ef(E_), in1=posq, op=ALU.mult),
                     reads=["E4", "b7pos"], writes=[S])
                P.op("dve", lambda e, kk=kk: e.tensor_reduce(out=s4[:, :, kk], in_=pr4[:], axis=AX.X, op=ALU.add),
                     reads=[S], writes=[S])
                P.op("dve", lambda e, E_=E_: e.tensor_tensor(out=pr4[:], in0=Ef(E_),
                                                             in1=bc(iota_e[:, None, :], [128, 4, 32]), op=ALU.mult),
                     reads=["E4", "iota_e", S], writes=[S])
                P.op("dve", lambda e, kk=kk: e.tensor_reduce(out=eid4[:, :, kk], in_=pr4[:], axis=AX.X, op=ALU.add),
                     reads=[S], writes=[S])
            f2 = lambda t_: t_[:].rearrange("p t k -> p (t k)")
            P.op("dve", lambda e: e.tensor_scalar(out=f2(s4), in0=f2(s4), scalar1=float(CAP - 1), scalar2=None,
                                                  op0=ALU.min), reads=[S], writes=[S])
            P.op("dve", lambda e: e.scalar_tensor_tensor(out=f2(rf4), in0=f2(eid4), scalar=float(CAP), in1=f2(s4),
                                                         op0=ALU.mult, op1=ALU.add), reads=[S], writes=[S])
            P.op("dve", lambda e: e.tensor_copy(out=ridx[:, T0:T0 + 4, :], in_=rf4[:]),
                 reads=[S], writes=[("ridx", q)])

        def S6b(q):
            T0 = q * 4
            for tl in range(4):
                i = T0 + tl
                for kk in range(2):
                    so = P.dma("pool", lambda e, i=i, kk=kk, tl=tl: e.indirect_dma_start(
                        out=inv_d[:, :], out_offset=bass.IndirectOffsetOnAxis(ap=ridx[:, i, kk:kk + 1], axis=0),
                        in_=invsrc[:, i * 2 + kk, :], in_offset=None),
                        s_si[tl], reads=[("ridx", q), "invsrc"], writes=[], deps=init_ops[0:1])
                    scat_ops.append(so)

        m2 = ar.mark()
        ar.reset(mF)
        NW = 3
        NXC0 = 5
        NXC = 13
        NYO = 6
        NY = 4
        NOT = 3
        yt = [ar.alloc([128, 2, D], BF16) for _ in range(NY)]
        ar.reset(mF)
        w1b = [None] * NW
        w3b = [None] * NW
        w2b = [None] * NW
        for sl_ in range(2):
            w1b[sl_] = ar.alloc([128, 8, 512], BF16)
            w3b[sl_] = ar.alloc([128, 8, 512], BF16)
            w2b[sl_] = ar.alloc([128, 4, D], BF16)
        assert ar.mark() <= mU, (ar.mark(), mU)
        ar.reset(mU)
        xc = [ar.alloc([128, D], F32) for _ in range(NXC0)]
        _save = ar.mark()
        ar.reset(mDg)
        xc += [ar.alloc([128, D], F32) for _ in range(NXC - NXC0)]
        assert ar.mark() <= mDg + 4 * 34 * 128
        ar.reset(_save)
        ot = [ar.alloc([128, D], F32) for _ in range(NOT)]
        w1b[2] = ar.alloc([128, 8, 512], BF16)
        w3b[2] = ar.alloc([128, 8, 512], BF16)
        w2b[2] = ar.alloc([128, 4, D], BF16)
        NXG = 3
        xg = [ar.alloc([128, NST, D], BF16) for _ in range(NXG)]
        xT = [ar.alloc([128, 8, CAP], BF16) for _ in range(2)]
        slt = [ar.alloc([128, CAP], F32) for _ in range(2)]
        actT = [ar.alloc([128, 4, CAP], BF16) for _ in range(2)]
        yo = [ar.alloc([128, D], BF16) for _ in range(NYO)]
        NINV = 6
        invs = [ar.alloc([128, NST, 16], I32) for _ in range(NINV)]
        m2end = ar.mark()
        ar.reset(m2)
        s_inv = [sem(f"s_inv{i}") for i in range(NINV)]
        s_w = [[sem(f"s_w{i}_{m}") for m in range(3)] for i in range(NW)]
        s_xg = [[sem(f"s_xg{i}_{j}") for j in range(NST)] for i in range(NXG)]
        s_yo = [sem(f"s_yo{i}") for i in range(NYO)]
        s_xc = [sem(f"s_xc{i}") for i in range(NXC)]
        s_yt = [sem(f"s_yt{i}") for i in range(NY)]
        s_ot = [sem(f"s_ot{i}") for i in range(NOT)]
        U_NAMES = [(nm, j_, q_) for nm in ("u", "v", "ab") for j_ in range(4) for q_ in range(NQ)] + ["u_halo", "v_halo"]

        s_wp = [[sem(f"s_wp{i}_{m}") for m in range(3)] for i in range(NW)]

        def load_w(e_, extra_writes=(), parts=(0, 1, 2)):
            sl = e_ % NW
            xw = list(extra_writes)
            if e_ < NPRE:
                rk = [("wp", e_, m) for m in range(3)]
                if 0 in parts:
                    P.dma("sp", lambda e, e_=e_, sl=sl: e.dma_start(
                        out=w1b[sl][:], in_=w1p[e_].rearrange("p (k f) -> p k f", k=8)), s_wp[sl][0],
                        reads=rk, writes=[("w1b", sl)] + xw)
                if 1 in parts:
                    P.dma("sp", lambda e, e_=e_, sl=sl: e.dma_start(
                        out=w3b[sl][:], in_=w3p[e_].rearrange("p (k f) -> p k f", k=8)), s_wp[sl][1],
                        reads=rk, writes=[("w3b", sl)] + xw)
                if 2 in parts:
                    P.dma("sp", lambda e, e_=e_, sl=sl: e.dma_start(
                        out=w2b[sl][:], in_=w2p[e_].rearrange("p (k f) -> p k f", k=4)), s_wp[sl][2],
                        reads=rk, writes=[("w2b", sl)] + xw)
                return
            assert tuple(parts) == (0, 1, 2)
            P.dma("pool", lambda e, e_=e_, sl=sl: e.dma_start(
                out=w1b[sl][:], in_=w1[e_].rearrange("(k p) f -> p k f", p=128)), s_w[sl][0], writes=[("w1b", sl)] + xw)
            P.dma("pool", lambda e, e_=e_, sl=sl: e.dma_start(
                out=w3b[sl][:], in_=w3[e_].rearrange("(k p) f -> p k f", p=128)), s_w[sl][1], writes=[("w3b", sl)] + xw)
            P.dma("pool", lambda e, e_=e_, sl=sl: e.dma_start(
                out=w2b[sl][:], in_=w2[e_].rearrange("(k p) f -> p k f", p=128)), s_w[sl][2], writes=[("w2b", sl)] + xw)

        for j in range(4):
            convB(0, j)
        for j in range(4):
            convA(0, j)
        def S5tile(q, tl):
            S5a(q, tl)
            if tl >= 1:
                S5b(q, tl - 1)
            if tl == 3:
                S5b(q, 3)

        for q in range(NQ):
            nxt = q + 1 < NQ
            last = q == NQ - 1
            if nxt:
                convB(q + 1, 0)
            S2(q)
            if 1 <= q < NQ - 1:
                S6(q - 1)
            if nxt:
                convB(q + 1, 1)
            if last:
                S5tile(q - 1, 0)
                S5tile(q - 1, 1)
            S3(q)
            if nxt:
                convB(q + 1, 2)
            if last:
                S5tile(q - 1, 2)
                S5tile(q - 1, 3)
            S4(q)
            if 1 <= q < NQ - 1:
                S6b(q - 1)
            if nxt:
                convB(q + 1, 3)
            if debug:
                dbg_ops.append(P.dma("sp", lambda e, q=q: e.dma_start(out=dbg["yT"][q], in_=yT[q % 2][:]), s_dbg,
                                     reads=[("yT", q % 2, c) for c in range(8)]))
            if nxt:
                for j in range(4):
                    convA(q + 1, j)
            if q == NQ - 2:
                wpre.extend([(0, 0), (0, 1), (0, 2), (1, 0), (1, 1), (1, 2)])
                continue
            if last:
                RT(q - 1)
                S5tile(q, 0)
                S6(q - 1)
                S5tile(q, 1)
                S6b(q - 1)
                S5tile(q, 2)
                S5tile(q, 3)
                RT(q)
            else:
                for tl in range(4):
                    S5tile(q, tl)
                RT(q)
        S6(NQ - 1)
        S6b(NQ - 1)
        while pre_pos[0] < len(pre_list):
            precast_next(P.ops[-1])
        if debug:
            dbg_ops.append(P.dma("sp", lambda e: e.dma_start(out=dbg["cw"], in_=cw[:]), s_dbg,
                                 reads=[("cw", q) for q in range(NQ)]))
            dbg_ops.append(P.dma("sp", lambda e: e.dma_start(out=dbg["ridx"], in_=ridx[:]), s_dbg,
                                 reads=[("ridx", q) for q in range(NQ)]))

        P.set_fence()
        ar.reset(m2end)
        g2T = prmT[:, :, 40:41]
        def load_xc(i):
            b = i % NXC
            P.dma("sp", lambda e, i=i, b=b: e.dma_start(out=xc[b][:], in_=x1_d[i * 128:(i + 1) * 128, :]),
                  s_xc[b], reads=[("x1_d", i)], writes=[("xc", b)])

        def load_inv(e_):
            sl = e_ % NINV
            P.dma("sp", lambda e, e_=e_, sl=sl: e.dma_start(
                out=invs[sl][:], in_=inv_d[e_ * CAP:(e_ + 1) * CAP, :].rearrange("(j p) c -> p j c", p=128)),
                s_inv[sl], writes=[("invs", sl)], deps=scat_ops)

        gbc_cache = []

        def gbc_reg(e):
            if not gbc_cache:
                gbc_cache.append(e.to_reg(S - 1))
            return gbc_cache[0]

        def gather_xg(e_):
            sl = e_ % NXG
            iv = e_ % NINV
            for j in range(NST):
                P.dma("pool", lambda e, sl=sl, iv=iv, j=j: e.indirect_dma_start(
                    out=xg[sl][:, j, :], out_offset=None, in_=xs_d[:, :],
                    in_offset=bass.IndirectOffsetOnAxis(ap=invs[iv][:, j, 8:9], axis=0),
                    bounds_check=gbc_reg(e), oob_is_err=False),
                    s_xg[sl][j], reads=[("invs", iv)], writes=[("xg", sl, j)], deps=xs_ops)

        for sl_ in range(NXG):
            P.op("dve", lambda e, sl_=sl_: e.memset(xg[sl_][:], 0.0), writes=[("xg", sl_, j_) for j_ in range(NST)])
        load_inv(0)
        load_inv(1)
        load_inv(2)
        gather_xg(0)
        gather_xg(1)
        for i in range(NXC0):
            load_xc(i)
        yg_ops = []
        yoc = [0]
        bc_cache = []

        def bc_reg(e):
            if not bc_cache:
                bc_cache.append(e.to_reg(2 * S - 1))
            return bc_cache[0]

        def ex_trans(e_, kp):
            p2 = e_ % 2
            gx = e_ % NXG
            tbk = kp % 2
            pTb = banks[tbk][:, :].bitcast(BF16)[:, 0:2 * CAP].rearrange("p (k t) -> p k t", t=CAP)
            for k2 in range(2):
                k = kp * 2 + k2
                for j in range(NST):
                    P.op("pe", lambda e, k=k, k2=k2, j=j, gx=gx, pTb=pTb: e.transpose(
                        out=pTb[:, k2, j * 128:(j + 1) * 128], in_=xg[gx][:, j, k * 128:(k + 1) * 128],
                        identity=identb[:]),
                        reads=[("xg", gx, j), "identb"], writes=[("bank", tbk)])
            P.op("dve", lambda e, kp=kp, p2=p2, pTb=pTb: e.tensor_tensor(
                out=xT[p2][:, kp * 2:(kp + 1) * 2, :], in0=pTb,
                in1=g2T[:, kp * 2:(kp + 1) * 2, :].broadcast_to([128, 2, CAP]), op=ALU.mult),
                reads=[("bank", tbk), "prmT"], writes=[("xT", p2, kp)])

        def ex_h13(e_):
            sl = e_ % NW
            p2 = e_ % 2
            for f in range(4):
                b1 = 2 + (f % 2)
                b3 = 4 + (f % 2)
                for k in range(8):
                    P.op("pe", lambda e, k=k, f=f, b1=b1, sl=sl, p2=p2: e.matmul(
                        banks[b1][:, 0:CAP], lhsT=w1b[sl][:, k, f * 128:(f + 1) * 128], rhs=xT[p2][:, k, :],
                        start=(k == 0), stop=(k == 7)),
                        reads=[("w1b", sl), ("xT", p2, k // 2)], writes=[("bank", b1)])
                for k in range(8):
                    P.op("pe", lambda e, k=k, f=f, b3=b3, sl=sl, p2=p2: e.matmul(
                        banks[b3][:, 0:CAP], lhsT=w3b[sl][:, k, f * 128:(f + 1) * 128], rhs=xT[p2][:, k, :],
                        start=(k == 0), stop=(k == 7)),
                        reads=[("w3b", sl), ("xT", p2, k // 2)], writes=[("bank", b3)])
                P.op("act", lambda e, f=f, b1=b1: e.activation(out=slt[f % 2][:], in_=banks[b1][:, 0:CAP], func=AF.Silu),
                     reads=[("bank", b1)], writes=[("slt", f % 2)])
                P.op("dve", lambda e, f=f, b3=b3, p2=p2: e.tensor_tensor(
                    out=actT[p2][:, f, :], in0=banks[b3][:, 0:CAP], in1=slt[f % 2][:], op=ALU.mult),
                    reads=[("bank", b3), ("slt", f % 2)], writes=[("actT", p2, f)])

        def ex_out(e_, j):
            sl = e_ % NW
            p2 = e_ % 2
            yb_ = yoc[0] % NYO
            yoc[0] += 1
            for h in range(2):
                ob = 6 + h
                for f in range(4):
                    P.op("pe", lambda e, f=f, j=j, h=h, ob=ob, sl=sl, p2=p2: e.matmul(
                        banks[ob][:, :], lhsT=actT[p2][:, f, j * 128:(j + 1) * 128],
                        rhs=w2b[sl][:, f, h * 512:(h + 1) * 512], start=(f == 0), stop=(f == 3)),
                        reads=[("w2b", sl), ("actT", p2, f)], writes=[("bank", ob)])
                if h == 0:
                    P.op("act", lambda e, h=h, ob=ob, yb_=yb_: e.activation(
                        out=yo[yb_][:, h * 512:(h + 1) * 512], in_=banks[ob][:, :], func=AF.Copy),
                        reads=[("bank", ob)], writes=[("yo", yb_, h)])
                else:
                    P.op("dve", lambda e, h=h, ob=ob, yb_=yb_: e.tensor_copy(
                        out=yo[yb_][:, h * 512:(h + 1) * 512], in_=banks[ob][:, :]),
                        reads=[("bank", ob)], writes=[("yo", yb_, h)])
            iv = e_ % NINV
            yg_ops.append(P.dma("pool", lambda e, j=j, iv=iv, yb_=yb_: e.indirect_dma_start(
                out=yt_d[:, :], out_offset=bass.IndirectOffsetOnAxis(ap=invs[iv][:, j, 0:1], axis=0),
                in_=yo[yb_][:], in_offset=None, bounds_check=bc_reg(e), oob_is_err=False),
                s_yo[yb_], reads=[("yo", yb_, 0), ("yo", yb_, 1), ("invs", iv)]))

        for kp in range(4):
            ex_trans(0, kp)
        for e_ in range(NE):
            if e_ + 2 < NE:
                gather_xg(e_ + 2)
                load_w(e_ + 2)
            if e_ + 3 < NE:
                load_inv(e_ + 3)
            if NXC0 + e_ < NXC:
                load_xc(NXC0 + e_)
            ex_h13(e_)
            nx = e_ + 1 < NE
            if nx:
                ex_trans(e_ + 1, 0)
                ex_trans(e_ + 1, 1)
            ex_out(e_, 0)
            if nx:
                ex_trans(e_ + 1, 2)
                ex_trans(e_ + 1, 3)
            ex_out(e_, 1)
            ex_out(e_, 2)

        out_ops = []

        def load_yt(i):
            yb6 = i % NY
            P.dma("sp", lambda e, i=i, yb6=yb6: e.dma_start(
                out=yt[yb6][:], in_=yt_d[i * 256:(i + 1) * 256, :].rearrange("(p k) d -> p k d", k=2)),
                s_yt[yb6], writes=[("yt", yb6)], deps=yg_ops)

        for i in range(NY):
            load_yt(i)

        def combine(i):
            yb6 = i % NY
            b = i % NXC
            P.op("dve", lambda e, i=i, yb6=yb6, b=b: e.scalar_tensor_tensor(
                out=xc[b][:], in0=yt[yb6][:, 0, :], scalar=cw[:, i, 0:1], in1=xc[b][:], op0=ALU.mult, op1=ALU.add),
                reads=[("yt", yb6), ("xc", b), ("cw", i // 4)], writes=[("xc", b)])
            P.op("dve", lambda e, i=i, yb6=yb6, b=b: e.scalar_tensor_tensor(
                out=xc[b][:], in0=yt[yb6][:, 1, :], scalar=cw[:, i, 1:2], in1=xc[b][:], op0=ALU.mult, op1=ALU.add),
                reads=[("yt", yb6), ("xc", b), ("cw", i // 4)], writes=[("xc", b)])

        combine(0)
        for i in range(NT):
            p2 = i % NOT
            b = i % NXC
            P.op("act", lambda e, i=i, b=b: e.activation(out=junkb[:], in_=xc[b][:], func=AF.Square,
                                                         accum_out=ssF[:, i:i + 1]),
                 reads=[("xc", b)], writes=["junkb", ("ssF", i)])
            P.op("act", lambda e, i=i: e.activation(out=rsF[:, i:i + 1], in_=ssF[:, i:i + 1], func=AF.Ln,
                                                    bias=epsc[:, 0:1], scale=1.0 / D),
                 reads=[("ssF", i), "epsc"], writes=[("rsF", i)])
            P.op("act", lambda e, i=i: e.activation(out=rsF[:, i:i + 1], in_=rsF[:, i:i + 1], func=AF.Exp, scale=-0.5),
                 reads=[("rsF", i)], writes=[("rsF", i)])
            if i + 1 < NT:
                combine(i + 1)
            P.op("dve", lambda e, i=i, p2=p2, b=b: e.scalar_tensor_tensor(
                out=ot[p2][:], in0=xc[b][:], scalar=rsF[:, i:i + 1], in1=gF_bc[:], op0=ALU.mult, op1=ALU.mult),
                reads=[("xc", b), ("rsF", i), "gF_bc"], writes=[("ot", p2)])
            out_ops.append(P.dma("sp", lambda e, i=i, p2=p2: e.dma_start(out=out[i * 128:(i + 1) * 128, :], in_=ot[p2][:]),
                                 s_ot[p2], reads=[("ot", p2)]))
            if i + NXC < NT:
                load_xc(i + NXC)
            if i + NY < NT:
                load_yt(i + NY)
        finals = out_ops[-NOT:] + (dbg_ops if debug else [])
        P.emit(st, final_waits=finals)
        if debug:
            print('arena mF', mF, 'mU', mU, 'peak', ar.peak, 'ARN', ARN, 'nops', len(P.ops), 'nsem', nsem[0])
    return nc


_IN_NAMES = ["norm_mix_g", "w_in", "conv_a_w", "conv_b_w", "conv_b_bias", "ln_b_g", "ln_b_b", "beta_a", "beta_b",
             "w_out", "norm_ffn_g", "w_route_group", "b_route_group", "w_route_expert", "b_route_expert",
             "w1", "w3", "w2"]


def make_in_maps(inputs):
    f = lambda a: np.ascontiguousarray(np.asarray(a, dtype=np.float32))
    shared = {}
    for n in _IN_NAMES:
        a = f(inputs[n])[0]
        if a.ndim == 1:
            a = a.reshape(1, -1)
        shared[n] = np.ascontiguousarray(a)
    shared["norm_final_g"] = f(inputs["norm_final_g"]).reshape(1, -1)
    x = f(inputs["x"])
    return [dict(shared, x=np.ascontiguousarray(x[c])) for c in range(8)]


def kernel(**inputs):
    nc = build_nc()
    in_maps = make_in_maps(inputs)
    res = run_bass_kernel_spmd(nc, in_maps, core_ids=list(range(8)))
    return np.stack([np.asarray(r["out"], dtype=np.float32) for r in res.results], axis=0)
```

```python
import contextlib
import numpy as np
import concourse.bass as bass
import concourse.mybir as mybir
from concourse.bass_utils import run_bass_kernel_spmd
from concourse.alu_op_type import AluOpType as ALU

F32 = mybir.dt.float32
BF16 = mybir.dt.bfloat16
I32 = mybir.dt.int32
AF = mybir.ActivationFunctionType
AX = mybir.AxisListType

S = 2048
D = 1024
NT = 16
NQ = 4
QW = 512
NE = 32
CAP = 384
NST = CAP // 128
NPRE = 8
BIGI = 2 * S
EPS = 1e-6
ENGS = ("pe", "act", "dve", "pool", "sp")


class Op:
    __slots__ = ("eng", "fn", "reads", "writes", "deps", "sem", "count",
                 "is_dma", "has_cons", "idx", "name", "nofence")

    def __init__(self, eng, fn, reads, writes, is_dma, sem, name):
        self.eng = eng
        self.fn = fn
        self.reads = reads
        self.writes = writes
        self.deps = []
        self.sem = sem
        self.count = None
        self.is_dma = is_dma
        self.has_cons = False
        self.name = name
        self.nofence = False


class Prog:
    def __init__(self, nc):
        self.nc = nc
        self.ops = []
        self.last_writer = {}
        self.readers = {}
        self.fence = []

    def set_fence(self):
        f = set()
        for w in self.last_writer.values():
            f.add(w)
        for rs in self.readers.values():
            for r in rs:
                f.add(r)
        f = {o for o in f if not o.nofence}
        latest = {}
        for o in f:
            k = (o.eng, id(o.sem) if o.is_dma else 0, o.is_dma)
            if k not in latest or latest[k].idx < o.idx:
                latest[k] = o
        keep = [o for o in f if o.is_dma] + [o for o in latest.values() if not o.is_dma]
        self.fence = keep

    def _add(self, op, extra_deps):
        deps = set()
        for k in op.reads:
            w = self.last_writer.get(k)
            if w is not None:
                deps.add(w)
        for k in op.writes:
            w = self.last_writer.get(k)
            if w is not None:
                deps.add(w)
            for r in self.readers.get(k, ()):
                deps.add(r)
        for d in extra_deps:
            if d is not None:
                deps.add(d)
        for d in self.fence:
            deps.add(d)
        deps.discard(op)
        if op.eng == "pe" and not op.is_dma:
            deps = {d for d in deps if not (d.eng == "pe" and not d.is_dma)}
        op.deps = sorted(deps, key=lambda d: d.idx)
        for d in op.deps:
            d.has_cons = True
        for k in op.reads:
            self.readers.setdefault(k, []).append(op)
        for k in op.writes:
            self.last_writer[k] = op
            self.readers[k] = []
        return op

    def op(self, eng, fn, reads=(), writes=(), deps=(), name=""):
        o = Op(eng, fn, tuple(reads), tuple(writes), False, None, name)
        o.idx = len(self.ops)
        self.ops.append(o)
        return self._add(o, deps)

    def dma(self, eng, fn, sem, reads=(), writes=(), deps=(), name="", nofence=False):
        o = Op(eng, fn, tuple(reads), tuple(writes), True, sem, name)
        o.idx = len(self.ops)
        o.has_cons = True
        o.nofence = nofence
        self.ops.append(o)
        return self._add(o, deps)

    def emit(self, st, final_waits=()):
        nc = self.nc
        esem = {e: st.enter_context(nc.semaphore("es_" + e)) for e in ENGS if e != "sp"}
        ecount = {e: 0 for e in ENGS}
        dcount = {}
        for o in self.ops:
            if o.is_dma:
                k = id(o.sem)
                dcount[k] = dcount.get(k, 0) + 16
                o.count = dcount[k]
            elif o.has_cons:
                ecount[o.eng] += 1
                o.count = ecount[o.eng]
                o.sem = esem[o.eng]
        per_eng = {e: [o for o in self.ops if o.eng == e] for e in ENGS}
        block = st.enter_context(nc.Block())

        def run(e, eo):
            waited = {}
            for o in per_eng[e]:
                need = {}
                for d in o.deps:
                    k = id(d.sem)
                    if k not in need or need[k][1] < d.count:
                        need[k] = (d.sem, d.count)
                for k, (sm, cnt) in need.items():
                    if waited.get(k, 0) >= cnt:
                        continue
                    eo.wait_ge(sm, cnt)
                    waited[k] = cnt
                ins = o.fn(eo)
                if o.is_dma:
                    ins.then_inc(o.sem, 16)
                elif o.has_cons:
                    ins.then_inc(o.sem, 1)
            if e == "sp":
                for o in final_waits:
                    eo.wait_ge(o.sem, o.count)

        @block.tensor
        def _(eo):
            run("pe", eo)

        @block.scalar
        def _(eo):
            run("act", eo)

        @block.vector
        def _(eo):
            run("dve", eo)

        @block.gpsimd
        def _(eo):
            run("pool", eo)

        @block.sync
        def _(eo):
            run("sp", eo)


class Arena:
    def __init__(self, A, n):
        self.A = A
        self.n = n
        self.off = 0

    def mark(self):
        return self.off

    def reset(self, m):
        self.off = m

    def alloc(self, shape, dt):
        size = {F32: 4, BF16: 2, I32: 4}[dt]
        assert shape[0] == 128
        ne = 1
        for s_ in shape[1:]:
            ne *= s_
        nb = ne * size
        nb = (nb + 31) // 32 * 32
        n2 = nb // 2
        self.peak = max(getattr(self, "peak", 0), self.off + n2)
        assert self.off + n2 <= self.n, ("arena overflow", self.off, n2, self.n)
        v = self.A[:, self.off:self.off + n2]
        self.off += n2
        if dt != BF16:
            v = v.bitcast(dt)
        v = v[:, 0:ne]
        if len(shape) == 3:
            return v.rearrange("p (a b) -> p a b", b=shape[2])
        if len(shape) == 4:
            return v.rearrange("p (a b c) -> p a b c", b=shape[2], c=shape[3])
        return v


def build_nc(debug=False):
    nc = bass.Bass("TRN2", target_bir_lowering=False)

    def din(name, shape, dt=F32):
        return nc.dram_tensor(name, shape, dt, kind="ExternalInput").ap()

    x = din("x", [S, D])
    norm_mix_g = din("norm_mix_g", [1, D])
    w_in = din("w_in", [D, 2560])
    conv_a_w = din("conv_a_w", [3, 512])
    conv_b_w = din("conv_b_w", [31, 512])
    conv_b_bias = din("conv_b_bias", [1, 512])
    ln_b_g = din("ln_b_g", [1, 512])
    ln_b_b = din("ln_b_b", [1, 512])
    beta_a = din("beta_a", [1, 512])
    beta_b = din("beta_b", [1, 512])
    w_out = din("w_out", [D, D])
    norm_ffn_g = din("norm_ffn_g", [1, D])
    w_rg = din("w_route_group", [D, 4])
    b_rg = din("b_route_group", [1, 4])
    w_re = din("w_route_expert", [D, 32])
    b_re = din("b_route_expert", [1, 32])
    w1 = din("w1", [NE, D, 512])
    w3 = din("w3", [NE, D, 512])
    w2 = din("w2", [NE, 512, D])
    norm_final_g = din("norm_final_g", [1, D])
    out = nc.dram_tensor("out", [S, D], F32, kind="ExternalOutput").ap()
    x1_d = nc.dram_tensor("x1_d", [S, D], F32, kind="Internal").ap()
    xs_d = nc.dram_tensor("xs_d", [S, D], BF16, kind="Internal").ap()
    yt_d = nc.dram_tensor("yt_d", [2 * S + 128, D], BF16, kind="Internal").ap()
    inv_d = nc.dram_tensor("inv_d", [NE * CAP, 16], I32, kind="Internal").ap()
    w1p = nc.dram_tensor("w1p", [NPRE, 128, 8 * 512], BF16, kind="Internal").ap()
    w3p = nc.dram_tensor("w3p", [NPRE, 128, 8 * 512], BF16, kind="Internal").ap()
    w2p = nc.dram_tensor("w2p", [NPRE, 128, 4 * D], BF16, kind="Internal").ap()
    dbg = {}
    if debug:
        dbg["u"] = nc.dram_tensor("dbg_u", [128, 4, S + 30], BF16, kind="ExternalOutput").ap()
        dbg["v"] = nc.dram_tensor("dbg_v", [128, 4, S + 2], BF16, kind="ExternalOutput").ap()
        dbg["ab"] = nc.dram_tensor("dbg_ab", [128, 4, S], BF16, kind="ExternalOutput").ap()
        dbg["x1"] = x1_d
        dbg["yT"] = nc.dram_tensor("dbg_yT", [NQ, 128, 8, QW], BF16, kind="ExternalOutput").ap()
        dbg["lg"] = nc.dram_tensor("dbg_lg", [NT, 128, 36], F32, kind="ExternalOutput").ap()
        dbg["cw"] = nc.dram_tensor("dbg_cw", [128, NT, 2], F32, kind="ExternalOutput").ap()
        dbg["ridx"] = nc.dram_tensor("dbg_ridx", [128, NT, 2], I32, kind="ExternalOutput").ap()

    with contextlib.ExitStack() as st:
        ARN = 106400
        A_t = st.enter_context(nc.sbuf_tensor("arena", [128, ARN], BF16))
        ar = Arena(A_t, ARN)
        banks = [st.enter_context(nc.psum_tensor(f"bank{i}", [128, 512], F32)) for i in range(8)]
        nsem = [0]

        def sem(name):
            nsem[0] += 1
            return st.enter_context(nc.semaphore(name))

        P = Prog(nc)

        identf = ar.alloc([128, 128], F32)
        identb = ar.alloc([128, 128], BF16)
        io_i = ar.alloc([128, 128], I32)
        B64 = ar.alloc([128, 128], BF16)
        O512b = ar.alloc([128, 128], BF16)
        O512f = ar.alloc([128, 128], F32)
        Ltri = ar.alloc([128, 128], BF16)
        ones_b = ar.alloc([128, 128], BF16)
        ones_row = ar.alloc([128, 128], F32)
        epsc = ar.alloc([128, 1], F32)
        iota_e = ar.alloc([128, 32], F32)
        iota_ei = ar.alloc([128, 32], I32)
        prmT = ar.alloc([128, 8, 41], F32)
        gF_bc = ar.alloc([128, D], F32)
        wr = ar.alloc([128, 8, 36], F32)
        br = ar.alloc([128, 36], F32)
        br_bc = ar.alloc([128, 36], F32)
        mDg = ar.mark()
        Dg = ar.alloc([128, 4 * 34, 128], BF16)
        ss1 = ar.alloc([128, NT], F32)
        rs1 = ar.alloc([128, NT], F32)
        ss2 = ar.alloc([128, NT], F32)
        rs2 = ar.alloc([128, NT], F32)
        ssF = ar.alloc([128, NT], F32)
        rsF = ar.alloc([128, NT], F32)
        cw = ar.alloc([128, NT, 2], F32)
        ridx = ar.alloc([128, NT, 2], I32)
        Mb = ar.alloc([128, NT, 32], BF16)
        lgs = ar.alloc([128, 36], F32)
        r_mg = ar.alloc([128, 8], F32)
        ohg = ar.alloc([128, 4], F32)
        eg = ar.alloc([128, 4], F32)
        t48 = ar.alloc([128, 4, 8], F32)
        lsel = ar.alloc([128, 8], F32)
        oh1 = ar.alloc([128, 8], F32)
        l2 = ar.alloc([128, 8], F32)
        oh2 = ar.alloc([128, 8], F32)
        E1 = ar.alloc([128, 4, 8], F32)
        E2 = ar.alloc([128, 4, 8], F32)
        j32 = ar.alloc([128, 32], F32)
        rsc = ar.alloc([128, 8], F32)
        junkb = ar.alloc([128, D], BF16)

        mF = ar.mark()
        u = ar.alloc([128, 4, S + 30], BF16)
        v = ar.alloc([128, 4, S + 2], BF16)
        ab = ar.alloc([128, 4, S], BF16)
        mU = ar.mark()
        woutb = ar.alloc([128, 8, D], BF16)
        ar.reset(mU)
        xb = [ar.alloc([128, D], F32) for _ in range(3)]
        xnb = [ar.alloc([128, D], BF16) for _ in range(2)]
        assert ar.mark() - mU == 8 * D
        prm = ar.alloc([128, D], F32)
        gF_row = ar.alloc([128, D], F32)

        s_prm = sem("s_prm")
        s_gf = sem("s_gf")
        s_br = sem("s_br")
        s_wr = sem("s_wr")

        P.op("pool", lambda e: e.iota(io_i[:], pattern=[[1, 128]], base=0, channel_multiplier=-1),
             writes=["io_i"])
        P.op("dve", lambda e: e.tensor_single_scalar(out=identf[:], in_=io_i[:], scalar=0, op=ALU.is_equal),
             reads=["io_i"], writes=["identf"])
        P.op("dve", lambda e: e.tensor_copy(out=identb[:], in_=identf[:]), reads=["identf"], writes=["identb"])
        P.op("dve", lambda e: e.tensor_single_scalar(out=Ltri[:], in_=io_i[:], scalar=0, op=ALU.is_gt),
             reads=["io_i"], writes=["Ltri"])
        P.op("dve", lambda e: e.memset(ones_b[:], 1.0), writes=["ones_b"])
        P.op("dve", lambda e: e.memset(ones_row[:], 1.0), writes=["ones_row"])
        P.op("dve", lambda e: e.memset(O512b[:], 1.0 / 512), writes=["O512b"])
        P.op("dve", lambda e: e.memset(O512f[:], 1.0 / 512), writes=["O512f"])
        P.op("dve", lambda e: e.memset(epsc[:], EPS), writes=["epsc"])
        P.op("pool", lambda e: e.memset(B64[:], 0.0), writes=["B64"])
        P.op("pool", lambda e: e.memset(B64[0:64, 0:64], 1.0 / 64), writes=["B64"])
        P.op("pool", lambda e: e.memset(B64[64:128, 64:128], 1.0 / 64), writes=["B64"])
        P.op("pool", lambda e: e.iota(iota_ei[:], pattern=[[1, 32]], base=0, channel_multiplier=0),
             writes=["iota_ei"])
        P.op("dve", lambda e: e.tensor_copy(out=iota_e[:], in_=iota_ei[:]), reads=["iota_ei"], writes=["iota_e"])
        P.op("pool", lambda e: e.memset(prm[:], 0.0), writes=["prm"])
        P.op("pool", lambda e: e.memset(u[:, :, 0:15], 0.0), writes=["u_halo"])
        P.op("pool", lambda e: e.memset(u[:, :, S + 15:S + 30], 0.0), writes=["u_halo"])
        P.op("pool", lambda e: e.memset(v[:, :, 0:1], 0.0), writes=["v_halo"])
        P.op("pool", lambda e: e.memset(v[:, :, S + 1:S + 2], 0.0), writes=["v_halo"])
        prm_rows = [(conv_a_w, 0, 3, 512), (conv_b_w, 3, 31, 512), (conv_b_bias, 34, 1, 512),
                    (ln_b_g, 35, 1, 512), (ln_b_b, 36, 1, 512), (beta_a, 37, 1, 512),
                    (beta_b, 38, 1, 512), (norm_mix_g, 39, 1, D), (norm_ffn_g, 40, 1, D)]
        for (src, r0, nr, w_) in prm_rows:
            P.dma("sp", lambda e, src=src, r0=r0, nr=nr, w_=w_: e.dma_start(out=prm[r0:r0 + nr, 0:w_], in_=src),
                  s_prm, writes=[("prmrow", r0)], deps=[P.last_writer["prm"]])
        P.dma("sp", lambda e: e.dma_start(out=gF_row[0:1, :], in_=norm_final_g), s_gf, writes=["gF_row"])
        P.dma("sp", lambda e: e.dma_start(out=br[0:1, 0:4], in_=b_rg), s_br, writes=[("br", 0)])
        P.dma("sp", lambda e: e.dma_start(out=br[0:1, 4:36], in_=b_re), s_br, writes=[("br", 1)])
        P.dma("sp", lambda e: e.dma_start(out=wr[:, :, 0:4], in_=w_rg.rearrange("(k p) c -> p k c", p=128)),
              s_wr, writes=[("wr", 0)])
        P.dma("sp", lambda e: e.dma_start(out=wr[:, :, 4:36], in_=w_re.rearrange("(k p) c -> p k c", p=128)),
              s_wr, writes=[("wr", 1)])
        for k in range(8):
            P.op("pe", lambda e, k=k: e.transpose(out=banks[0][:, k * 41:(k + 1) * 41],
                                                 in_=prm[0:41, k * 128:(k + 1) * 128],
                                                 identity=identf[0:41, 0:41]),
                 reads=["prm", "identf"] + [("prmrow", r[1]) for r in prm_rows], writes=[("bank", 0)])
        P.op("dve", lambda e: e.tensor_copy(out=prmT[:], in_=banks[0][:, 0:8 * 41].rearrange("p (k c) -> p k c", c=41)),
             reads=[("bank", 0)], writes=["prmT"])
        for h in range(2):
            P.op("pe", lambda e, h=h: e.matmul(banks[1 + h][:, :], lhsT=ones_row[0:1, :],
                                              rhs=gF_row[0:1, h * 512:(h + 1) * 512], start=True, stop=True),
                 reads=["ones_row", "gF_row"], writes=[("bank", 1 + h)])
            P.op("dve", lambda e, h=h: e.tensor_copy(out=gF_bc[:, h * 512:(h + 1) * 512], in_=banks[1 + h][:, :]),
                 reads=[("bank", 1 + h)], writes=["gF_bc"])
        P.op("pe", lambda e: e.matmul(banks[3][:, 0:36], lhsT=ones_row[0:1, :], rhs=br[0:1, :], start=True, stop=True),
             reads=["ones_row", ("br", 0), ("br", 1)], writes=[("bank", 3)])
        P.op("dve", lambda e: e.tensor_copy(out=br_bc[:], in_=banks[3][:, 0:36]), reads=[("bank", 3)], writes=["br_bc"])
        P.op("dve", lambda e: e.tensor_tensor(out=wr[:], in0=wr[:],
                                              in1=prmT[:, :, 40:41].broadcast_to([128, 8, 36]), op=ALU.mult),
             reads=[("wr", 0), ("wr", 1), "prmT"], writes=["wr"])
        hT = ar.alloc([128, 8, S], BF16)
        winb = ar.alloc([128, 8, 2560], BF16)
        tmpA = [ar.alloc([128, QW], F32) for _ in range(2)]
        s_xb = [sem(f"s_xb{i}") for i in range(3)]
        zt = ar.alloc([128, 2048], BF16)
        bigt = ar.alloc([128, NE * CAP * 16 // 128], I32)
        s_init = sem("s_init")
        P.op("pool", lambda e: e.memset(zt[:], 0.0), writes=["zt"])
        P.op("pool", lambda e: e.memset(bigt[:], BIGI), writes=["bigt"])
        init_ops = []
        init_list = [("inv", 0)] + [("yt", r0) for r0 in range(0, 2 * S, 2048)]
        init_pos = [0]

        def init_next(dep):
            if init_pos[0] >= len(init_list):
                return
            kind, r0 = init_list[init_pos[0]]
            init_pos[0] += 1
            if kind == "inv":
                init_ops.append(P.dma("sp", lambda e: e.dma_start(
                    out=inv_d.rearrange("(p a) c -> p (a c)", p=128), in_=bigt[:]), s_init, reads=["bigt"], deps=[dep]))
                return
            dst = yt_d
            init_ops.append(P.dma("sp", lambda e, r0=r0, dst=dst: e.dma_start(
                out=dst[r0:r0 + 2048, :].rearrange("(p a2 a1) d -> p a2 (a1 d)", p=128, a2=8, a1=2),
                in_=zt[:, None, :].broadcast_to([128, 8, 2048])), s_init, reads=["zt"], deps=[dep]))
        s_pre = [sem(f"s_pre{i}") for i in range(NPRE)]
        pre_list = [(e_, m) for e_ in range(NPRE) for m in range(3)]
        pre_pos = [0]

        def precast_next(dep):
            if pre_pos[0] >= len(pre_list):
                return
            e_, m = pre_list[pre_pos[0]]
            pre_pos[0] += 1
            src = (w1, w3, w2)[m][e_].rearrange("(k p) f -> p k f", p=128)
            dst = (w1p, w3p, w2p)[m][e_].rearrange("p (k f) -> p k f", k=(8, 8, 4)[m])
            P.dma("pool", lambda e, src=src, dst=dst: e.dma_start(out=dst, in_=src), s_pre[e_],
                  writes=[("wp", e_, m)], deps=[dep], nofence=True)
        s_win = [sem(f"s_win{i}") for i in range(5)]
        def load_win(blk, deps=()):
            P.dma("pool", lambda e, blk=blk: e.dma_start(
                out=winb[:, :, blk * 512:(blk + 1) * 512],
                in_=w_in[:, blk * 512:(blk + 1) * 512].rearrange("(k p) c -> p k c", p=128)),
                s_win[blk], writes=[("winb", blk)], deps=deps)
        load_win(3)
        load_win(4)
        win_after = {7: 0, 11: 2, 15: 1}
        def ph0_prep(i):
            b = i % 3
            n = i % 2
            xl = P.dma("sp", lambda e, i=i, b=b: e.dma_start(out=xb[b][:], in_=x[i * 128:(i + 1) * 128, :]),
                       s_xb[b], writes=[("xb", b)])
            if i in win_after:
                load_win(win_after[i], deps=[xl])
            P.op("act", lambda e, i=i, b=b: e.activation(out=junkb[:], in_=xb[b][:], func=AF.Square,
                                                         accum_out=ss1[:, i:i + 1]),
                 reads=[("xb", b)], writes=["junkb", ("ss1", i)])
            P.op("act", lambda e, i=i: e.activation(out=rs1[:, i:i + 1], in_=ss1[:, i:i + 1], func=AF.Ln,
                                                    bias=epsc[:, 0:1], scale=1.0 / D),
                 reads=[("ss1", i), "epsc"], writes=[("rs1", i)])
            P.op("act", lambda e, i=i: e.activation(out=rs1[:, i:i + 1], in_=rs1[:, i:i + 1], func=AF.Exp,
                                                    scale=-0.5),
                 reads=[("rs1", i)], writes=[("rs1", i)])
            P.op("dve", lambda e, i=i, b=b, n=n: e.tensor_scalar(out=xnb[n][:], in0=xb[b][:],
                                                                scalar1=rs1[:, i:i + 1], scalar2=None, op0=ALU.mult),
                 reads=[("xb", b), ("rs1", i)], writes=[("xnb", n)])

        def ph0_tr(i):
            n = i % 2
            pb = banks[i % 2]
            pT = pb[:, :].bitcast(BF16).rearrange("p (k t) -> p k t", t=128)
            for k in range(8):
                P.op("pe", lambda e, k=k, n=n, pT=pT: e.transpose(out=pT[:, k, :], in_=xnb[n][:, k * 128:(k + 1) * 128],
                                                                 identity=identb[:]),
                     reads=[("xnb", n), "identb"], writes=[("bank", i % 2)])
            P.op("dve", lambda e, i=i, pT=pT: e.tensor_tensor(
                out=hT[:, :, i * 128:(i + 1) * 128], in0=pT,
                in1=prmT[:, :, 39:40].broadcast_to([128, 8, 128]), op=ALU.mult),
                reads=[("bank", i % 2), "prmT"], writes=[("hT", i)])

        def proj(bank, col0, q):
            for k in range(8):
                P.op("pe", lambda e, k=k, bank=bank, col0=col0, q=q: e.matmul(
                    banks[bank][:, :], lhsT=winb[:, k, col0:col0 + 128], rhs=hT[:, k, q * QW:(q + 1) * QW],
                    start=(k == 0), stop=(k == 7)),
                    reads=[("winb", col0 // 512)] + [("hT", 4 * q + t_) for t_ in range(4)], writes=[("bank", bank)])

        step_c = [0]

        def stepA(j, q):
            step = step_c[0]
            b0 = 2 + 2 * (step % 3)
            step += 1
            step_c[0] = step
            proj(b0, 1536 + j * 128, q)
            proj(b0 + 1, 2048 + j * 128, q)
            t = tmpA[step % 2]
            tk = ("tmpA", step % 2)
            P.op("act", lambda e, t=t, b0=b0: e.activation(out=t[:], in_=banks[b0 + 1][:, :], func=AF.Exp,
                                                           scale=-1.0),
                 reads=[("bank", b0 + 1)], writes=[tk])
            P.op("act", lambda e, t=t: e.activation(out=t[:], in_=t[:], func=AF.Ln, bias=ones_row[:, 0:1], scale=1.0),
                 reads=[tk, "ones_row"], writes=[tk])
            P.op("act", lambda e, t=t: e.activation(out=t[:], in_=t[:], func=AF.Exp, scale=-1.0),
                 reads=[tk], writes=[tk])
            P.op("dve", lambda e, t=t, b0=b0, j=j, q=q: e.tensor_tensor(
                out=u[:, j, 15 + q * QW:15 + (q + 1) * QW], in0=banks[b0][:, :], in1=t[:], op=ALU.mult),
                reads=[tk, ("bank", b0)], writes=[("u", j, q)])

        PRO = 5
        ph0_prep(0)
        ph0_prep(1)
        for i in range(PRO):
            ph0_tr(i)
            if i + 2 <= PRO:
                ph0_prep(i + 2)
        sA = 0
        for q in range(NQ):
            for j in range(4):
                t_ = PRO + sA
                sA += 1
                if t_ + 1 < NT:
                    ph0_prep(t_ + 1)
                stepA(j, q)
                if t_ < NT:
                    ph0_tr(t_)
        step = step_c[0]
        for j in range(4):
            for tp in range(34):
                eng = "pool" if (tp % 2 == 0) else "dve"
                P.op(eng, lambda e, j=j, tp=tp: e.tensor_scalar(
                    out=Dg[:, j * 34 + tp, :], in0=identf[:], scalar1=prmT[:, j, tp:tp + 1],
                    scalar2=1.0, op0=ALU.mult, op1=ALU.mult),
                    reads=["identf", "prmT"], writes=[("Dg", j, tp)])

        s_wout = [sem(f"s_wout{i}") for i in range(2)]
        for h in range(2):
            P.dma("pool", lambda e, h=h: e.dma_start(
                out=woutb[:, :, h * 512:(h + 1) * 512],
                in_=w_out[:, h * 512:(h + 1) * 512].rearrange("(k p) c -> p k c", p=128)),
                s_wout[h], writes=[("woutb", h)] + [("xb", b_) for b_ in range(3)] + [("xnb", n_) for n_ in range(2)])
        for j in range(4):
            for q in range(NQ):
                b0 = 2 + 2 * (step % 3)
                step += 1
                proj(b0, 0 + j * 128, q)
                proj(b0 + 1, 1024 + j * 128, q)
                t = tmpA[step % 2]
                P.op("act", lambda e, t=t, b0=b0: e.activation(out=t[:], in_=banks[b0][:, :], func=AF.Copy),
                     reads=[("bank", b0)], writes=[("tmpA", step % 2)])
                P.op("dve", lambda e, t=t, b0=b0, j=j, q=q: e.tensor_tensor(
                    out=v[:, j, 1 + q * QW:1 + (q + 1) * QW], in0=banks[b0 + 1][:, :], in1=t[:], op=ALU.mult),
                    reads=[("tmpA", step % 2), ("bank", b0 + 1)], writes=[("v", j, q)])
                if step % 4 == 0:
                    precast_next(P.ops[-1])
                if step % 3 == 1:
                    init_next(P.ops[-1])
        for j in range(4):
            for q in range(NQ):
                b0 = 2 + (step % 6)
                step += 1
                proj(b0, 512 + j * 128, q)
                P.op("act", lambda e, b0=b0, j=j, q=q: e.activation(
                    out=ab[:, j, q * QW:(q + 1) * QW], in_=banks[b0][:, :], func=AF.Copy),
                    reads=[("bank", b0)], writes=[("ab", j, q)])
                if step % 4 == 0:
                    precast_next(P.ops[-1])
                if step % 3 == 1:
                    init_next(P.ops[-1])

        while init_pos[0] < len(init_list):
            init_next(P.ops[-1])
        if debug:
            s_dbg = sem("s_dbg")
            dbg_ops = []
            dbg_ops.append(P.dma("sp", lambda e: e.dma_start(out=dbg["u"], in_=u[:]), s_dbg,
                                 reads=[("u", j, q) for j in range(4) for q in range(4)] + ["u_halo"]))
            dbg_ops.append(P.dma("sp", lambda e: e.dma_start(out=dbg["v"], in_=v[:]), s_dbg,
                                 reads=[("v", j, q) for j in range(4) for q in range(4)] + ["v_halo"]))
            dbg_ops.append(P.dma("sp", lambda e: e.dma_start(out=dbg["ab"], in_=ab[:]), s_dbg,
                                 reads=[("ab", j, q) for j in range(4) for q in range(4)]))

        P.set_fence()
        ar.reset(mU)
        ar.alloc([128, 8, D], BF16)
        cbuf = [ar.alloc([128, 4, QW], F32) for _ in range(2)]
        csq = [ar.alloc([128, 4, QW], BF16) for _ in range(2)]
        mean_sb = ar.alloc([128, QW], F32)
        var_sb = ar.alloc([128, QW], F32)
        t_sb = [ar.alloc([128, QW], F32) for _ in range(4)]
        yab = [ar.alloc([128, QW], F32) for _ in range(4)]
        ysq = [ar.alloc([128, QW], BF16) for _ in range(4)]
        rsH = [ar.alloc([128, QW], F32) for _ in range(2)] * 2
        yT = [ar.alloc([128, 8, QW], BF16) for _ in range(2)]
        xr = [ar.alloc([128, D], F32) for _ in range(2)]
        xsb = [ar.alloc([128, D], BF16) for _ in range(4)]
        lgs4 = ar.alloc([128, 4, 36], F32)
        rq = ar.alloc([128, 8, 4], F32)
        ohg4 = ar.alloc([128, 4, 4], F32)
        d4 = ar.alloc([128, 4, 4], F32)
        t48_4 = ar.alloc([128, 4, 4, 8], F32)
        lsel4 = ar.alloc([128, 4, 8], F32)
        oh1_4 = ar.alloc([128, 4, 8], F32)
        l2_4 = ar.alloc([128, 4, 8], F32)
        oh2_4 = ar.alloc([128, 4, 8], F32)
        E1_4 = ar.alloc([128, 4, 4, 8], F32)
        E2_4 = ar.alloc([128, 4, 4, 8], F32)
        pr4 = ar.alloc([128, 4, 32], F32)
        s4 = ar.alloc([128, 4, 2], F32)
        eid4 = ar.alloc([128, 4, 2], F32)
        rf4 = ar.alloc([128, 4, 2], F32)
        xsT = ar.alloc([128, 8, 128], F32)
        s_xr = [sem(f"s_xr{i}") for i in range(2)]
        s_x1 = [sem(f"s_x1{i}") for i in range(2)]
        s_sc = [sem(f"s_sc{i}") for i in range(4)]
        s_si = [sem(f"s_si{i}") for i in range(4)]
        invsrc = ar.alloc([128, NT * 2, 16], I32)
        P.op("pool", lambda e: e.iota(invsrc[:].rearrange("p (i k) c -> p i k c", k=2),
                                      pattern=[[256, NT], [1, 2], [0, 16]], base=0, channel_multiplier=2),
             writes=["invsrc"])
        P.op("pool", lambda e: e.iota(invsrc[:].rearrange("p (i k) c -> p i k c", k=2)[:, :, :, 8:16],
                                      pattern=[[128, NT], [0, 2], [0, 8]], base=0, channel_multiplier=1),
             writes=["invsrc"])
        scat_ops = []
        xs_ops = []
        x1_ops = {}

        def convB(q, j):
            bk = j % 2
            cb = cbuf[q % 2]
            cs = csq[q % 2]
            for tp in range(31):
                P.op("pe", lambda e, j=j, tp=tp, bk=bk, q=q: e.matmul(
                    banks[bk][:, :], lhsT=Dg[:, j * 34 + 3 + tp, :],
                    rhs=u[:, j, q * QW + tp:q * QW + tp + QW], start=(tp == 0), stop=(tp == 30)),
                    reads=[("Dg", j, 3 + tp)] + [("u", j, qq) for qq in (q - 1, q, q + 1) if 0 <= qq < NQ] + ["u_halo"],
                    writes=[("bank", bk)])
            P.op("act", lambda e, j=j, bk=bk, cb=cb: e.activation(
                out=cb[:, j, :], in_=banks[bk][:, :], func=AF.Identity, bias=prmT[:, j, 34:35], scale=1.0),
                reads=[("bank", bk), "prmT"], writes=[("cb", q % 2, j)])
            P.op("act", lambda e, j=j, bk=bk, cs=cs: e.activation(
                out=cs[:, j, :], in_=banks[bk][:, :], func=AF.Square, bias=prmT[:, j, 34:35], scale=1.0),
                reads=[("bank", bk), "prmT"], writes=[("cs", q % 2, j)])
            precast_next(P.ops[-1])

        def convA(q, j):
            bk = j % 2
            for tp in range(3):
                P.op("pe", lambda e, j=j, tp=tp, bk=bk, q=q: e.matmul(
                    banks[bk][:, :], lhsT=Dg[:, j * 34 + tp, :],
                    rhs=v[:, j, q * QW + tp:q * QW + tp + QW], start=(tp == 0), stop=(tp == 2)),
                    reads=[("Dg", j, tp)] + [("v", j, qq) for qq in (q - 1, q, q + 1) if 0 <= qq < NQ] + ["v_halo"],
                    writes=[("bank", bk)])
            P.op("dve", lambda e, j=j, bk=bk, q=q: e.tensor_tensor(
                out=yab[j][:], in0=banks[bk][:, :], in1=ab[:, j, q * QW:(q + 1) * QW], op=ALU.mult),
                reads=[("bank", bk), ("ab", j, q)], writes=[("yab", j)])
            P.op("pool", lambda e, j=j: e.tensor_tensor(out=ysq[j][:], in0=yab[j][:], in1=yab[j][:], op=ALU.mult),
                 reads=[("yab", j)], writes=[("ysq", j)])

        def head_stats(q, j, beta_col, ych):
            sbk = 2 + (j % 2)
            yq = yT[q % 2]
            P.op("pe", lambda e, j=j, sbk=sbk: e.matmul(banks[sbk][:, :], lhsT=B64[:], rhs=ysq[j][:],
                                                       start=True, stop=True),
                 reads=["B64", ("ysq", j)], writes=[("bank", sbk)])
            P.op("act", lambda e, j=j, sbk=sbk: e.activation(out=rsH[j][:], in_=banks[sbk][:, :], func=AF.Ln,
                                                             bias=epsc[:, 0:1], scale=1.0),
                 reads=[("bank", sbk), "epsc"], writes=[("rsH", j % 2)])
            P.op("act", lambda e, j=j: e.activation(out=rsH[j][:], in_=rsH[j][:], func=AF.Exp, scale=-0.5),
                 reads=[("rsH", j % 2)], writes=[("rsH", j % 2)])
            P.op("dve", lambda e, j=j, yq=yq, ych=ych, beta_col=beta_col: e.scalar_tensor_tensor(
                out=yq[:, ych, :], in0=yab[j][:], scalar=prmT[:, j, beta_col:beta_col + 1], in1=rsH[j][:],
                op0=ALU.mult, op1=ALU.mult),
                reads=[("yab", j), ("rsH", j % 2), "prmT"], writes=[("yT", q % 2, ych)])

        def S2(q):
            cb = cbuf[q % 2]
            cs = csq[q % 2]
            for j in range(4):
                P.op("pe", lambda e, j=j, cb=cb: e.matmul(banks[2][:, :], lhsT=O512f[:], rhs=cb[:, j, :],
                                                         start=(j == 0), stop=(j == 3)),
                     reads=["O512f", ("cb", q % 2, j)], writes=[("bank", 2)])
            for j in range(4):
                P.op("pe", lambda e, j=j, cs=cs: e.matmul(banks[3][:, :], lhsT=O512b[:], rhs=cs[:, j, :],
                                                         start=(j == 0), stop=(j == 3)),
                     reads=["O512b", ("cs", q % 2, j)], writes=[("bank", 3)])
            P.op("act", lambda e: e.activation(out=mean_sb[:], in_=banks[2][:, :], func=AF.Copy),
                 reads=[("bank", 2)], writes=["mean_sb"])
            P.op("dve", lambda e: e.tensor_tensor(out=var_sb[:], in0=mean_sb[:], in1=mean_sb[:], op=ALU.mult),
                 reads=["mean_sb"], writes=["var_sb"])
            P.op("dve", lambda e: e.tensor_tensor(out=var_sb[:], in0=banks[3][:, :], in1=var_sb[:], op=ALU.subtract),
                 reads=[("bank", 3), "var_sb"], writes=["var_sb"])
            P.op("act", lambda e: e.activation(out=var_sb[:], in_=var_sb[:], func=AF.Ln, bias=epsc[:, 0:1], scale=1.0),
                 reads=["var_sb", "epsc"], writes=["var_sb"])
            P.op("act", lambda e: e.activation(out=var_sb[:], in_=var_sb[:], func=AF.Exp, scale=-0.5),
                 reads=["var_sb"], writes=["var_sb"])
            for j in range(4):
                head_stats(q, j, 37, j)

        def S3(q):
            cb = cbuf[q % 2]
            for j in range(4):
                P.op("dve", lambda e, j=j, cb=cb: e.tensor_tensor(out=t_sb[j][:], in0=cb[:, j, :], in1=mean_sb[:],
                                                                 op=ALU.subtract),
                     reads=[("cb", q % 2, j), "mean_sb"], writes=[("t_sb", j)])
            for j in range(4):
                P.op("dve", lambda e, j=j: e.tensor_tensor(out=t_sb[j][:], in0=t_sb[j][:], in1=var_sb[:], op=ALU.mult),
                     reads=[("t_sb", j), "var_sb"], writes=[("t_sb", j)])
            for j in range(4):
                P.op("act", lambda e, j=j: e.activation(
                    out=yab[j][:], in_=t_sb[j][:], func=AF.Silu, bias=prmT[:, j, 36:37], scale=prmT[:, j, 35:36]),
                    reads=[("t_sb", j), "prmT"], writes=[("yab", j)])
            for j in range(4):
                P.op("pool", lambda e, j=j: e.tensor_tensor(out=ysq[j][:], in0=yab[j][:], in1=yab[j][:], op=ALU.mult),
                     reads=[("yab", j)], writes=[("ysq", j)])

        def S4(q):
            for j in range(4):
                head_stats(q, j, 38, 4 + j)

        wpre = []

        def S5a(q, tl):
            i = q * 4 + tl
            p2 = i % 2
            yq = yT[q % 2]
            P.dma("sp", lambda e, i=i, p2=p2: e.dma_start(out=xr[p2][:], in_=x[i * 128:(i + 1) * 128, :]),
                  s_xr[p2], writes=[("xr", p2, 0), ("xr", p2, 1)])
            if wpre:
                e0_, m0_ = wpre.pop(0)
                load_w(e0_, extra_writes=U_NAMES, parts=(m0_,))
            for h in range(2):
                for c in range(8):
                    P.op("pe", lambda e, h=h, c=c, tl=tl, yq=yq: e.matmul(
                        banks[4 + h][:, :], lhsT=yq[:, c, tl * 128:(tl + 1) * 128],
                        rhs=woutb[:, c, h * 512:(h + 1) * 512], start=(c == 0), stop=(c == 7)),
                        reads=[("yT", q % 2, c), ("woutb", h)], writes=[("bank", 4 + h)])
                P.op("dve", lambda e, h=h, p2=p2: e.tensor_tensor(
                    out=xr[p2][:, h * 512:(h + 1) * 512], in0=banks[4 + h][:, :],
                    in1=xr[p2][:, h * 512:(h + 1) * 512], op=ALU.add),
                    reads=[("bank", 4 + h), ("xr", p2, h)], writes=[("xr", p2, h)])
            xk = [("xr", p2, 0), ("xr", p2, 1)]
            x1_ops[i] = P.dma("sp", lambda e, i=i, p2=p2: e.dma_start(out=x1_d[i * 128:(i + 1) * 128, :], in_=xr[p2][:]),
                              s_x1[p2], reads=xk, writes=[("x1_d", i)])
            P.op("act", lambda e, i=i, p2=p2: e.activation(out=junkb[:], in_=xr[p2][:], func=AF.Square,
                                                           accum_out=ss2[:, i:i + 1]),
                 reads=xk, writes=["junkb", ("ss2", i)])
            P.op("act", lambda e, i=i: e.activation(out=rs2[:, i:i + 1], in_=ss2[:, i:i + 1], func=AF.Ln,
                                                    bias=epsc[:, 0:1], scale=1.0 / D),
                 reads=[("ss2", i), "epsc"], writes=[("rs2", i)])
            P.op("act", lambda e, i=i: e.activation(out=rs2[:, i:i + 1], in_=rs2[:, i:i + 1], func=AF.Exp,
                                                    scale=-0.5),
                 reads=[("rs2", i)], writes=[("rs2", i)])
            P.op("pool", lambda e, i=i, p2=p2, tl=tl: e.tensor_scalar(out=xsb[tl][:], in0=xr[p2][:],
                                                                     scalar1=rs2[:, i:i + 1], scalar2=1.0,
                                                                     op0=ALU.mult, op1=ALU.mult),
                 reads=xk + [("rs2", i)], writes=[("xsb", tl)])
            xs_ops.append(P.dma("pool", lambda e, i=i, tl=tl: e.dma_start(out=xs_d[i * 128:(i + 1) * 128, :], in_=xsb[tl][:]),
                                s_sc[tl], reads=[("xsb", tl)], writes=[("xs_d", i)]))

        def S5b(q, tl):
            i = q * 4 + tl
            p2 = i % 2
            xk = [("xr", p2, 0), ("xr", p2, 1)]
            for hh in range(2):
                tbk = (6, 2, 3)[(2 * i + hh) % 3]
                pT6 = banks[tbk][:, :].rearrange("p (k t) -> p k t", t=128)
                for k4 in range(4):
                    k = hh * 4 + k4
                    P.op("pe", lambda e, k=k, k4=k4, p2=p2, pT6=pT6: e.transpose(
                        out=pT6[:, k4, :], in_=xr[p2][:, k * 128:(k + 1) * 128], identity=identf[:]),
                        reads=xk + ["identf"], writes=[("bank", tbk)])
                P.op("dve", lambda e, hh=hh, pT6=pT6: e.tensor_copy(out=xsT[:, hh * 4:(hh + 1) * 4, :], in_=pT6),
                     reads=[("bank", tbk)], writes=[("xsT", hh)])
            lgp = banks[7][:, tl * 36:(tl + 1) * 36]
            for k in range(8):
                P.op("pe", lambda e, k=k, lgp=lgp: e.matmul(lgp, lhsT=xsT[:, k, :], rhs=wr[:, k, :],
                                                           start=(k == 0), stop=(k == 7)),
                     reads=[("xsT", k // 4), "wr"], writes=[("b7lg", tl)])
            P.op("dve", lambda e, i=i, tl=tl, lgp=lgp: e.scalar_tensor_tensor(
                out=lgs4[:, tl, :], in0=lgp, scalar=rs2[:, i:i + 1], in1=br_bc[:], op0=ALU.mult, op1=ALU.add),
                reads=[("b7lg", tl), ("rs2", i), "br_bc"], writes=[("lgs4", tl)])
            if debug:
                dbg_ops.append(P.dma("sp", lambda e, i=i, tl=tl: e.dma_start(out=dbg["lg"][i], in_=lgs4[:, tl, :]), s_dbg,
                                     reads=[("lgs4", tl)]))

        def bc(ap2, shape):
            return ap2.broadcast_to(shape)

        def RT(q):
            R = "rt"
            T0 = q * 4
            lg_g = lgs4[:, :, 0:4]
            P.op("dve", lambda e: e.tensor_reduce(out=rq[:, 0, :], in_=lg_g, axis=AX.X, op=ALU.max),
                 reads=[("lgs4", t_) for t_ in range(4)], writes=[R])
            P.op("dve", lambda e: e.tensor_tensor(out=ohg4[:], in0=lg_g, in1=bc(rq[:, 0, :, None], [128, 4, 4]),
                                                  op=ALU.is_ge), reads=[R], writes=[R])
            P.op("dve", lambda e: e.tensor_tensor(out=d4[:], in0=lg_g, in1=bc(rq[:, 0, :, None], [128, 4, 4]),
                                                  op=ALU.subtract), reads=[R], writes=[R])
            P.op("act", lambda e: e.activation(out=d4[:], in_=d4[:], func=AF.Exp), reads=[R], writes=[R])
            P.op("dve", lambda e: e.tensor_reduce(out=rq[:, 1, :], in_=d4[:], axis=AX.X, op=ALU.add),
                 reads=[R], writes=[R])
            P.op("dve", lambda e: e.reciprocal(out=rq[:, 2, :], in_=rq[:, 1, :]), reads=[R], writes=[R])
            le4 = lgs4[:, :, 4:36].rearrange("p t (g x) -> p t g x", x=8)
            P.op("dve", lambda e: e.tensor_tensor(out=t48_4[:], in0=le4, in1=bc(ohg4[:, :, :, None], [128, 4, 4, 8]),
                                                  op=ALU.mult), reads=[R], writes=[R])
            P.op("dve", lambda e: e.tensor_reduce(out=lsel4[:], in_=t48_4[:].rearrange("p t g x -> p t x g"),
                                                  axis=AX.X, op=ALU.add), reads=[R], writes=[R])
            P.op("dve", lambda e: e.tensor_reduce(out=rq[:, 3, :], in_=lsel4[:], axis=AX.X, op=ALU.max),
                 reads=[R], writes=[R])
            P.op("dve", lambda e: e.tensor_tensor(out=oh1_4[:], in0=lsel4[:], in1=bc(rq[:, 3, :, None], [128, 4, 8]),
                                                  op=ALU.is_ge), reads=[R], writes=[R])
            fl = lambda t_: t_[:].rearrange("p t x -> p (t x)")
            P.op("dve", lambda e: e.scalar_tensor_tensor(out=fl(l2_4), in0=fl(oh1_4), scalar=-1e30, in1=fl(lsel4),
                                                         op0=ALU.mult, op1=ALU.add), reads=[R], writes=[R])
            P.op("dve", lambda e: e.tensor_reduce(out=rq[:, 4, :], in_=l2_4[:], axis=AX.X, op=ALU.max),
                 reads=[R], writes=[R])
            P.op("dve", lambda e: e.tensor_tensor(out=oh2_4[:], in0=l2_4[:], in1=bc(rq[:, 4, :, None], [128, 4, 8]),
                                                  op=ALU.is_ge), reads=[R], writes=[R])
            P.op("dve", lambda e: e.tensor_tensor(out=rq[:, 5, :], in0=rq[:, 4, :], in1=rq[:, 3, :],
                                                  op=ALU.subtract), reads=[R], writes=[R])
            P.op("act", lambda e: e.activation(out=rq[:, 5, :], in_=rq[:, 5, :], func=AF.Exp), reads=[R], writes=[R])
            P.op("dve", lambda e: e.tensor_scalar(out=rq[:, 5, :], in0=rq[:, 5, :], scalar1=1.0, scalar2=None,
                                                  op0=ALU.add), reads=[R], writes=[R])
            P.op("dve", lambda e: e.reciprocal(out=rq[:, 6, :], in_=rq[:, 5, :]), reads=[R], writes=[R])
            P.op("dve", lambda e: e.tensor_tensor(out=cw[:, T0:T0 + 4, 0], in0=rq[:, 6, :], in1=rq[:, 2, :],
                                                  op=ALU.mult), reads=[R], writes=[R, ("cw", q)])
            P.op("dve", lambda e: e.tensor_tensor(out=cw[:, T0:T0 + 4, 1], in0=rq[:, 2, :], in1=cw[:, T0:T0 + 4, 0],
                                                  op=ALU.subtract), reads=[R], writes=[R, ("cw", q)])
            P.op("dve", lambda e: e.tensor_tensor(out=E1_4[:], in0=bc(ohg4[:, :, :, None], [128, 4, 4, 8]),
                                                  in1=bc(oh1_4[:, :, None, :], [128, 4, 4, 8]), op=ALU.mult),
                 reads=[R], writes=[R, "E4"])
            P.op("dve", lambda e: e.tensor_tensor(out=E2_4[:], in0=bc(ohg4[:, :, :, None], [128, 4, 4, 8]),
                                                  in1=bc(oh2_4[:, :, None, :], [128, 4, 4, 8]), op=ALU.mult),
                 reads=[R], writes=[R, "E4"])
            P.op("dve", lambda e: e.tensor_tensor(
                out=Mb[:, T0:T0 + 4, :].rearrange("p t (g x) -> p t g x", x=8), in0=E1_4[:], in1=E2_4[:], op=ALU.add),
                reads=[R], writes=[("Mb", q)])

        def S6(q):
            T0 = q * 4
            posq = banks[7][:, 256:384].rearrange("p (t x) -> p t x", x=32)
            for tl in range(4):
                i = T0 + tl
                P.op("pe", lambda e, i=i, tl=tl: e.matmul(posq[:, tl, :], lhsT=Ltri[:], rhs=Mb[:, i, :], start=True,
                                                          stop=(i == 0)),
                     reads=["Ltri", ("Mb", q)], writes=["b7pos"])
                for jj in range(i):
                    P.op("pe", lambda e, jj=jj, i=i, tl=tl: e.matmul(posq[:, tl, :], lhsT=ones_b[:], rhs=Mb[:, jj, :],
                                                                    start=False, stop=(jj == i - 1)),
                         reads=["ones_b", ("Mb", jj // 4)], writes=["b7pos"])
            Ef = lambda E_: E_[:].rearrange("p t g x -> p t (g x)")
            S = "s6"
            for kk, E_ in enumerate((E1_4, E2_4)):
                P.op("dve", lambda e, E_=E_: e.tensor_tensor(out=pr4[:], in0=Ef(E_), in1=posq, op=ALU.mult),
                     reads=["E4", "b7pos"], writes=[S])
                P.op("dve", lambda e, kk=kk: e.tensor_reduce(out=s4[:, :, kk], in_=pr4[:], axis=AX.X, op=ALU.add),
                     reads=[S], writes=[S])
                P.op("dve", lambda e, E_=E_: e.tensor_tensor(out=pr4[:], in0=Ef(E_),
                                                             in1=bc(iota_e[:, None, :], [128, 4, 32]), op=ALU.mult),
                     reads=["E4", "iota_e", S], writes=[S])
                P.op("dve", lambda e, kk=kk: e.tensor_reduce(out=eid4[:, :, kk], in_=pr4[:], axis=AX.X, op=ALU.add),
                     reads=[S], writes=[S])
            f2 = lambda t_: t_[:].rearrange("p t k -> p (t k)")
            P.op("dve", lambda e: e.tensor_scalar(out=f2(s4), in0=f2(s4), scalar1=float(CAP - 1), scalar2=None,
                                                  op0=ALU.min), reads=[S], writes=[S])
            P.op("dve", lambda e: e.scalar_tensor_tensor(out=f2(rf4), in0=f2(eid4), scalar=float(CAP), in1=f2(s4),
                                                         op0=ALU.mult, op1=ALU.add), reads=[S], writes=[S])
            P.op("dve", lambda e: e.tensor_copy(out=ridx[:, T0:T0 + 4, :], in_=rf4[:]),
                 reads=[S], writes=[("ridx", q)])

        def S6b(q):
            T0 = q * 4
            for tl in range(4):
                i = T0 + tl
                for kk in range(2):
                    so = P.dma("pool", lambda e, i=i, kk=kk, tl=tl: e.indirect_dma_start(
                        out=inv_d[:, :], out_offset=bass.IndirectOffsetOnAxis(ap=ridx[:, i, kk:kk + 1], axis=0),
                        in_=invsrc[:, i * 2 + kk, :], in_offset=None),
                        s_si[tl], reads=[("ridx", q), "invsrc"], writes=[], deps=init_ops[0:1])
                    scat_ops.append(so)

        m2 = ar.mark()
        ar.reset(mF)
        NW = 3
        NXC0 = 5
        NXC = 13
        NYO = 6
        NY = 4
        NOT = 3
        yt = [ar.alloc([128, 2, D], BF16) for _ in range(NY)]
        ar.reset(mF)
        w1b = [None] * NW
        w3b = [None] * NW
        w2b = [None] * NW
        for sl_ in range(2):
            w1b[sl_] = ar.alloc([128, 8, 512], BF16)
            w3b[sl_] = ar.alloc([128, 8, 512], BF16)
            w2b[sl_] = ar.alloc([128, 4, D], BF16)
        assert ar.mark() <= mU, (ar.mark(), mU)
        ar.reset(mU)
        xc = [ar.alloc([128, D], F32) for _ in range(NXC0)]
        _save = ar.mark()
        ar.reset(mDg)
        xc += [ar.alloc([128, D], F32) for _ in range(NXC - NXC0)]
        assert ar.mark() <= mDg + 4 * 34 * 128
        ar.reset(_save)
        ot = [ar.alloc([128, D], F32) for _ in range(NOT)]
        w1b[2] = ar.alloc([128, 8, 512], BF16)
        w3b[2] = ar.alloc([128, 8, 512], BF16)
        w2b[2] = ar.alloc([128, 4, D], BF16)
        NXG = 3
        xg = [ar.alloc([128, NST, D], BF16) for _ in range(NXG)]
        xT = [ar.alloc([128, 8, CAP], BF16) for _ in range(2)]
        slt = [ar.alloc([128, CAP], F32) for _ in range(2)]
        actT = [ar.alloc([128, 4, CAP], BF16) for _ in range(2)]
        yo = [ar.alloc([128, D], BF16) for _ in range(NYO)]
        NINV = 6
        invs = [ar.alloc([128, NST, 16], I32) for _ in range(NINV)]
        m2end = ar.mark()
        ar.reset(m2)
        s_inv = [sem(f"s_inv{i}") for i in range(NINV)]
        s_w = [[sem(f"s_w{i}_{m}") for m in range(3)] for i in range(NW)]
        s_xg = [[sem(f"s_xg{i}_{j}") for j in range(NST)] for i in range(NXG)]
        s_yo = [sem(f"s_yo{i}") for i in range(NYO)]
        s_xc = [sem(f"s_xc{i}") for i in range(NXC)]
        s_yt = [sem(f"s_yt{i}") for i in range(NY)]
        s_ot = [sem(f"s_ot{i}") for i in range(NOT)]
        U_NAMES = [(nm, j_, q_) for nm in ("u", "v", "ab") for j_ in range(4) for q_ in range(NQ)] + ["u_halo", "v_halo"]

        s_wp = [[sem(f"s_wp{i}_{m}") for m in range(3)] for i in range(NW)]

        def load_w(e_, extra_writes=(), parts=(0, 1, 2)):
            sl = e_ % NW
            xw = list(extra_writes)
            if e_ < NPRE:
                rk = [("wp", e_, m) for m in range(3)]
                if 0 in parts:
                    P.dma("sp", lambda e, e_=e_, sl=sl: e.dma_start(
                        out=w1b[sl][:], in_=w1p[e_].rearrange("p (k f) -> p k f", k=8)), s_wp[sl][0],
                        reads=rk, writes=[("w1b", sl)] + xw)
                if 1 in parts:
                    P.dma("sp", lambda e, e_=e_, sl=sl: e.dma_start(
                        out=w3b[sl][:], in_=w3p[e_].rearrange("p (k f) -> p k f", k=8)), s_wp[sl][1],
                        reads=rk, writes=[("w3b", sl)] + xw)
                if 2 in parts:
                    P.dma("sp", lambda e, e_=e_, sl=sl: e.dma_start(
                        out=w2b[sl][:], in_=w2p[e_].rearrange("p (k f) -> p k f", k=4)), s_wp[sl][2],
                        reads=rk, writes=[("w2b", sl)] + xw)
                return
            assert tuple(parts) == (0, 1, 2)
            P.dma("pool", lambda e, e_=e_, sl=sl: e.dma_start(
                out=w1b[sl][:], in_=w1[e_].rearrange("(k p) f -> p k f", p=128)), s_w[sl][0], writes=[("w1b", sl)] + xw)
            P.dma("pool", lambda e, e_=e_, sl=sl: e.dma_start(
                out=w3b[sl][:], in_=w3[e_].rearrange("(k p) f -> p k f", p=128)), s_w[sl][1], writes=[("w3b", sl)] + xw)
            P.dma("pool", lambda e, e_=e_, sl=sl: e.dma_start(
                out=w2b[sl][:], in_=w2[e_].rearrange("(k p) f -> p k f", p=128)), s_w[sl][2], writes=[("w2b", sl)] + xw)

        for j in range(4):
            convB(0, j)
        for j in range(4):
            convA(0, j)
        def S5tile(q, tl):
            S5a(q, tl)
            if tl >= 1:
                S5b(q, tl - 1)
            if tl == 3:
                S5b(q, 3)

        for q in range(NQ):
            nxt = q + 1 < NQ
            last = q == NQ - 1
            if nxt:
                convB(q + 1, 0)
            S2(q)
            if 1 <= q < NQ - 1:
                S6(q - 1)
            if nxt:
                convB(q + 1, 1)
            if last:
                S5tile(q - 1, 0)
                S5tile(q - 1, 1)
            S3(q)
            if nxt:
                convB(q + 1, 2)
            if last:
                S5tile(q - 1, 2)
                S5tile(q - 1, 3)
            S4(q)
            if 1 <= q < NQ - 1:
                S6b(q - 1)
            if nxt:
                convB(q + 1, 3)
            if debug:
                dbg_ops.append(P.dma("sp", lambda e, q=q: e.dma_start(out=dbg["yT"][q], in_=yT[q % 2][:]), s_dbg,
                                     reads=[("yT", q % 2, c) for c in range(8)]))
            if nxt:
                for j in range(4):
                    convA(q + 1, j)
            if q == NQ - 2:
                wpre.extend([(0, 0), (0, 1), (0, 2), (1, 0), (1, 1), (1, 2)])
                continue
            if last:
                RT(q - 1)
                S5tile(q, 0)
                S6(q - 1)
                S5tile(q, 1)
                S6b(q - 1)
                S5tile(q, 2)
                S5tile(q, 3)
                RT(q)
            else:
                for tl in range(4):
                    S5tile(q, tl)
                RT(q)
        S6(NQ - 1)
        S6b(NQ - 1)
        while pre_pos[0] < len(pre_list):
            precast_next(P.ops[-1])
        if debug:
            dbg_ops.append(P.dma("sp", lambda e: e.dma_start(out=dbg["cw"], in_=cw[:]), s_dbg,
                                 reads=[("cw", q) for q in range(NQ)]))
            dbg_ops.append(P.dma("sp", lambda e: e.dma_start(out=dbg["ridx"], in_=ridx[:]), s_dbg,
                                 reads=[("ridx", q) for q in range(NQ)]))

        P.set_fence()
        ar.reset(m2end)
        g2T = prmT[:, :, 40:41]
        def load_xc(i):
            b = i % NXC
            P.dma("sp", lambda e, i=i, b=b: e.dma_start(out=xc[b][:], in_=x1_d[i * 128:(i + 1) * 128, :]),
                  s_xc[b], reads=[("x1_d", i)], writes=[("xc", b)])

        def load_inv(e_):
            sl = e_ % NINV
            P.dma("sp", lambda e, e_=e_, sl=sl: e.dma_start(
                out=invs[sl][:], in_=inv_d[e_ * CAP:(e_ + 1) * CAP, :].rearrange("(j p) c -> p j c", p=128)),
                s_inv[sl], writes=[("invs", sl)], deps=scat_ops)

        gbc_cache = []

        def gbc_reg(e):
            if not gbc_cache:
                gbc_cache.append(e.to_reg(S - 1))
            return gbc_cache[0]

        def gather_xg(e_):
            sl = e_ % NXG
            iv = e_ % NINV
            for j in range(NST):
                P.dma("pool", lambda e, sl=sl, iv=iv, j=j: e.indirect_dma_start(
                    out=xg[sl][:, j, :], out_offset=None, in_=xs_d[:, :],
                    in_offset=bass.IndirectOffsetOnAxis(ap=invs[iv][:, j, 8:9], axis=0),
                    bounds_check=gbc_reg(e), oob_is_err=False),
                    s_xg[sl][j], reads=[("invs", iv)], writes=[("xg", sl, j)], deps=xs_ops)

        for sl_ in range(NXG):
            P.op("dve", lambda e, sl_=sl_: e.memset(xg[sl_][:], 0.0), writes=[("xg", sl_, j_) for j_ in range(NST)])
        load_inv(0)
        load_inv(1)
        load_inv(2)
        gather_xg(0)
        gather_xg(1)
        for i in range(NXC0):
            load_xc(i)
        yg_ops = []
        yoc = [0]
        bc_cache = []

        def bc_reg(e):
            if not bc_cache:
                bc_cache.append(e.to_reg(2 * S - 1))
            return bc_cache[0]

        def ex_trans(e_, kp):
            p2 = e_ % 2
            gx = e_ % NXG
            tbk = kp % 2
            pTb = banks[tbk][:, :].bitcast(BF16)[:, 0:2 * CAP].rearrange("p (k t) -> p k t", t=CAP)
            for k2 in range(2):
                k = kp * 2 + k2
                for j in range(NST):
                    P.op("pe", lambda e, k=k, k2=k2, j=j, gx=gx, pTb=pTb: e.transpose(
                        out=pTb[:, k2, j * 128:(j + 1) * 128], in_=xg[gx][:, j, k * 128:(k + 1) * 128],
                        identity=identb[:]),
                        reads=[("xg", gx, j), "identb"], writes=[("bank", tbk)])
            P.op("dve", lambda e, kp=kp, p2=p2, pTb=pTb: e.tensor_tensor(
                out=xT[p2][:, kp * 2:(kp + 1) * 2, :], in0=pTb,
                in1=g2T[:, kp * 2:(kp + 1) * 2, :].broadcast_to([128, 2, CAP]), op=ALU.mult),
                reads=[("bank", tbk), "prmT"], writes=[("xT", p2, kp)])

        def ex_h13(e_):
            sl = e_ % NW
            p2 = e_ % 2
            for f in range(4):
                b1 = 2 + (f % 2)
                b3 = 4 + (f % 2)
                for k in range(8):
                    P.op("pe", lambda e, k=k, f=f, b1=b1, sl=sl, p2=p2: e.matmul(
                        banks[b1][:, 0:CAP], lhsT=w1b[sl][:, k, f * 128:(f + 1) * 128], rhs=xT[p2][:, k, :],
                        start=(k == 0), stop=(k == 7)),
                        reads=[("w1b", sl), ("xT", p2, k // 2)], writes=[("bank", b1)])
                for k in range(8):
                    P.op("pe", lambda e, k=k, f=f, b3=b3, sl=sl, p2=p2: e.matmul(
                        banks[b3][:, 0:CAP], lhsT=w3b[sl][:, k, f * 128:(f + 1) * 128], rhs=xT[p2][:, k, :],
                        start=(k == 0), stop=(k == 7)),
                        reads=[("w3b", sl), ("xT", p2, k // 2)], writes=[("bank", b3)])
                P.op("act", lambda e, f=f, b1=b1: e.activation(out=slt[f % 2][:], in_=banks[b1][:, 0:CAP], func=AF.Silu),
                     reads=[("bank", b1)], writes=[("slt", f % 2)])
                P.op("dve", lambda e, f=f, b3=b3, p2=p2: e.tensor_tensor(
                    out=actT[p2][:, f, :], in0=banks[b3][:, 0:CAP], in1=slt[f % 2][:], op=ALU.mult),
                    reads=[("bank", b3), ("slt", f % 2)], writes=[("actT", p2, f)])

        def ex_out(e_, j):
            sl = e_ % NW
            p2 = e_ % 2
            yb_ = yoc[0] % NYO
            yoc[0] += 1
            for h in range(2):
                ob = 6 + h
                for f in range(4):
                    P.op("pe", lambda e, f=f, j=j, h=h, ob=ob, sl=sl, p2=p2: e.matmul(
                        banks[ob][:, :], lhsT=actT[p2][:, f, j * 128:(j + 1) * 128],
                        rhs=w2b[sl][:, f, h * 512:(h + 1) * 512], start=(f == 0), stop=(f == 3)),
                        reads=[("w2b", sl), ("actT", p2, f)], writes=[("bank", ob)])
                if h == 0:
                    P.op("act", lambda e, h=h, ob=ob, yb_=yb_: e.activation(
                        out=yo[yb_][:, h * 512:(h + 1) * 512], in_=banks[ob][:, :], func=AF.Copy),
                        reads=[("bank", ob)], writes=[("yo", yb_, h)])
                else:
                    P.op("dve", lambda e, h=h, ob=ob, yb_=yb_: e.tensor_copy(
                        out=yo[yb_][:, h * 512:(h + 1) * 512], in_=banks[ob][:, :]),
                        reads=[("bank", ob)], writes=[("yo", yb_, h)])
            iv = e_ % NINV
            yg_ops.append(P.dma("pool", lambda e, j=j, iv=iv, yb_=yb_: e.indirect_dma_start(
                out=yt_d[:, :], out_offset=bass.IndirectOffsetOnAxis(ap=invs[iv][:, j, 0:1], axis=0),
                in_=yo[yb_][:], in_offset=None, bounds_check=bc_reg(e), oob_is_err=False),
                s_yo[yb_], reads=[("yo", yb_, 0), ("yo", yb_, 1), ("invs", iv)]))

        for kp in range(4):
            ex_trans(0, kp)
        for e_ in range(NE):
            if e_ + 2 < NE:
                gather_xg(e_ + 2)
                load_w(e_ + 2)
            if e_ + 3 < NE:
                load_inv(e_ + 3)
            if NXC0 + e_ < NXC:
                load_xc(NXC0 + e_)
            ex_h13(e_)
            nx = e_ + 1 < NE
            if nx:
                ex_trans(e_ + 1, 0)
                ex_trans(e_ + 1, 1)
            ex_out(e_, 0)
            if nx:
                ex_trans(e_ + 1, 2)
                ex_trans(e_ + 1, 3)
            ex_out(e_, 1)
            ex_out(e_, 2)

        out_ops = []

        def load_yt(i):
            yb6 = i % NY
            P.dma("sp", lambda e, i=i, yb6=yb6: e.dma_start(
                out=yt[yb6][:], in_=yt_d[i * 256:(i + 1) * 256, :].rearrange("(p k) d -> p k d", k=2)),
                s_yt[yb6], writes=[("yt", yb6)], deps=yg_ops)

        for i in range(NY):
            load_yt(i)

        def combine(i):
            yb6 = i % NY
            b = i % NXC
            P.op("dve", lambda e, i=i, yb6=yb6, b=b: e.scalar_tensor_tensor(
                out=xc[b][:], in0=yt[yb6][:, 0, :], scalar=cw[:, i, 0:1], in1=xc[b][:], op0=ALU.mult, op1=ALU.add),
                reads=[("yt", yb6), ("xc", b), ("cw", i // 4)], writes=[("xc", b)])
            P.op("dve", lambda e, i=i, yb6=yb6, b=b: e.scalar_tensor_tensor(
                out=xc[b][:], in0=yt[yb6][:, 1, :], scalar=cw[:, i, 1:2], in1=xc[b][:], op0=ALU.mult, op1=ALU.add),
                reads=[("yt", yb6), ("xc", b), ("cw", i // 4)], writes=[("xc", b)])

        combine(0)
        for i in range(NT):
            p2 = i % NOT
            b = i % NXC
            P.op("act", lambda e, i=i, b=b: e.activation(out=junkb[:], in_=xc[b][:], func=AF.Square,
                                                         accum_out=ssF[:, i:i + 1]),
                 reads=[("xc", b)], writes=["junkb", ("ssF", i)])
            P.op("act", lambda e, i=i: e.activation(out=rsF[:, i:i + 1], in_=ssF[:, i:i + 1], func=AF.Ln,
                                                    bias=epsc[:, 0:1], scale=1.0 / D),
                 reads=[("ssF", i), "epsc"], writes=[("rsF", i)])
            P.op("act", lambda e, i=i: e.activation(out=rsF[:, i:i + 1], in_=rsF[:, i:i + 1], func=AF.Exp, scale=-0.5),
                 reads=[("rsF", i)], writes=[("rsF", i)])
            if i + 1 < NT:
                combine(i + 1)
            P.op("dve", lambda e, i=i, p2=p2, b=b: e.scalar_tensor_tensor(
                out=ot[p2][:], in0=xc[b][:], scalar=rsF[:, i:i + 1], in1=gF_bc[:], op0=ALU.mult, op1=ALU.mult),
                reads=[("xc", b), ("rsF", i), "gF_bc"], writes=[("ot", p2)])
            out_ops.append(P.dma("sp", lambda e, i=i, p2=p2: e.dma_start(out=out[i * 128:(i + 1) * 128, :], in_=ot[p2][:]),
                                 s_ot[p2], reads=[("ot", p2)]))
            if i + NXC < NT:
                load_xc(i + NXC)
            if i + NY < NT:
                load_yt(i + NY)
        finals = out_ops[-NOT:] + (dbg_ops if debug else [])
        P.emit(st, final_waits=finals)
        if debug:
            print('arena mF', mF, 'mU', mU, 'peak', ar.peak, 'ARN', ARN, 'nops', len(P.ops), 'nsem', nsem[0])
    return nc


_IN_NAMES = ["norm_mix_g", "w_in", "conv_a_w", "conv_b_w", "conv_b_bias", "ln_b_g", "ln_b_b", "beta_a", "beta_b",
             "w_out", "norm_ffn_g", "w_route_group", "b_route_group", "w_route_expert", "b_route_expert",
             "w1", "w3", "w2"]


def make_in_maps(inputs):
    f = lambda a: np.ascontiguousarray(np.asarray(a, dtype=np.float32))
    shared = {}
    for n in _IN_NAMES:
        a = f(inputs[n])[0]
        if a.ndim == 1:
            a = a.reshape(1, -1)
        shared[n] = np.ascontiguousarray(a)
    shared["norm_final_g"] = f(inputs["norm_final_g"]).reshape(1, -1)
    x = f(inputs["x"])
    return [dict(shared, x=np.ascontiguousarray(x[c])) for c in range(8)]


def kernel(**inputs):
    nc = build_nc()
    in_maps = make_in_maps(inputs)
    res = run_bass_kernel_spmd(nc, in_maps, core_ids=list(range(8)))
    return np.stack([np.asarray(r["out"], dtype=np.float32) for r in res.results], axis=0)
```

```python
import contextlib
import numpy as np
import concourse.bass as bass
import concourse.mybir as mybir
from concourse.bass_utils import run_bass_kernel_spmd
from concourse.alu_op_type import AluOpType as ALU

F32 = mybir.dt.float32
BF16 = mybir.dt.bfloat16
I32 = mybir.dt.int32
AF = mybir.ActivationFunctionType
AX = mybir.AxisListType

S = 2048
D = 1024
NT = 16
NQ = 4
QW = 512
NE = 32
CAP = 384
NST = CAP // 128
NPRE = 12
BIGI = 2 * S
EPS = 1e-6
ENGS = ("pe", "act", "dve", "pool", "sp")


class Op:
    __slots__ = ("eng", "fn", "reads", "writes", "deps", "sem", "count",
                 "is_dma", "has_cons", "idx", "name", "nofence")

    def __init__(self, eng, fn, reads, writes, is_dma, sem, name):
        self.eng = eng
        self.fn = fn
        self.reads = reads
        self.writes = writes
        self.deps = []
        self.sem = sem
        self.count = None
        self.is_dma = is_dma
        self.has_cons = False
        self.name = name
        self.nofence = False


class Prog:
    def __init__(self, nc):
        self.nc = nc
        self.ops = []
        self.last_writer = {}
        self.readers = {}
        self.fence = []

    def set_fence(self):
        f = set()
        for w in self.last_writer.values():
            f.add(w)
        for rs in self.readers.values():
            for r in rs:
                f.add(r)
        f = {o for o in f if not o.nofence}
        latest = {}
        for o in f:
            k = (o.eng, id(o.sem) if o.is_dma else 0, o.is_dma)
            if k not in latest or latest[k].idx < o.idx:
                latest[k] = o
        keep = [o for o in f if o.is_dma] + [o for o in latest.values() if not o.is_dma]
        self.fence = keep

    def _add(self, op, extra_deps):
        deps = set()
        for k in op.reads:
            w = self.last_writer.get(k)
            if w is not None:
                deps.add(w)
        for k in op.writes:
            w = self.last_writer.get(k)
            if w is not None:
                deps.add(w)
            for r in self.readers.get(k, ()):
                deps.add(r)
        for d in extra_deps:
            if d is not None:
                deps.add(d)
        for d in self.fence:
            deps.add(d)
        deps.discard(op)
        if op.eng == "pe" and not op.is_dma:
            deps = {d for d in deps if not (d.eng == "pe" and not d.is_dma)}
        op.deps = sorted(deps, key=lambda d: d.idx)
        for d in op.deps:
            d.has_cons = True
        for k in op.reads:
            self.readers.setdefault(k, []).append(op)
        for k in op.writes:
            self.last_writer[k] = op
            self.readers[k] = []
        return op

    def op(self, eng, fn, reads=(), writes=(), deps=(), name=""):
        o = Op(eng, fn, tuple(reads), tuple(writes), False, None, name)
        o.idx = len(self.ops)
        self.ops.append(o)
        return self._add(o, deps)

    def dma(self, eng, fn, sem, reads=(), writes=(), deps=(), name="", nofence=False):
        o = Op(eng, fn, tuple(reads), tuple(writes), True, sem, name)
        o.idx = len(self.ops)
        o.has_cons = True
        o.nofence = nofence
        self.ops.append(o)
        return self._add(o, deps)

    def emit(self, st, final_waits=()):
        nc = self.nc
        esem = {e: st.enter_context(nc.semaphore("es_" + e)) for e in ENGS if e != "sp"}
        ecount = {e: 0 for e in ENGS}
        dcount = {}
        for o in self.ops:
            if o.is_dma:
                k = id(o.sem)
                dcount[k] = dcount.get(k, 0) + 16
                o.count = dcount[k]
            elif o.has_cons:
                ecount[o.eng] += 1
                o.count = ecount[o.eng]
                o.sem = esem[o.eng]
        per_eng = {e: [o for o in self.ops if o.eng == e] for e in ENGS}
        block = st.enter_context(nc.Block())

        def run(e, eo):
            waited = {}
            for o in per_eng[e]:
                need = {}
                for d in o.deps:
                    k = id(d.sem)
                    if k not in need or need[k][1] < d.count:
                        need[k] = (d.sem, d.count)
                for k, (sm, cnt) in need.items():
                    if waited.get(k, 0) >= cnt:
                        continue
                    eo.wait_ge(sm, cnt)
                    waited[k] = cnt
                ins = o.fn(eo)
                if o.is_dma:
                    ins.then_inc(o.sem, 16)
                elif o.has_cons:
                    ins.then_inc(o.sem, 1)
            if e == "sp":
                for o in final_waits:
                    eo.wait_ge(o.sem, o.count)

        @block.tensor
        def _(eo):
            run("pe", eo)

        @block.scalar
        def _(eo):
            run("act", eo)

        @block.vector
        def _(eo):
            run("dve", eo)

        @block.gpsimd
        def _(eo):
            run("pool", eo)

        @block.sync
        def _(eo):
            run("sp", eo)


class Arena:
    def __init__(self, A, n):
        self.A = A
        self.n = n
        self.off = 0

    def mark(self):
        return self.off

    def reset(self, m):
        self.off = m

    def alloc(self, shape, dt):
        size = {F32: 4, BF16: 2, I32: 4}[dt]
        assert shape[0] == 128
        ne = 1
        for s_ in shape[1:]:
            ne *= s_
        nb = ne * size
        nb = (nb + 31) // 32 * 32
        n2 = nb // 2
        self.peak = max(getattr(self, "peak", 0), self.off + n2)
        assert self.off + n2 <= self.n, ("arena overflow", self.off, n2, self.n)
        v = self.A[:, self.off:self.off + n2]
        self.off += n2
        if dt != BF16:
            v = v.bitcast(dt)
        v = v[:, 0:ne]
        if len(shape) == 3:
            return v.rearrange("p (a b) -> p a b", b=shape[2])
        if len(shape) == 4:
            return v.rearrange("p (a b c) -> p a b c", b=shape[2], c=shape[3])
        return v


def build_nc(debug=False):
    nc = bass.Bass("TRN2", target_bir_lowering=False)

    def din(name, shape, dt=F32):
        return nc.dram_tensor(name, shape, dt, kind="ExternalInput").ap()

    x = din("x", [S, D])
    norm_mix_g = din("norm_mix_g", [1, D])
    w_in = din("w_in", [D, 2560])
    conv_a_w = din("conv_a_w", [3, 512])
    conv_b_w = din("conv_b_w", [31, 512])
    conv_b_bias = din("conv_b_bias", [1, 512])
    ln_b_g = din("ln_b_g", [1, 512])
    ln_b_b = din("ln_b_b", [1, 512])
    beta_a = din("beta_a", [1, 512])
    beta_b = din("beta_b", [1, 512])
    w_out = din("w_out", [D, D])
    norm_ffn_g = din("norm_ffn_g", [1, D])
    w_rg = din("w_route_group", [D, 4])
    b_rg = din("b_route_group", [1, 4])
    w_re = din("w_route_expert", [D, 32])
    b_re = din("b_route_expert", [1, 32])
    w1 = din("w1", [NE, D, 512])
    w3 = din("w3", [NE, D, 512])
    w2 = din("w2", [NE, 512, D])
    norm_final_g = din("norm_final_g", [1, D])
    out = nc.dram_tensor("out", [S, D], F32, kind="ExternalOutput").ap()
    x1_d = nc.dram_tensor("x1_d", [S, D], F32, kind="Internal").ap()
    xs_d = nc.dram_tensor("xs_d", [S, D], BF16, kind="Internal").ap()
    yt_d = nc.dram_tensor("yt_d", [2 * S + 128, D], BF16, kind="Internal").ap()
    inv_d = nc.dram_tensor("inv_d", [NE * CAP, 16], I32, kind="Internal").ap()
    w1p = nc.dram_tensor("w1p", [NPRE, 128, 8 * 512], BF16, kind="Internal").ap()
    w3p = nc.dram_tensor("w3p", [NPRE, 128, 8 * 512], BF16, kind="Internal").ap()
    w2p = nc.dram_tensor("w2p", [NPRE, 128, 4 * D], BF16, kind="Internal").ap()
    dbg = {}
    if debug:
        dbg["u"] = nc.dram_tensor("dbg_u", [128, 4, S + 30], BF16, kind="ExternalOutput").ap()
        dbg["v"] = nc.dram_tensor("dbg_v", [128, 4, S + 2], BF16, kind="ExternalOutput").ap()
        dbg["ab"] = nc.dram_tensor("dbg_ab", [128, 4, S], BF16, kind="ExternalOutput").ap()
        dbg["x1"] = x1_d
        dbg["yT"] = nc.dram_tensor("dbg_yT", [NQ, 128, 8, QW], BF16, kind="ExternalOutput").ap()
        dbg["lg"] = nc.dram_tensor("dbg_lg", [NT, 128, 36], F32, kind="ExternalOutput").ap()
        dbg["cw"] = nc.dram_tensor("dbg_cw", [128, NT, 2], F32, kind="ExternalOutput").ap()
        dbg["ridx"] = nc.dram_tensor("dbg_ridx", [128, NT, 2], I32, kind="ExternalOutput").ap()

    with contextlib.ExitStack() as st:
        ARN = 106400
        A_t = st.enter_context(nc.sbuf_tensor("arena", [128, ARN], BF16))
        ar = Arena(A_t, ARN)
        banks = [st.enter_context(nc.psum_tensor(f"bank{i}", [128, 512], F32)) for i in range(8)]
        nsem = [0]

        def sem(name):
            nsem[0] += 1
            return st.enter_context(nc.semaphore(name))

        P = Prog(nc)

        identf = ar.alloc([128, 128], F32)
        identb = ar.alloc([128, 128], BF16)
        io_i = ar.alloc([128, 128], I32)
        B64 = ar.alloc([128, 128], BF16)
        O512b = ar.alloc([128, 128], BF16)
        O512f = ar.alloc([128, 128], F32)
        Ltri = ar.alloc([128, 128], BF16)
        ones_b = ar.alloc([128, 128], BF16)
        ones_row = ar.alloc([128, 128], F32)
        epsc = ar.alloc([128, 1], F32)
        iota_e = ar.alloc([128, 32], F32)
        iota_ei = ar.alloc([128, 32], I32)
        prmT = ar.alloc([128, 8, 41], F32)
        gF_bc = ar.alloc([128, D], F32)
        wr = ar.alloc([128, 8, 36], F32)
        br = ar.alloc([128, 36], F32)
        br_bc = ar.alloc([128, 36], F32)
        mDg = ar.mark()
        Dg = ar.alloc([128, 4 * 34, 128], BF16)
        ss1 = ar.alloc([128, NT], F32)
        rs1 = ar.alloc([128, NT], F32)
        ss2 = ar.alloc([128, NT], F32)
        rs2 = ar.alloc([128, NT], F32)
        ssF = ar.alloc([128, NT], F32)
        rsF = ar.alloc([128, NT], F32)
        cw = ar.alloc([128, NT, 2], F32)
        ridx = ar.alloc([128, NT, 2], I32)
        Mb = ar.alloc([128, NT, 32], BF16)
        lgs = ar.alloc([128, 36], F32)
        r_mg = ar.alloc([128, 8], F32)
        ohg = ar.alloc([128, 4], F32)
        eg = ar.alloc([128, 4], F32)
        t48 = ar.alloc([128, 4, 8], F32)
        lsel = ar.alloc([128, 8], F32)
        oh1 = ar.alloc([128, 8], F32)
        l2 = ar.alloc([128, 8], F32)
        oh2 = ar.alloc([128, 8], F32)
        E1 = ar.alloc([128, 4, 8], F32)
        E2 = ar.alloc([128, 4, 8], F32)
        j32 = ar.alloc([128, 32], F32)
        rsc = ar.alloc([128, 8], F32)
        junkb = ar.alloc([128, D], BF16)

        mF = ar.mark()
        u = ar.alloc([128, 4, S + 30], BF16)
        v = ar.alloc([128, 4, S + 2], BF16)
        ab = ar.alloc([128, 4, S], BF16)
        mU = ar.mark()
        woutb = ar.alloc([128, 8, D], BF16)
        ar.reset(mU)
        xb = [ar.alloc([128, D], F32) for _ in range(3)]
        xnb = [ar.alloc([128, D], BF16) for _ in range(2)]
        assert ar.mark() - mU == 8 * D
        prm = ar.alloc([128, D], F32)
        gF_row = ar.alloc([128, D], F32)

        s_prm = sem("s_prm")
        s_gf = sem("s_gf")
        s_br = sem("s_br")
        s_wr = sem("s_wr")

        P.op("pool", lambda e: e.iota(io_i[:], pattern=[[1, 128]], base=0, channel_multiplier=-1),
             writes=["io_i"])
        P.op("dve", lambda e: e.tensor_single_scalar(out=identf[:], in_=io_i[:], scalar=0, op=ALU.is_equal),
             reads=["io_i"], writes=["identf"])
        P.op("dve", lambda e: e.tensor_copy(out=identb[:], in_=identf[:]), reads=["identf"], writes=["identb"])
        P.op("dve", lambda e: e.tensor_single_scalar(out=Ltri[:], in_=io_i[:], scalar=0, op=ALU.is_gt),
             reads=["io_i"], writes=["Ltri"])
        P.op("dve", lambda e: e.memset(ones_b[:], 1.0), writes=["ones_b"])
        P.op("dve", lambda e: e.memset(ones_row[:], 1.0), writes=["ones_row"])
        P.op("dve", lambda e: e.memset(O512b[:], 1.0 / 512), writes=["O512b"])
        P.op("dve", lambda e: e.memset(O512f[:], 1.0 / 512), writes=["O512f"])
        P.op("dve", lambda e: e.memset(epsc[:], EPS), writes=["epsc"])
        P.op("pool", lambda e: e.memset(B64[:], 0.0), writes=["B64"])
        P.op("pool", lambda e: e.memset(B64[0:64, 0:64], 1.0 / 64), writes=["B64"])
        P.op("pool", lambda e: e.memset(B64[64:128, 64:128], 1.0 / 64), writes=["B64"])
        P.op("pool", lambda e: e.iota(iota_ei[:], pattern=[[1, 32]], base=0, channel_multiplier=0),
             writes=["iota_ei"])
        P.op("dve", lambda e: e.tensor_copy(out=iota_e[:], in_=iota_ei[:]), reads=["iota_ei"], writes=["iota_e"])
        P.op("pool", lambda e: e.memset(prm[:], 0.0), writes=["prm"])
        P.op("pool", lambda e: e.memset(u[:, :, 0:15], 0.0), writes=["u_halo"])
        P.op("pool", lambda e: e.memset(u[:, :, S + 15:S + 30], 0.0), writes=["u_halo"])
        P.op("pool", lambda e: e.memset(v[:, :, 0:1], 0.0), writes=["v_halo"])
        P.op("pool", lambda e: e.memset(v[:, :, S + 1:S + 2], 0.0), writes=["v_halo"])
        prm_rows = [(conv_a_w, 0, 3, 512), (conv_b_w, 3, 31, 512), (conv_b_bias, 34, 1, 512),
                    (ln_b_g, 35, 1, 512), (ln_b_b, 36, 1, 512), (beta_a, 37, 1, 512),
                    (beta_b, 38, 1, 512), (norm_mix_g, 39, 1, D), (norm_ffn_g, 40, 1, D)]
        for (src, r0, nr, w_) in prm_rows:
            P.dma("sp", lambda e, src=src, r0=r0, nr=nr, w_=w_: e.dma_start(out=prm[r0:r0 + nr, 0:w_], in_=src),
                  s_prm, writes=[("prmrow", r0)], deps=[P.last_writer["prm"]])
        P.dma("sp", lambda e: e.dma_start(out=gF_row[0:1, :], in_=norm_final_g), s_gf, writes=["gF_row"])
        P.dma("sp", lambda e: e.dma_start(out=br[0:1, 0:4], in_=b_rg), s_br, writes=[("br", 0)])
        P.dma("sp", lambda e: e.dma_start(out=br[0:1, 4:36], in_=b_re), s_br, writes=[("br", 1)])
        P.dma("sp", lambda e: e.dma_start(out=wr[:, :, 0:4], in_=w_rg.rearrange("(k p) c -> p k c", p=128)),
              s_wr, writes=[("wr", 0)])
        P.dma("sp", lambda e: e.dma_start(out=wr[:, :, 4:36], in_=w_re.rearrange("(k p) c -> p k c", p=128)),
              s_wr, writes=[("wr", 1)])
        for k in range(8):
            P.op("pe", lambda e, k=k: e.transpose(out=banks[0][:, k * 41:(k + 1) * 41],
                                                 in_=prm[0:41, k * 128:(k + 1) * 128],
                                                 identity=identf[0:41, 0:41]),
                 reads=["prm", "identf"] + [("prmrow", r[1]) for r in prm_rows], writes=[("bank", 0)])
        P.op("dve", lambda e: e.tensor_copy(out=prmT[:], in_=banks[0][:, 0:8 * 41].rearrange("p (k c) -> p k c", c=41)),
             reads=[("bank", 0)], writes=["prmT"])
        for h in range(2):
            P.op("pe", lambda e, h=h: e.matmul(banks[1 + h][:, :], lhsT=ones_row[0:1, :],
                                              rhs=gF_row[0:1, h * 512:(h + 1) * 512], start=True, stop=True),
                 reads=["ones_row", "gF_row"], writes=[("bank", 1 + h)])
            P.op("dve", lambda e, h=h: e.tensor_copy(out=gF_bc[:, h * 512:(h + 1) * 512], in_=banks[1 + h][:, :]),
                 reads=[("bank", 1 + h)], writes=["gF_bc"])
        P.op("pe", lambda e: e.matmul(banks[3][:, 0:36], lhsT=ones_row[0:1, :], rhs=br[0:1, :], start=True, stop=True),
             reads=["ones_row", ("br", 0), ("br", 1)], writes=[("bank", 3)])
        P.op("dve", lambda e: e.tensor_copy(out=br_bc[:], in_=banks[3][:, 0:36]), reads=[("bank", 3)], writes=["br_bc"])
        P.op("dve", lambda e: e.tensor_tensor(out=wr[:], in0=wr[:],
                                              in1=prmT[:, :, 40:41].broadcast_to([128, 8, 36]), op=ALU.mult),
             reads=[("wr", 0), ("wr", 1), "prmT"], writes=["wr"])
        hT = ar.alloc([128, 8, S], BF16)
        winb = ar.alloc([128, 8, 2560], BF16)
        tmpA = [ar.alloc([128, QW], F32) for _ in range(2)]
        s_xb = [sem(f"s_xb{i}") for i in range(3)]
        zt = ar.alloc([128, 2048], BF16)
        bigt = ar.alloc([128, NE * CAP * 16 // 128], I32)
        s_init = sem("s_init")
        P.op("pool", lambda e: e.memset(zt[:], 0.0), writes=["zt"])
        P.op("pool", lambda e: e.memset(bigt[:], BIGI), writes=["bigt"])
        init_ops = []
        init_list = [("inv", 0)] + [("yt", r0) for r0 in range(0, 2 * S, 2048)]
        init_pos = [0]

        def init_next(dep):
            if init_pos[0] >= len(init_list):
                return
            kind, r0 = init_list[init_pos[0]]
            init_pos[0] += 1
            if kind == "inv":
                init_ops.append(P.dma("sp", lambda e: e.dma_start(
                    out=inv_d.rearrange("(p a) c -> p (a c)", p=128), in_=bigt[:]), s_init, reads=["bigt"], deps=[dep]))
                return
            dst = yt_d
            init_ops.append(P.dma("sp", lambda e, r0=r0, dst=dst: e.dma_start(
                out=dst[r0:r0 + 2048, :].rearrange("(p a2 a1) d -> p a2 (a1 d)", p=128, a2=8, a1=2),
                in_=zt[:, None, :].broadcast_to([128, 8, 2048])), s_init, reads=["zt"], deps=[dep]))
        s_pre = [sem(f"s_pre{i}") for i in range(6)] * ((NPRE + 5) // 6)
        pre_list = [(e_, m) for e_ in range(NPRE) for m in range(3)]
        pre_pos = [0]

        def precast_next(dep):
            if pre_pos[0] >= len(pre_list):
                return
            e_, m = pre_list[pre_pos[0]]
            pre_pos[0] += 1
            src = (w1, w3, w2)[m][e_].rearrange("(k p) f -> p k f", p=128)
            dst = (w1p, w3p, w2p)[m][e_].rearrange("p (k f) -> p k f", k=(8, 8, 4)[m])
            P.dma("pool", lambda e, src=src, dst=dst: e.dma_start(out=dst, in_=src), s_pre[e_],
                  writes=[("wp", e_, m)], deps=[dep], nofence=True)
        s_win = [sem(f"s_win{i}") for i in range(5)]
        def load_win(blk, deps=()):
            P.dma("pool", lambda e, blk=blk: e.dma_start(
                out=winb[:, :, blk * 512:(blk + 1) * 512],
                in_=w_in[:, blk * 512:(blk + 1) * 512].rearrange("(k p) c -> p k c", p=128)),
                s_win[blk], writes=[("winb", blk)], deps=deps)
        load_win(3)
        load_win(4)
        win_after = {7: 0, 11: 2, 15: 1}
        def ph0_prep(i):
            b = i % 3
            n = i % 2
            xl = P.dma("sp", lambda e, i=i, b=b: e.dma_start(out=xb[b][:], in_=x[i * 128:(i + 1) * 128, :]),
                       s_xb[b], writes=[("xb", b)])
            if i in win_after:
                load_win(win_after[i], deps=[xl])
            P.op("act", lambda e, i=i, b=b: e.activation(out=junkb[:], in_=xb[b][:], func=AF.Square,
                                                         accum_out=ss1[:, i:i + 1]),
                 reads=[("xb", b)], writes=["junkb", ("ss1", i)])
            P.op("act", lambda e, i=i: e.activation(out=rs1[:, i:i + 1], in_=ss1[:, i:i + 1], func=AF.Ln,
                                                    bias=epsc[:, 0:1], scale=1.0 / D),
                 reads=[("ss1", i), "epsc"], writes=[("rs1", i)])
            P.op("act", lambda e, i=i: e.activation(out=rs1[:, i:i + 1], in_=rs1[:, i:i + 1], func=AF.Exp,
                                                    scale=-0.5),
                 reads=[("rs1", i)], writes=[("rs1", i)])
            P.op("dve", lambda e, i=i, b=b, n=n: e.tensor_scalar(out=xnb[n][:], in0=xb[b][:],
                                                                scalar1=rs1[:, i:i + 1], scalar2=None, op0=ALU.mult),
                 reads=[("xb", b), ("rs1", i)], writes=[("xnb", n)])

        def ph0_tr(i):
            n = i % 2
            pb = banks[i % 2]
            pT = pb[:, :].bitcast(BF16).rearrange("p (k t) -> p k t", t=128)
            for k in range(8):
                P.op("pe", lambda e, k=k, n=n, pT=pT: e.transpose(out=pT[:, k, :], in_=xnb[n][:, k * 128:(k + 1) * 128],
                                                                 identity=identb[:]),
                     reads=[("xnb", n), "identb"], writes=[("bank", i % 2)])
            P.op("dve", lambda e, i=i, pT=pT: e.tensor_tensor(
                out=hT[:, :, i * 128:(i + 1) * 128], in0=pT,
                in1=prmT[:, :, 39:40].broadcast_to([128, 8, 128]), op=ALU.mult),
                reads=[("bank", i % 2), "prmT"], writes=[("hT", i)])

        def proj(bank, col0, q):
            for k in range(8):
                P.op("pe", lambda e, k=k, bank=bank, col0=col0, q=q: e.matmul(
                    banks[bank][:, :], lhsT=winb[:, k, col0:col0 + 128], rhs=hT[:, k, q * QW:(q + 1) * QW],
                    start=(k == 0), stop=(k == 7)),
                    reads=[("winb", col0 // 512)] + [("hT", 4 * q + t_) for t_ in range(4)], writes=[("bank", bank)])

        step_c = [0]

        def stepA(j, q):
            step = step_c[0]
            b0 = 2 + 2 * (step % 3)
            step += 1
            step_c[0] = step
            proj(b0, 1536 + j * 128, q)
            proj(b0 + 1, 2048 + j * 128, q)
            t = tmpA[step % 2]
            tk = ("tmpA", step % 2)
            P.op("act", lambda e, t=t, b0=b0: e.activation(out=t[:], in_=banks[b0 + 1][:, :], func=AF.Exp,
                                                           scale=-1.0),
                 reads=[("bank", b0 + 1)], writes=[tk])
            P.op("act", lambda e, t=t: e.activation(out=t[:], in_=t[:], func=AF.Ln, bias=ones_row[:, 0:1], scale=1.0),
                 reads=[tk, "ones_row"], writes=[tk])
            P.op("act", lambda e, t=t: e.activation(out=t[:], in_=t[:], func=AF.Exp, scale=-1.0),
                 reads=[tk], writes=[tk])
            P.op("dve", lambda e, t=t, b0=b0, j=j, q=q: e.tensor_tensor(
                out=u[:, j, 15 + q * QW:15 + (q + 1) * QW], in0=banks[b0][:, :], in1=t[:], op=ALU.mult),
                reads=[tk, ("bank", b0)], writes=[("u", j, q)])

        PRO = 5
        ph0_prep(0)
        ph0_prep(1)
        for i in range(PRO):
            ph0_tr(i)
            if i + 2 <= PRO:
                ph0_prep(i + 2)
        sA = 0
        for q in range(NQ):
            for j in range(4):
                t_ = PRO + sA
                sA += 1
                if t_ + 1 < NT:
                    ph0_prep(t_ + 1)
                stepA(j, q)
                if t_ < NT:
                    ph0_tr(t_)
        step = step_c[0]
        for j in range(4):
            for tp in range(34):
                eng = "pool" if (tp % 2 == 0) else "dve"
                P.op(eng, lambda e, j=j, tp=tp: e.tensor_scalar(
                    out=Dg[:, j * 34 + tp, :], in0=identf[:], scalar1=prmT[:, j, tp:tp + 1],
                    scalar2=1.0, op0=ALU.mult, op1=ALU.mult),
                    reads=["identf", "prmT"], writes=[("Dg", j, tp)])

        s_wout = [sem(f"s_wout{i}") for i in range(2)]
        for h in range(2):
            P.dma("pool", lambda e, h=h: e.dma_start(
                out=woutb[:, :, h * 512:(h + 1) * 512],
                in_=w_out[:, h * 512:(h + 1) * 512].rearrange("(k p) c -> p k c", p=128)),
                s_wout[h], writes=[("woutb", h)] + [("xb", b_) for b_ in range(3)] + [("xnb", n_) for n_ in range(2)])
        for j in range(4):
            for q in range(NQ):
                b0 = 2 + 2 * (step % 3)
                step += 1
                proj(b0, 0 + j * 128, q)
                proj(b0 + 1, 1024 + j * 128, q)
                t = tmpA[step % 2]
                P.op("act", lambda e, t=t, b0=b0: e.activation(out=t[:], in_=banks[b0][:, :], func=AF.Copy),
                     reads=[("bank", b0)], writes=[("tmpA", step % 2)])
                P.op("dve", lambda e, t=t, b0=b0, j=j, q=q: e.tensor_tensor(
                    out=v[:, j, 1 + q * QW:1 + (q + 1) * QW], in0=banks[b0 + 1][:, :], in1=t[:], op=ALU.mult),
                    reads=[("tmpA", step % 2), ("bank", b0 + 1)], writes=[("v", j, q)])
                if step % 4 == 0:
                    precast_next(P.ops[-1])
                if step % 3 == 1:
                    init_next(P.ops[-1])
        for j in range(4):
            for q in range(NQ):
                b0 = 2 + (step % 6)
                step += 1
                proj(b0, 512 + j * 128, q)
                P.op("act", lambda e, b0=b0, j=j, q=q: e.activation(
                    out=ab[:, j, q * QW:(q + 1) * QW], in_=banks[b0][:, :], func=AF.Copy),
                    reads=[("bank", b0)], writes=[("ab", j, q)])
                if step % 4 == 0:
                    precast_next(P.ops[-1])
                if step % 3 == 1:
                    init_next(P.ops[-1])

        while init_pos[0] < len(init_list):
            init_next(P.ops[-1])
        if debug:
            s_dbg = sem("s_dbg")
            dbg_ops = []
            dbg_ops.append(P.dma("sp", lambda e: e.dma_start(out=dbg["u"], in_=u[:]), s_dbg,
                                 reads=[("u", j, q) for j in range(4) for q in range(4)] + ["u_halo"]))
            dbg_ops.append(P.dma("sp", lambda e: e.dma_start(out=dbg["v"], in_=v[:]), s_dbg,
                                 reads=[("v", j, q) for j in range(4) for q in range(4)] + ["v_halo"]))
            dbg_ops.append(P.dma("sp", lambda e: e.dma_start(out=dbg["ab"], in_=ab[:]), s_dbg,
                                 reads=[("ab", j, q) for j in range(4) for q in range(4)]))

        P.set_fence()
        ar.reset(mU)
        ar.alloc([128, 8, D], BF16)
        cbuf = [ar.alloc([128, 4, QW], F32) for _ in range(2)]
        csq = [ar.alloc([128, 4, QW], BF16) for _ in range(2)]
        mean_sb = ar.alloc([128, QW], F32)
        var_sb = ar.alloc([128, QW], F32)
        t_sb = [ar.alloc([128, QW], F32) for _ in range(4)]
        yab = [ar.alloc([128, QW], F32) for _ in range(4)]
        ysq = [ar.alloc([128, QW], BF16) for _ in range(4)]
        rsH = [ar.alloc([128, QW], F32) for _ in range(2)] * 2
        yT = [ar.alloc([128, 8, QW], BF16) for _ in range(2)]
        xr = [ar.alloc([128, D], F32) for _ in range(2)]
        xsb = [ar.alloc([128, D], BF16) for _ in range(4)]
        lgs4 = ar.alloc([128, 4, 36], F32)
        rq = ar.alloc([128, 8, 4], F32)
        ohg4 = ar.alloc([128, 4, 4], F32)
        d4 = ar.alloc([128, 4, 4], F32)
        t48_4 = ar.alloc([128, 4, 4, 8], F32)
        lsel4 = ar.alloc([128, 4, 8], F32)
        oh1_4 = ar.alloc([128, 4, 8], F32)
        l2_4 = ar.alloc([128, 4, 8], F32)
        oh2_4 = ar.alloc([128, 4, 8], F32)
        E1_4 = ar.alloc([128, 4, 4, 8], F32)
        E2_4 = ar.alloc([128, 4, 4, 8], F32)
        pr4 = ar.alloc([128, 4, 32], F32)
        s4 = ar.alloc([128, 4, 2], F32)
        eid4 = ar.alloc([128, 4, 2], F32)
        rf4 = ar.alloc([128, 4, 2], F32)
        xsT = ar.alloc([128, 8, 128], F32)
        s_xr = [sem(f"s_xr{i}") for i in range(2)]
        s_x1 = [sem(f"s_x1{i}") for i in range(2)]
        s_sc = [sem(f"s_sc{i}") for i in range(4)]
        s_si = [sem(f"s_si{i}") for i in range(4)]
        invsrc = ar.alloc([128, NT * 2, 16], I32)
        P.op("pool", lambda e: e.iota(invsrc[:].rearrange("p (i k) c -> p i k c", k=2),
                                      pattern=[[256, NT], [1, 2], [0, 16]], base=0, channel_multiplier=2),
             writes=["invsrc"])
        P.op("pool", lambda e: e.iota(invsrc[:].rearrange("p (i k) c -> p i k c", k=2)[:, :, :, 8:16],
                                      pattern=[[128, NT], [0, 2], [0, 8]], base=0, channel_multiplier=1),
             writes=["invsrc"])
        scat_ops = []
        xs_ops = []
        x1_ops = {}

        def convB(q, j):
            bk = j % 2
            cb = cbuf[q % 2]
            cs = csq[q % 2]
            for tp in range(31):
                P.op("pe", lambda e, j=j, tp=tp, bk=bk, q=q: e.matmul(
                    banks[bk][:, :], lhsT=Dg[:, j * 34 + 3 + tp, :],
                    rhs=u[:, j, q * QW + tp:q * QW + tp + QW], start=(tp == 0), stop=(tp == 30)),
                    reads=[("Dg", j, 3 + tp)] + [("u", j, qq) for qq in (q - 1, q, q + 1) if 0 <= qq < NQ] + ["u_halo"],
                    writes=[("bank", bk)])
            P.op("act", lambda e, j=j, bk=bk, cb=cb: e.activation(
                out=cb[:, j, :], in_=banks[bk][:, :], func=AF.Identity, bias=prmT[:, j, 34:35], scale=1.0),
                reads=[("bank", bk), "prmT"], writes=[("cb", q % 2, j)])
            P.op("act", lambda e, j=j, bk=bk, cs=cs: e.activation(
                out=cs[:, j, :], in_=banks[bk][:, :], func=AF.Square, bias=prmT[:, j, 34:35], scale=1.0),
                reads=[("bank", bk), "prmT"], writes=[("cs", q % 2, j)])
            precast_next(P.ops[-1])

        def convA(q, j):
            bk = j % 2
            for tp in range(3):
                P.op("pe", lambda e, j=j, tp=tp, bk=bk, q=q: e.matmul(
                    banks[bk][:, :], lhsT=Dg[:, j * 34 + tp, :],
                    rhs=v[:, j, q * QW + tp:q * QW + tp + QW], start=(tp == 0), stop=(tp == 2)),
                    reads=[("Dg", j, tp)] + [("v", j, qq) for qq in (q - 1, q, q + 1) if 0 <= qq < NQ] + ["v_halo"],
                    writes=[("bank", bk)])
            P.op("dve", lambda e, j=j, bk=bk, q=q: e.tensor_tensor(
                out=yab[j][:], in0=banks[bk][:, :], in1=ab[:, j, q * QW:(q + 1) * QW], op=ALU.mult),
                reads=[("bank", bk), ("ab", j, q)], writes=[("yab", j)])
            P.op("pool", lambda e, j=j: e.tensor_tensor(out=ysq[j][:], in0=yab[j][:], in1=yab[j][:], op=ALU.mult),
                 reads=[("yab", j)], writes=[("ysq", j)])

        def head_stats(q, j, beta_col, ych):
            sbk = 2 + (j % 2)
            yq = yT[q % 2]
            P.op("pe", lambda e, j=j, sbk=sbk: e.matmul(banks[sbk][:, :], lhsT=B64[:], rhs=ysq[j][:],
                                                       start=True, stop=True),
                 reads=["B64", ("ysq", j)], writes=[("bank", sbk)])
            P.op("act", lambda e, j=j, sbk=sbk: e.activation(out=rsH[j][:], in_=banks[sbk][:, :], func=AF.Ln,
                                                             bias=epsc[:, 0:1], scale=1.0),
                 reads=[("bank", sbk), "epsc"], writes=[("rsH", j % 2)])
            P.op("act", lambda e, j=j: e.activation(out=rsH[j][:], in_=rsH[j][:], func=AF.Exp, scale=-0.5),
                 reads=[("rsH", j % 2)], writes=[("rsH", j % 2)])
            P.op("dve", lambda e, j=j, yq=yq, ych=ych, beta_col=beta_col: e.scalar_tensor_tensor(
                out=yq[:, ych, :], in0=yab[j][:], scalar=prmT[:, j, beta_col:beta_col + 1], in1=rsH[j][:],
                op0=ALU.mult, op1=ALU.mult),
                reads=[("yab", j), ("rsH", j % 2), "prmT"], writes=[("yT", q % 2, ych)])

        def S2(q):
            cb = cbuf[q % 2]
            cs = csq[q % 2]
            for j in range(4):
                P.op("pe", lambda e, j=j, cb=cb: e.matmul(banks[2][:, :], lhsT=O512f[:], rhs=cb[:, j, :],
                                                         start=(j == 0), stop=(j == 3)),
                     reads=["O512f", ("cb", q % 2, j)], writes=[("bank", 2)])
            for j in range(4):
                P.op("pe", lambda e, j=j, cs=cs: e.matmul(banks[3][:, :], lhsT=O512b[:], rhs=cs[:, j, :],
                                                         start=(j == 0), stop=(j == 3)),
                     reads=["O512b", ("cs", q % 2, j)], writes=[("bank", 3)])
            P.op("act", lambda e: e.activation(out=mean_sb[:], in_=banks[2][:, :], func=AF.Copy),
                 reads=[("bank", 2)], writes=["mean_sb"])
            P.op("dve", lambda e: e.tensor_tensor(out=var_sb[:], in0=mean_sb[:], in1=mean_sb[:], op=ALU.mult),
                 reads=["mean_sb"], writes=["var_sb"])
            P.op("dve", lambda e: e.tensor_tensor(out=var_sb[:], in0=banks[3][:, :], in1=var_sb[:], op=ALU.subtract),
                 reads=[("bank", 3), "var_sb"], writes=["var_sb"])
            P.op("act", lambda e: e.activation(out=var_sb[:], in_=var_sb[:], func=AF.Ln, bias=epsc[:, 0:1], scale=1.0),
                 reads=["var_sb", "epsc"], writes=["var_sb"])
            P.op("act", lambda e: e.activation(out=var_sb[:], in_=var_sb[:], func=AF.Exp, scale=-0.5),
                 reads=["var_sb"], writes=["var_sb"])
            for j in range(4):
                head_stats(q, j, 37, j)

        def S3(q):
            cb = cbuf[q % 2]
            for j in range(4):
                P.op("dve", lambda e, j=j, cb=cb: e.tensor_tensor(out=t_sb[j][:], in0=cb[:, j, :], in1=mean_sb[:],
                                                                 op=ALU.subtract),
                     reads=[("cb", q % 2, j), "mean_sb"], writes=[("t_sb", j)])
            for j in range(4):
                P.op("dve", lambda e, j=j: e.tensor_tensor(out=t_sb[j][:], in0=t_sb[j][:], in1=var_sb[:], op=ALU.mult),
                     reads=[("t_sb", j), "var_sb"], writes=[("t_sb", j)])
            for j in range(4):
                P.op("act", lambda e, j=j: e.activation(
                    out=yab[j][:], in_=t_sb[j][:], func=AF.Silu, bias=prmT[:, j, 36:37], scale=prmT[:, j, 35:36]),
                    reads=[("t_sb", j), "prmT"], writes=[("yab", j)])
            for j in range(4):
                P.op("pool", lambda e, j=j: e.tensor_tensor(out=ysq[j][:], in0=yab[j][:], in1=yab[j][:], op=ALU.mult),
                     reads=[("yab", j)], writes=[("ysq", j)])

        def S4(q):
            for j in range(4):
                head_stats(q, j, 38, 4 + j)

        wpre = []

        def S5a(q, tl):
            i = q * 4 + tl
            p2 = i % 2
            yq = yT[q % 2]
            P.dma("sp", lambda e, i=i, p2=p2: e.dma_start(out=xr[p2][:], in_=x[i * 128:(i + 1) * 128, :]),
                  s_xr[p2], writes=[("xr", p2, 0), ("xr", p2, 1)])
            if wpre:
                e0_, m0_ = wpre.pop(0)
                load_w(e0_, extra_writes=U_NAMES, parts=(m0_,))
            for h in range(2):
                for c in range(8):
                    P.op("pe", lambda e, h=h, c=c, tl=tl, yq=yq: e.matmul(
                        banks[4 + h][:, :], lhsT=yq[:, c, tl * 128:(tl + 1) * 128],
                        rhs=woutb[:, c, h * 512:(h + 1) * 512], start=(c == 0), stop=(c == 7)),
                        reads=[("yT", q % 2, c), ("woutb", h)], writes=[("bank", 4 + h)])
                P.op("dve", lambda e, h=h, p2=p2: e.tensor_tensor(
                    out=xr[p2][:, h * 512:(h + 1) * 512], in0=banks[4 + h][:, :],
                    in1=xr[p2][:, h * 512:(h + 1) * 512], op=ALU.add),
                    reads=[("bank", 4 + h), ("xr", p2, h)], writes=[("xr", p2, h)])
            xk = [("xr", p2, 0), ("xr", p2, 1)]
            x1_ops[i] = P.dma("sp", lambda e, i=i, p2=p2: e.dma_start(out=x1_d[i * 128:(i + 1) * 128, :], in_=xr[p2][:]),
                              s_x1[p2], reads=xk, writes=[("x1_d", i)])
            P.op("act", lambda e, i=i, p2=p2: e.activation(out=junkb[:], in_=xr[p2][:], func=AF.Square,
                                                           accum_out=ss2[:, i:i + 1]),
                 reads=xk, writes=["junkb", ("ss2", i)])
            P.op("act", lambda e, i=i: e.activation(out=rs2[:, i:i + 1], in_=ss2[:, i:i + 1], func=AF.Ln,
                                                    bias=epsc[:, 0:1], scale=1.0 / D),
                 reads=[("ss2", i), "epsc"], writes=[("rs2", i)])
            P.op("act", lambda e, i=i: e.activation(out=rs2[:, i:i + 1], in_=rs2[:, i:i + 1], func=AF.Exp,
                                                    scale=-0.5),
                 reads=[("rs2", i)], writes=[("rs2", i)])
            P.op("pool", lambda e, i=i, p2=p2, tl=tl: e.tensor_scalar(out=xsb[tl][:], in0=xr[p2][:],
                                                                     scalar1=rs2[:, i:i + 1], scalar2=1.0,
                                                                     op0=ALU.mult, op1=ALU.mult),
                 reads=xk + [("rs2", i)], writes=[("xsb", tl)])
            xs_ops.append(P.dma("pool", lambda e, i=i, tl=tl: e.dma_start(out=xs_d[i * 128:(i + 1) * 128, :], in_=xsb[tl][:]),
                                s_sc[tl], reads=[("xsb", tl)], writes=[("xs_d", i)]))
            if q >= 1:
                precast_next(P.ops[-1])

        def S5b(q, tl):
            i = q * 4 + tl
            p2 = i % 2
            xk = [("xr", p2, 0), ("xr", p2, 1)]
            for hh in range(2):
                tbk = (6, 2, 3)[(2 * i + hh) % 3]
                pT6 = banks[tbk][:, :].rearrange("p (k t) -> p k t", t=128)
                for k4 in range(4):
                    k = hh * 4 + k4
                    P.op("pe", lambda e, k=k, k4=k4, p2=p2, pT6=pT6: e.transpose(
                        out=pT6[:, k4, :], in_=xr[p2][:, k * 128:(k + 1) * 128], identity=identf[:]),
                        reads=xk + ["identf"], writes=[("bank", tbk)])
                P.op("dve", lambda e, hh=hh, pT6=pT6: e.tensor_copy(out=xsT[:, hh * 4:(hh + 1) * 4, :], in_=pT6),
                     reads=[("bank", tbk)], writes=[("xsT", hh)])
            lgp = banks[7][:, tl * 36:(tl + 1) * 36]
            for k in range(8):
                P.op("pe", lambda e, k=k, lgp=lgp: e.matmul(lgp, lhsT=xsT[:, k, :], rhs=wr[:, k, :],
                                                           start=(k == 0), stop=(k == 7)),
                     reads=[("xsT", k // 4), "wr"], writes=[("b7lg", tl)])
            P.op("dve", lambda e, i=i, tl=tl, lgp=lgp: e.scalar_tensor_tensor(
                out=lgs4[:, tl, :], in0=lgp, scalar=rs2[:, i:i + 1], in1=br_bc[:], op0=ALU.mult, op1=ALU.add),
                reads=[("b7lg", tl), ("rs2", i), "br_bc"], writes=[("lgs4", tl)])
            if debug:
                dbg_ops.append(P.dma("sp", lambda e, i=i, tl=tl: e.dma_start(out=dbg["lg"][i], in_=lgs4[:, tl, :]), s_dbg,
                                     reads=[("lgs4", tl)]))

        def bc(ap2, shape):
            return ap2.broadcast_to(shape)

        def RT(q):
            R = "rt"
            T0 = q * 4
            lg_g = lgs4[:, :, 0:4]
            P.op("dve", lambda e: e.tensor_reduce(out=rq[:, 0, :], in_=lg_g, axis=AX.X, op=ALU.max),
                 reads=[("lgs4", t_) for t_ in range(4)], writes=[R])
            P.op("dve", lambda e: e.tensor_tensor(out=ohg4[:], in0=lg_g, in1=bc(rq[:, 0, :, None], [128, 4, 4]),
                                                  op=ALU.is_ge), reads=[R], writes=[R])
            P.op("dve", lambda e: e.tensor_tensor(out=d4[:], in0=lg_g, in1=bc(rq[:, 0, :, None], [128, 4, 4]),
                                                  op=ALU.subtract), reads=[R], writes=[R])
            P.op("act", lambda e: e.activation(out=d4[:], in_=d4[:], func=AF.Exp), reads=[R], writes=[R])
            P.op("dve", lambda e: e.tensor_reduce(out=rq[:, 1, :], in_=d4[:], axis=AX.X, op=ALU.add),
                 reads=[R], writes=[R])
            P.op("dve", lambda e: e.reciprocal(out=rq[:, 2, :], in_=rq[:, 1, :]), reads=[R], writes=[R])
            le4 = lgs4[:, :, 4:36].rearrange("p t (g x) -> p t g x", x=8)
            P.op("dve", lambda e: e.tensor_tensor(out=t48_4[:], in0=le4, in1=bc(ohg4[:, :, :, None], [128, 4, 4, 8]),
                                                  op=ALU.mult), reads=[R], writes=[R])
            P.op("dve", lambda e: e.tensor_reduce(out=lsel4[:], in_=t48_4[:].rearrange("p t g x -> p t x g"),
                                                  axis=AX.X, op=ALU.add), reads=[R], writes=[R])
            P.op("dve", lambda e: e.tensor_reduce(out=rq[:, 3, :], in_=lsel4[:], axis=AX.X, op=ALU.max),
                 reads=[R], writes=[R])
            P.op("dve", lambda e: e.tensor_tensor(out=oh1_4[:], in0=lsel4[:], in1=bc(rq[:, 3, :, None], [128, 4, 8]),
                                                  op=ALU.is_ge), reads=[R], writes=[R])
            fl = lambda t_: t_[:].rearrange("p t x -> p (t x)")
            P.op("dve", lambda e: e.scalar_tensor_tensor(out=fl(l2_4), in0=fl(oh1_4), scalar=-1e30, in1=fl(lsel4),
                                                         op0=ALU.mult, op1=ALU.add), reads=[R], writes=[R])
            P.op("dve", lambda e: e.tensor_reduce(out=rq[:, 4, :], in_=l2_4[:], axis=AX.X, op=ALU.max),
                 reads=[R], writes=[R])
            P.op("dve", lambda e: e.tensor_tensor(out=oh2_4[:], in0=l2_4[:], in1=bc(rq[:, 4, :, None], [128, 4, 8]),
                                                  op=ALU.is_ge), reads=[R], writes=[R])
            P.op("dve", lambda e: e.tensor_tensor(out=rq[:, 5, :], in0=rq[:, 4, :], in1=rq[:, 3, :],
                                                  op=ALU.subtract), reads=[R], writes=[R])
            P.op("act", lambda e: e.activation(out=rq[:, 5, :], in_=rq[:, 5, :], func=AF.Exp), reads=[R], writes=[R])
            P.op("dve", lambda e: e.tensor_scalar(out=rq[:, 5, :], in0=rq[:, 5, :], scalar1=1.0, scalar2=None,
                                                  op0=ALU.add), reads=[R], writes=[R])
            P.op("dve", lambda e: e.reciprocal(out=rq[:, 6, :], in_=rq[:, 5, :]), reads=[R], writes=[R])
            P.op("dve", lambda e: e.tensor_tensor(out=cw[:, T0:T0 + 4, 0], in0=rq[:, 6, :], in1=rq[:, 2, :],
                                                  op=ALU.mult), reads=[R], writes=[R, ("cw", q)])
            P.op("dve", lambda e: e.tensor_tensor(out=cw[:, T0:T0 + 4, 1], in0=rq[:, 2, :], in1=cw[:, T0:T0 + 4, 0],
                                                  op=ALU.subtract), reads=[R], writes=[R, ("cw", q)])
            P.op("dve", lambda e: e.tensor_tensor(out=E1_4[:], in0=bc(ohg4[:, :, :, None], [128, 4, 4, 8]),
                                                  in1=bc(oh1_4[:, :, None, :], [128, 4, 4, 8]), op=ALU.mult),
                 reads=[R], writes=[R, "E4"])
            P.op("dve", lambda e: e.tensor_tensor(out=E2_4[:], in0=bc(ohg4[:, :, :, None], [128, 4, 4, 8]),
                                                  in1=bc(oh2_4[:, :, None, :], [128, 4, 4, 8]), op=ALU.mult),
                 reads=[R], writes=[R, "E4"])
            P.op("dve", lambda e: e.tensor_tensor(
                out=Mb[:, T0:T0 + 4, :].rearrange("p t (g x) -> p t g x", x=8), in0=E1_4[:], in1=E2_4[:], op=ALU.add),
                reads=[R], writes=[("Mb", q)])

        def S6(q):
            T0 = q * 4
            posq = banks[7][:, 256:384].rearrange("p (t x) -> p t x", x=32)
            for tl in range(4):
                i = T0 + tl
                P.op("pe", lambda e, i=i, tl=tl: e.matmul(posq[:, tl, :], lhsT=Ltri[:], rhs=Mb[:, i, :], start=True,
                                                          stop=(i == 0)),
                     reads=["Ltri", ("Mb", q)], writes=["b7pos"])
                for jj in range(i):
                    P.op("pe", lambda e, jj=jj, i=i, tl=tl: e.matmul(posq[:, tl, :], lhsT=ones_b[:], rhs=Mb[:, jj, :],
                                                                    start=False, stop=(jj == i - 1)),
                         reads=["ones_b", ("Mb", jj // 4)], writes=["b7pos"])
            Ef = lambda E_: E_[:].rearrange("p t g x -> p t (g x)")
            S = "s6"
            for kk, E_ in enumerate((E1_4, E2_4)):
                P.op("dve", lambda e, E_=E_: e.tensor_tensor(out=pr4[:], in0=Ef(E_), in1=posq, op=ALU.mult),
                     reads=["E4", "b7pos"], writes=[S])
                P.op("dve", lambda e, kk=kk: e.tensor_reduce(out=s4[:, :, kk], in_=pr4[:], axis=AX.X, op=ALU.add),
                     reads=[S], writes=[S])
                P.op("dve", lambda e, E_=E_: e.tensor_tensor(out=pr4[:], in0=Ef(E_),
                                                             in1=bc(iota_e[:, None, :], [128, 4, 32]), op=ALU.mult),
                     reads=["E4", "iota_e", S], writes=[S])
                P.op("dve", lambda e, kk=kk: e.tensor_reduce(out=eid4[:, :, kk], in_=pr4[:], axis=AX.X, op=ALU.add),
                     reads=[S], writes=[S])
            f2 = lambda t_: t_[:].rearrange("p t k -> p (t k)")
            P.op("dve", lambda e: e.tensor_scalar(out=f2(s4), in0=f2(s4), scalar1=float(CAP - 1), scalar2=None,
                                                  op0=ALU.min), reads=[S], writes=[S])
            P.op("dve", lambda e: e.scalar_tensor_tensor(out=f2(rf4), in0=f2(eid4), scalar=float(CAP), in1=f2(s4),
                                                         op0=ALU.mult, op1=ALU.add), reads=[S], writes=[S])
            P.op("dve", lambda e: e.tensor_copy(out=ridx[:, T0:T0 + 4, :], in_=rf4[:]),
                 reads=[S], writes=[("ridx", q)])

        def S6b(q):
            T0 = q * 4
            for tl in range(4):
                i = T0 + tl
                for kk in range(2):
                    so = P.dma("pool", lambda e, i=i, kk=kk, tl=tl: e.indirect_dma_start(
                        out=inv_d[:, :], out_offset=bass.IndirectOffsetOnAxis(ap=ridx[:, i, kk:kk + 1], axis=0),
                        in_=invsrc[:, i * 2 + kk, :], in_offset=None),
                        s_si[tl], reads=[("ridx", q), "invsrc"], writes=[], deps=init_ops[0:1])
                    scat_ops.append(so)

        m2 = ar.mark()
        ar.reset(mF)
        NW = 3
        NXC0 = 5
        NXC = 13
        NYO = 6
        NY = 4
        NOT = 3
        yt = [ar.alloc([128, 2, D], BF16) for _ in range(NY)]
        ar.reset(mF)
        w1b = [None] * NW
        w3b = [None] * NW
        w2b = [None] * NW
        for sl_ in range(2):
            w1b[sl_] = ar.alloc([128, 8, 512], BF16)
            w3b[sl_] = ar.alloc([128, 8, 512], BF16)
            w2b[sl_] = ar.alloc([128, 4, D], BF16)
        assert ar.mark() <= mU, (ar.mark(), mU)
        ar.reset(mU)
        xc = [ar.alloc([128, D], F32) for _ in range(NXC0)]
        _save = ar.mark()
        ar.reset(mDg)
        xc += [ar.alloc([128, D], F32) for _ in range(NXC - NXC0)]
        assert ar.mark() <= mDg + 4 * 34 * 128
        ar.reset(_save)
        ot = [ar.alloc([128, D], F32) for _ in range(NOT)]
        w1b[2] = ar.alloc([128, 8, 512], BF16)
        w3b[2] = ar.alloc([128, 8, 512], BF16)
        w2b[2] = ar.alloc([128, 4, D], BF16)
        NXG = 3
        xg = [ar.alloc([128, NST, D], BF16) for _ in range(NXG)]
        xT = [ar.alloc([128, 8, CAP], BF16) for _ in range(2)]
        slt = [ar.alloc([128, CAP], F32) for _ in range(2)]
        actT = [ar.alloc([128, 4, CAP], BF16) for _ in range(2)]
        yo = [ar.alloc([128, D], BF16) for _ in range(NYO)]
        NINV = 6
        invs = [ar.alloc([128, NST, 16], I32) for _ in range(NINV)]
        m2end = ar.mark()
        ar.reset(m2)
        s_inv = [sem(f"s_inv{i}") for i in range(NINV)]
        s_w = [[sem(f"s_w{i}_{m}") for m in range(3)] for i in range(NW)]
        s_xg = [[sem(f"s_xg{i}_{j}") for j in range(NST)] for i in range(NXG)]
        s_yo = [sem(f"s_yo{i}") for i in range(NYO)]
        s_xc = [sem(f"s_xc{i}") for i in range(NXC)]
        s_yt = [sem(f"s_yt{i}") for i in range(NY)]
        s_ot = [sem(f"s_ot{i}") for i in range(NOT)]
        U_NAMES = [(nm, j_, q_) for nm in ("u", "v", "ab") for j_ in range(4) for q_ in range(NQ)] + ["u_halo", "v_halo"]

        s_wp = [[sem(f"s_wp{i}_{m}") for m in range(3)] for i in range(NW)]

        def load_w(e_, extra_writes=(), parts=(0, 1, 2)):
            sl = e_ % NW
            xw = list(extra_writes)
            if e_ < NPRE:
                rk = [("wp", e_, m) for m in range(3)]
                if 0 in parts:
                    P.dma("sp", lambda e, e_=e_, sl=sl: e.dma_start(
                        out=w1b[sl][:], in_=w1p[e_].rearrange("p (k f) -> p k f", k=8)), s_wp[sl][0],
                        reads=rk, writes=[("w1b", sl)] + xw)
                if 1 in parts:
                    P.dma("sp", lambda e, e_=e_, sl=sl: e.dma_start(
                        out=w3b[sl][:], in_=w3p[e_].rearrange("p (k f) -> p k f", k=8)), s_wp[sl][1],
                        reads=rk, writes=[("w3b", sl)] + xw)
                if 2 in parts:
                    P.dma("sp", lambda e, e_=e_, sl=sl: e.dma_start(
                        out=w2b[sl][:], in_=w2p[e_].rearrange("p (k f) -> p k f", k=4)), s_wp[sl][2],
                        reads=rk, writes=[("w2b", sl)] + xw)
                return
            assert tuple(parts) == (0, 1, 2)
            P.dma("pool", lambda e, e_=e_, sl=sl: e.dma_start(
                out=w1b[sl][:], in_=w1[e_].rearrange("(k p) f -> p k f", p=128)), s_w[sl][0], writes=[("w1b", sl)] + xw)
            P.dma("pool", lambda e, e_=e_, sl=sl: e.dma_start(
                out=w3b[sl][:], in_=w3[e_].rearrange("(k p) f -> p k f", p=128)), s_w[sl][1], writes=[("w3b", sl)] + xw)
            P.dma("pool", lambda e, e_=e_, sl=sl: e.dma_start(
                out=w2b[sl][:], in_=w2[e_].rearrange("(k p) f -> p k f", p=128)), s_w[sl][2], writes=[("w2b", sl)] + xw)

        for j in range(4):
            convB(0, j)
        for j in range(4):
            convA(0, j)
        def S5tile(q, tl):
            S5a(q, tl)
            if tl >= 1:
                S5b(q, tl - 1)
            if tl == 3:
                S5b(q, 3)

        for q in range(NQ):
            nxt = q + 1 < NQ
            last = q == NQ - 1
            if nxt:
                convB(q + 1, 0)
            S2(q)
            if 1 <= q < NQ - 1:
                S6(q - 1)
            if nxt:
                convB(q + 1, 1)
            if last:
                S5tile(q - 1, 0)
                S5tile(q - 1, 1)
            S3(q)
            if nxt:
                convB(q + 1, 2)
            if last:
                S5tile(q - 1, 2)
                S5tile(q - 1, 3)
            S4(q)
            if 1 <= q < NQ - 1:
                S6b(q - 1)
            if nxt:
                convB(q + 1, 3)
            if debug:
                dbg_ops.append(P.dma("sp", lambda e, q=q: e.dma_start(out=dbg["yT"][q], in_=yT[q % 2][:]), s_dbg,
                                     reads=[("yT", q % 2, c) for c in range(8)]))
            if nxt:
                for j in range(4):
                    convA(q + 1, j)
            if q == NQ - 2:
                wpre.extend([(0, 0), (0, 1), (0, 2), (1, 0), (1, 1), (1, 2)])
                continue
            if last:
                RT(q - 1)
                S5tile(q, 0)
                S6(q - 1)
                S5tile(q, 1)
                S6b(q - 1)
                S5tile(q, 2)
                S5tile(q, 3)
                RT(q)
            else:
                for tl in range(4):
                    S5tile(q, tl)
                RT(q)
        S6(NQ - 1)
        S6b(NQ - 1)
        while pre_pos[0] < len(pre_list):
            precast_next(P.ops[-1])
        if debug:
            dbg_ops.append(P.dma("sp", lambda e: e.dma_start(out=dbg["cw"], in_=cw[:]), s_dbg,
                                 reads=[("cw", q) for q in range(NQ)]))
            dbg_ops.append(P.dma("sp", lambda e: e.dma_start(out=dbg["ridx"], in_=ridx[:]), s_dbg,
                                 reads=[("ridx", q) for q in range(NQ)]))

        P.set_fence()
        ar.reset(m2end)
        g2T = prmT[:, :, 40:41]
        def load_xc(i):
            b = i % NXC
            P.dma("sp", lambda e, i=i, b=b: e.dma_start(out=xc[b][:], in_=x1_d[i * 128:(i + 1) * 128, :]),
                  s_xc[b], reads=[("x1_d", i)], writes=[("xc", b)])

        def load_inv(e_):
            sl = e_ % NINV
            P.dma("sp", lambda e, e_=e_, sl=sl: e.dma_start(
                out=invs[sl][:], in_=inv_d[e_ * CAP:(e_ + 1) * CAP, :].rearrange("(j p) c -> p j c", p=128)),
                s_inv[sl], writes=[("invs", sl)], deps=scat_ops)

        gbc_cache = []

        def gbc_reg(e):
            if not gbc_cache:
                gbc_cache.append(e.to_reg(S - 1))
            return gbc_cache[0]

        def gather_xg(e_):
            sl = e_ % NXG
            iv = e_ % NINV
            for j in range(NST):
                P.dma("pool", lambda e, sl=sl, iv=iv, j=j: e.indirect_dma_start(
                    out=xg[sl][:, j, :], out_offset=None, in_=xs_d[:, :],
                    in_offset=bass.IndirectOffsetOnAxis(ap=invs[iv][:, j, 8:9], axis=0),
                    bounds_check=gbc_reg(e), oob_is_err=False),
                    s_xg[sl][j], reads=[("invs", iv)], writes=[("xg", sl, j)], deps=xs_ops)

        for sl_ in range(NXG):
            P.op("dve", lambda e, sl_=sl_: e.memset(xg[sl_][:], 0.0), writes=[("xg", sl_, j_) for j_ in range(NST)])
        load_inv(0)
        load_inv(1)
        load_inv(2)
        gather_xg(0)
        gather_xg(1)
        for i in range(NXC0):
            load_xc(i)
        yg_ops = []
        yoc = [0]
        bc_cache = []

        def bc_reg(e):
            if not bc_cache:
                bc_cache.append(e.to_reg(2 * S - 1))
            return bc_cache[0]

        def ex_trans(e_, kp):
            p2 = e_ % 2
            gx = e_ % NXG
            tbk = kp % 2
            pTb = banks[tbk][:, :].bitcast(BF16)[:, 0:2 * CAP].rearrange("p (k t) -> p k t", t=CAP)
            for k2 in range(2):
                k = kp * 2 + k2
                for j in range(NST):
                    P.op("pe", lambda e, k=k, k2=k2, j=j, gx=gx, pTb=pTb: e.transpose(
                        out=pTb[:, k2, j * 128:(j + 1) * 128], in_=xg[gx][:, j, k * 128:(k + 1) * 128],
                        identity=identb[:]),
                        reads=[("xg", gx, j), "identb"], writes=[("bank", tbk)])
            P.op("dve", lambda e, kp=kp, p2=p2, pTb=pTb: e.tensor_tensor(
                out=xT[p2][:, kp * 2:(kp + 1) * 2, :], in0=pTb,
                in1=g2T[:, kp * 2:(kp + 1) * 2, :].broadcast_to([128, 2, CAP]), op=ALU.mult),
                reads=[("bank", tbk), "prmT"], writes=[("xT", p2, kp)])

        def ex_h13(e_):
            sl = e_ % NW
            p2 = e_ % 2
            for f in range(4):
                b1 = 2 + (f % 2)
                b3 = 4 + (f % 2)
                for k in range(8):
                    P.op("pe", lambda e, k=k, f=f, b1=b1, sl=sl, p2=p2: e.matmul(
                        banks[b1][:, 0:CAP], lhsT=w1b[sl][:, k, f * 128:(f + 1) * 128], rhs=xT[p2][:, k, :],
                        start=(k == 0), stop=(k == 7)),
                        reads=[("w1b", sl), ("xT", p2, k // 2)], writes=[("bank", b1)])
                for k in range(8):
                    P.op("pe", lambda e, k=k, f=f, b3=b3, sl=sl, p2=p2: e.matmul(
                        banks[b3][:, 0:CAP], lhsT=w3b[sl][:, k, f * 128:(f + 1) * 128], rhs=xT[p2][:, k, :],
                        start=(k == 0), stop=(k == 7)),
                        reads=[("w3b", sl), ("xT", p2, k // 2)], writes=[("bank", b3)])
                P.op("act", lambda e, f=f, b1=b1: e.activation(out=slt[f % 2][:], in_=banks[b1][:, 0:CAP], func=AF.Silu),
                     reads=[("bank", b1)], writes=[("slt", f % 2)])
                P.op("dve", lambda e, f=f, b3=b3, p2=p2: e.tensor_tensor(
                    out=actT[p2][:, f, :], in0=banks[b3][:, 0:CAP], in1=slt[f % 2][:], op=ALU.mult),
                    reads=[("bank", b3), ("slt", f % 2)], writes=[("actT", p2, f)])

        def ex_out(e_, j):
            sl = e_ % NW
            p2 = e_ % 2
            yb_ = yoc[0] % NYO
            yoc[0] += 1
            for h in range(2):
                ob = 6 + h
                for f in range(4):
                    P.op("pe", lambda e, f=f, j=j, h=h, ob=ob, sl=sl, p2=p2: e.matmul(
                        banks[ob][:, :], lhsT=actT[p2][:, f, j * 128:(j + 1) * 128],
                        rhs=w2b[sl][:, f, h * 512:(h + 1) * 512], start=(f == 0), stop=(f == 3)),
                        reads=[("w2b", sl), ("actT", p2, f)], writes=[("bank", ob)])
                if h == 0:
                    P.op("act", lambda e, h=h, ob=ob, yb_=yb_: e.activation(
                        out=yo[yb_][:, h * 512:(h + 1) * 512], in_=banks[ob][:, :], func=AF.Copy),
                        reads=[("bank", ob)], writes=[("yo", yb_, h)])
                else:
                    P.op("dve", lambda e, h=h, ob=ob, yb_=yb_: e.tensor_copy(
                        out=yo[yb_][:, h * 512:(h + 1) * 512], in_=banks[ob][:, :]),
                        reads=[("bank", ob)], writes=[("yo", yb_, h)])
            iv = e_ % NINV
            yg_ops.append(P.dma("pool", lambda e, j=j, iv=iv, yb_=yb_: e.indirect_dma_start(
                out=yt_d[:, :], out_offset=bass.IndirectOffsetOnAxis(ap=invs[iv][:, j, 0:1], axis=0),
                in_=yo[yb_][:], in_offset=None, bounds_check=bc_reg(e), oob_is_err=False),
                s_yo[yb_], reads=[("yo", yb_, 0), ("yo", yb_, 1), ("invs", iv)]))

        for kp in range(4):
            ex_trans(0, kp)
        for e_ in range(NE):
            if e_ + 2 < NE:
                gather_xg(e_ + 2)
                load_w(e_ + 2)
            if e_ + 3 < NE:
                load_inv(e_ + 3)
            if NXC0 + e_ < NXC:
                load_xc(NXC0 + e_)
            ex_h13(e_)
            nx = e_ + 1 < NE
            if nx:
                ex_trans(e_ + 1, 0)
                ex_trans(e_ + 1, 1)
            ex_out(e_, 0)
            if nx:
                ex_trans(e_ + 1, 2)
                ex_trans(e_ + 1, 3)
            ex_out(e_, 1)
            ex_out(e_, 2)

        out_ops = []

        def load_yt(i):
            yb6 = i % NY
            P.dma("sp", lambda e, i=i, yb6=yb6: e.dma_start(
                out=yt[yb6][:], in_=yt_d[i * 256:(i + 1) * 256, :].rearrange("(p k) d -> p k d", k=2)),
                s_yt[yb6], writes=[("yt", yb6)], deps=yg_ops)

        for i in range(NY):
            load_yt(i)

        def combine(i):
            yb6 = i % NY
            b = i % NXC
            P.op("dve", lambda e, i=i, yb6=yb6, b=b: e.scalar_tensor_tensor(
                out=xc[b][:], in0=yt[yb6][:, 0, :], scalar=cw[:, i, 0:1], in1=xc[b][:], op0=ALU.mult, op1=ALU.add),
                reads=[("yt", yb6), ("xc", b), ("cw", i // 4)], writes=[("xc", b)])
            P.op("dve", lambda e, i=i, yb6=yb6, b=b: e.scalar_tensor_tensor(
                out=xc[b][:], in0=yt[yb6][:, 1, :], scalar=cw[:, i, 1:2], in1=xc[b][:], op0=ALU.mult, op1=ALU.add),
                reads=[("yt", yb6), ("xc", b), ("cw", i // 4)], writes=[("xc", b)])

        combine(0)
        for i in range(NT):
            p2 = i % NOT
            b = i % NXC
            P.op("act", lambda e, i=i, b=b: e.activation(out=junkb[:], in_=xc[b][:], func=AF.Square,
                                                         accum_out=ssF[:, i:i + 1]),
                 reads=[("xc", b)], writes=["junkb", ("ssF", i)])
            P.op("act", lambda e, i=i: e.activation(out=rsF[:, i:i + 1], in_=ssF[:, i:i + 1], func=AF.Ln,
                                                    bias=epsc[:, 0:1], scale=1.0 / D),
                 reads=[("ssF", i), "epsc"], writes=[("rsF", i)])
            P.op("act", lambda e, i=i: e.activation(out=rsF[:, i:i + 1], in_=rsF[:, i:i + 1], func=AF.Exp, scale=-0.5),
                 reads=[("rsF", i)], writes=[("rsF", i)])
            if i + 1 < NT:
                combine(i + 1)
            P.op("dve", lambda e, i=i, p2=p2, b=b: e.scalar_tensor_tensor(
                out=ot[p2][:], in0=xc[b][:], scalar=rsF[:, i:i + 1], in1=gF_bc[:], op0=ALU.mult, op1=ALU.mult),
                reads=[("xc", b), ("rsF", i), "gF_bc"], writes=[("ot", p2)])
            out_ops.append(P.dma("sp", lambda e, i=i, p2=p2: e.dma_start(out=out[i * 128:(i + 1) * 128, :], in_=ot[p2][:]),
                                 s_ot[p2], reads=[("ot", p2)]))
            if i + NXC < NT:
                load_xc(i + NXC)
            if i + NY < NT:
                load_yt(i + NY)
        finals = out_ops[-NOT:] + (dbg_ops if debug else [])
        P.emit(st, final_waits=finals)
        if debug:
            print('arena mF', mF, 'mU', mU, 'peak', ar.peak, 'ARN', ARN, 'nops', len(P.ops), 'nsem', nsem[0])
    return nc


_IN_NAMES = ["norm_mix_g", "w_in", "conv_a_w", "conv_b_w", "conv_b_bias", "ln_b_g", "ln_b_b", "beta_a", "beta_b",
             "w_out", "norm_ffn_g", "w_route_group", "b_route_group", "w_route_expert", "b_route_expert",
             "w1", "w3", "w2"]


def make_in_maps(inputs):
    f = lambda a: np.ascontiguousarray(np.asarray(a, dtype=np.float32))
    shared = {}
    for n in _IN_NAMES:
        a = f(inputs[n])[0]
        if a.ndim == 1:
            a = a.reshape(1, -1)
        shared[n] = np.ascontiguousarray(a)
    shared["norm_final_g"] = f(inputs["norm_final_g"]).reshape(1, -1)
    x = f(inputs["x"])
    return [dict(shared, x=np.ascontiguousarray(x[c])) for c in range(8)]


def kernel(**inputs):
    nc = build_nc()
    in_maps = make_in_maps(inputs)
    res = run_bass_kernel_spmd(nc, in_maps, core_ids=list(range(8)))
    return np.stack([np.asarray(r["out"], dtype=np.float32) for r in res.results], axis=0)
```

```python
import contextlib
import numpy as np
import concourse.bass as bass
import concourse.mybir as mybir
from concourse.bass_utils import run_bass_kernel_spmd
from concourse.alu_op_type import AluOpType as ALU

F32 = mybir.dt.float32
BF16 = mybir.dt.bfloat16
I32 = mybir.dt.int32
AF = mybir.ActivationFunctionType
AX = mybir.AxisListType

S = 2048
D = 1024
NT = 16
NQ = 4
QW = 512
NE = 32
CAP = 384
NST = CAP // 128
NPRE = 8
BIGI = 2 * S
EPS = 1e-6
ENGS = ("pe", "act", "dve", "pool", "sp")


class Op:
    __slots__ = ("eng", "fn", "reads", "writes", "deps", "sem", "count",
                 "is_dma", "has_cons", "idx", "name", "nofence")

    def __init__(self, eng, fn, reads, writes, is_dma, sem, name):
        self.eng = eng
        self.fn = fn
        self.reads = reads
        self.writes = writes
        self.deps = []
        self.sem = sem
        self.count = None
        self.is_dma = is_dma
        self.has_cons = False
        self.name = name
        self.nofence = False


class Prog:
    def __init__(self, nc):
        self.nc = nc
        self.ops = []
        self.last_writer = {}
        self.readers = {}
        self.fence = []

    def set_fence(self):
        f = set()
        for w in self.last_writer.values():
            f.add(w)
        for rs in self.readers.values():
            for r in rs:
                f.add(r)
        f = {o for o in f if not o.nofence}
        latest = {}
        for o in f:
            k = (o.eng, id(o.sem) if o.is_dma else 0, o.is_dma)
            if k not in latest or latest[k].idx < o.idx:
                latest[k] = o
        keep = [o for o in f if o.is_dma] + [o for o in latest.values() if not o.is_dma]
        self.fence = keep

    def _add(self, op, extra_deps):
        deps = set()
        for k in op.reads:
            w = self.last_writer.get(k)
            if w is not None:
                deps.add(w)
        for k in op.writes:
            w = self.last_writer.get(k)
            if w is not None:
                deps.add(w)
            for r in self.readers.get(k, ()):
                deps.add(r)
        for d in extra_deps:
            if d is not None:
                deps.add(d)
        for d in self.fence:
            deps.add(d)
        deps.discard(op)
        if op.eng == "pe" and not op.is_dma:
            deps = {d for d in deps if not (d.eng == "pe" and not d.is_dma)}
        op.deps = sorted(deps, key=lambda d: d.idx)
        for d in op.deps:
            d.has_cons = True
        for k in op.reads:
            self.readers.setdefault(k, []).append(op)
        for k in op.writes:
            self.last_writer[k] = op
            self.readers[k] = []
        return op

    def op(self, eng, fn, reads=(), writes=(), deps=(), name=""):
        o = Op(eng, fn, tuple(reads), tuple(writes), False, None, name)
        o.idx = len(self.ops)
        self.ops.append(o)
        return self._add(o, deps)

    def dma(self, eng, fn, sem, reads=(), writes=(), deps=(), name="", nofence=False):
        o = Op(eng, fn, tuple(reads), tuple(writes), True, sem, name)
        o.idx = len(self.ops)
        o.has_cons = True
        o.nofence = nofence
        self.ops.append(o)
        return self._add(o, deps)

    def emit(self, st, final_waits=()):
        nc = self.nc
        esem = {e: st.enter_context(nc.semaphore("es_" + e)) for e in ENGS if e != "sp"}
        ecount = {e: 0 for e in ENGS}
        dcount = {}
        for o in self.ops:
            if o.is_dma:
                k = id(o.sem)
                dcount[k] = dcount.get(k, 0) + 16
                o.count = dcount[k]
            elif o.has_cons:
                ecount[o.eng] += 1
                o.count = ecount[o.eng]
                o.sem = esem[o.eng]
        per_eng = {e: [o for o in self.ops if o.eng == e] for e in ENGS}
        block = st.enter_context(nc.Block())

        def run(e, eo):
            waited = {}
            for o in per_eng[e]:
                need = {}
                for d in o.deps:
                    k = id(d.sem)
                    if k not in need or need[k][1] < d.count:
                        need[k] = (d.sem, d.count)
                for k, (sm, cnt) in need.items():
                    if waited.get(k, 0) >= cnt:
                        continue
                    eo.wait_ge(sm, cnt)
                    waited[k] = cnt
                ins = o.fn(eo)
                if o.is_dma:
                    ins.then_inc(o.sem, 16)
                elif o.has_cons:
                    ins.then_inc(o.sem, 1)
            if e == "sp":
                for o in final_waits:
                    eo.wait_ge(o.sem, o.count)

        @block.tensor
        def _(eo):
            run("pe", eo)

        @block.scalar
        def _(eo):
            run("act", eo)

        @block.vector
        def _(eo):
            run("dve", eo)

        @block.gpsimd
        def _(eo):
            run("pool", eo)

        @block.sync
        def _(eo):
            run("sp", eo)


class Arena:
    def __init__(self, A, n):
        self.A = A
        self.n = n
        self.off = 0

    def mark(self):
        return self.off

    def reset(self, m):
        self.off = m

    def alloc(self, shape, dt):
        size = {F32: 4, BF16: 2, I32: 4}[dt]
        assert shape[0] == 128
        ne = 1
        for s_ in shape[1:]:
            ne *= s_
        nb = ne * size
        nb = (nb + 31) // 32 * 32
        n2 = nb // 2
        self.peak = max(getattr(self, "peak", 0), self.off + n2)
        assert self.off + n2 <= self.n, ("arena overflow", self.off, n2, self.n)
        v = self.A[:, self.off:self.off + n2]
        self.off += n2
        if dt != BF16:
            v = v.bitcast(dt)
        v = v[:, 0:ne]
        if len(shape) == 3:
            return v.rearrange("p (a b) -> p a b", b=shape[2])
        if len(shape) == 4:
            return v.rearrange("p (a b c) -> p a b c", b=shape[2], c=shape[3])
        return v


def build_nc(debug=False):
    nc = bass.Bass("TRN2", target_bir_lowering=False)

    def din(name, shape, dt=F32):
        return nc.dram_tensor(name, shape, dt, kind="ExternalInput").ap()

    x = din("x", [S, D])
    norm_mix_g = din("norm_mix_g", [1, D])
    w_in = din("w_in", [D, 2560])
    conv_a_w = din("conv_a_w", [3, 512])
    conv_b_w = din("conv_b_w", [31, 512])
    conv_b_bias = din("conv_b_bias", [1, 512])
    ln_b_g = din("ln_b_g", [1, 512])
    ln_b_b = din("ln_b_b", [1, 512])
    beta_a = din("beta_a", [1, 512])
    beta_b = din("beta_b", [1, 512])
    w_out = din("w_out", [D, D])
    norm_ffn_g = din("norm_ffn_g", [1, D])
    w_rg = din("w_route_group", [D, 4])
    b_rg = din("b_route_group", [1, 4])
    w_re = din("w_route_expert", [D, 32])
    b_re = din("b_route_expert", [1, 32])
    w1 = din("w1", [NE, D, 512])
    w3 = din("w3", [NE, D, 512])
    w2 = din("w2", [NE, 512, D])
    norm_final_g = din("norm_final_g", [1, D])
    out = nc.dram_tensor("out", [S, D], F32, kind="ExternalOutput").ap()
    x1_d = nc.dram_tensor("x1_d", [S, D], F32, kind="Internal").ap()
    xs_d = nc.dram_tensor("xs_d", [S, D], BF16, kind="Internal").ap()
    yt_d = nc.dram_tensor("yt_d", [2 * S + 128, D], BF16, kind="Internal").ap()
    inv_d = nc.dram_tensor("inv_d", [NE * CAP, 16], I32, kind="Internal").ap()
    w1p = nc.dram_tensor("w1p", [NPRE, 128, 8 * 512], BF16, kind="Internal").ap()
    w3p = nc.dram_tensor("w3p", [NPRE, 128, 8 * 512], BF16, kind="Internal").ap()
    w2p = nc.dram_tensor("w2p", [NPRE, 128, 4 * D], BF16, kind="Internal").ap()
    dbg = {}
    if debug:
        dbg["u"] = nc.dram_tensor("dbg_u", [128, 4, S + 30], BF16, kind="ExternalOutput").ap()
        dbg["v"] = nc.dram_tensor("dbg_v", [128, 4, S + 2], BF16, kind="ExternalOutput").ap()
        dbg["ab"] = nc.dram_tensor("dbg_ab", [128, 4, S], BF16, kind="ExternalOutput").ap()
        dbg["x1"] = x1_d
        dbg["yT"] = nc.dram_tensor("dbg_yT", [NQ, 128, 8, QW], BF16, kind="ExternalOutput").ap()
        dbg["lg"] = nc.dram_tensor("dbg_lg", [NT, 128, 36], F32, kind="ExternalOutput").ap()
        dbg["cw"] = nc.dram_tensor("dbg_cw", [128, NT, 2], F32, kind="ExternalOutput").ap()
        dbg["ridx"] = nc.dram_tensor("dbg_ridx", [128, NT, 2], I32, kind="ExternalOutput").ap()

    with contextlib.ExitStack() as st:
        ARN = 106400
        A_t = st.enter_context(nc.sbuf_tensor("arena", [128, ARN], BF16))
        ar = Arena(A_t, ARN)
        banks = [st.enter_context(nc.psum_tensor(f"bank{i}", [128, 512], F32)) for i in range(8)]
        nsem = [0]

        def sem(name):
            nsem[0] += 1
            return st.enter_context(nc.semaphore(name))

        P = Prog(nc)

        identf = ar.alloc([128, 128], F32)
        identb = ar.alloc([128, 128], BF16)
        io_i = ar.alloc([128, 128], I32)
        B64 = ar.alloc([128, 128], BF16)
        O512b = ar.alloc([128, 128], BF16)
        O512f = ar.alloc([128, 128], F32)
        Ltri = ar.alloc([128, 128], BF16)
        ones_b = ar.alloc([128, 128], BF16)
        ones_row = ar.alloc([128, 128], F32)
        epsc = ar.alloc([128, 1], F32)
        iota_e = ar.alloc([128, 32], F32)
        iota_ei = ar.alloc([128, 32], I32)
        prmT = ar.alloc([128, 8, 41], F32)
        gF_bc = ar.alloc([128, D], F32)
        wr = ar.alloc([128, 8, 36], F32)
        br = ar.alloc([128, 36], F32)
        br_bc = ar.alloc([128, 36], F32)
        mDg = ar.mark()
        Dg = ar.alloc([128, 4 * 34, 128], BF16)
        ss1 = ar.alloc([128, NT], F32)
        rs1 = ar.alloc([128, NT], F32)
        ss2 = ar.alloc([128, NT], F32)
        rs2 = ar.alloc([128, NT], F32)
        ssF = ar.alloc([128, NT], F32)
        rsF = ar.alloc([128, NT], F32)
        cw = ar.alloc([128, NT, 2], F32)
        ridx = ar.alloc([128, NT, 2], I32)
        Mb = ar.alloc([128, NT, 32], BF16)
        lgs = ar.alloc([128, 36], F32)
        r_mg = ar.alloc([128, 8], F32)
        ohg = ar.alloc([128, 4], F32)
        eg = ar.alloc([128, 4], F32)
        t48 = ar.alloc([128, 4, 8], F32)
        lsel = ar.alloc([128, 8], F32)
        oh1 = ar.alloc([128, 8], F32)
        l2 = ar.alloc([128, 8], F32)
        oh2 = ar.alloc([128, 8], F32)
        E1 = ar.alloc([128, 4, 8], F32)
        E2 = ar.alloc([128, 4, 8], F32)
        j32 = ar.alloc([128, 32], F32)
        rsc = ar.alloc([128, 8], F32)
        junkb = ar.alloc([128, D], BF16)

        mF = ar.mark()
        u = ar.alloc([128, 4, S + 30], BF16)
        v = ar.alloc([128, 4, S + 2], BF16)
        ab = ar.alloc([128, 4, S], BF16)
        mU = ar.mark()
        woutb = ar.alloc([128, 8, D], BF16)
        ar.reset(mU)
        xb = [ar.alloc([128, D], F32) for _ in range(3)]
        xnb = [ar.alloc([128, D], BF16) for _ in range(2)]
        assert ar.mark() - mU == 8 * D
        prm = ar.alloc([128, D], F32)
        gF_row = ar.alloc([128, D], F32)

        s_prm = sem("s_prm")
        s_gf = sem("s_gf")
        s_br = sem("s_br")
        s_wr = sem("s_wr")

        P.op("pool", lambda e: e.iota(io_i[:], pattern=[[1, 128]], base=0, channel_multiplier=-1),
             writes=["io_i"])
        P.op("dve", lambda e: e.tensor_single_scalar(out=identf[:], in_=io_i[:], scalar=0, op=ALU.is_equal),
             reads=["io_i"], writes=["identf"])
        P.op("dve", lambda e: e.tensor_copy(out=identb[:], in_=identf[:]), reads=["identf"], writes=["identb"])
        P.op("dve", lambda e: e.tensor_single_scalar(out=Ltri[:], in_=io_i[:], scalar=0, op=ALU.is_gt),
             reads=["io_i"], writes=["Ltri"])
        P.op("dve", lambda e: e.memset(ones_b[:], 1.0), writes=["ones_b"])
        P.op("dve", lambda e: e.memset(ones_row[:], 1.0), writes=["ones_row"])
        P.op("dve", lambda e: e.memset(O512b[:], 1.0 / 512), writes=["O512b"])
        P.op("dve", lambda e: e.memset(O512f[:], 1.0 / 512), writes=["O512f"])
        P.op("dve", lambda e: e.memset(epsc[:], EPS), writes=["epsc"])
        P.op("pool", lambda e: e.memset(B64[:], 0.0), writes=["B64"])
        P.op("pool", lambda e: e.memset(B64[0:64, 0:64], 1.0 / 64), writes=["B64"])
        P.op("pool", lambda e: e.memset(B64[64:128, 64:128], 1.0 / 64), writes=["B64"])
        P.op("pool", lambda e: e.iota(iota_ei[:], pattern=[[1, 32]], base=0, channel_multiplier=0),
             writes=["iota_ei"])
        P.op("dve", lambda e: e.tensor_copy(out=iota_e[:], in_=iota_ei[:]), reads=["iota_ei"], writes=["iota_e"])
        P.op("pool", lambda e: e.memset(prm[:], 0.0), writes=["prm"])
        P.op("pool", lambda e: e.memset(u[:, :, 0:15], 0.0), writes=["u_halo"])
        P.op("pool", lambda e: e.memset(u[:, :, S + 15:S + 30], 0.0), writes=["u_halo"])
        P.op("pool", lambda e: e.memset(v[:, :, 0:1], 0.0), writes=["v_halo"])
        P.op("pool", lambda e: e.memset(v[:, :, S + 1:S + 2], 0.0), writes=["v_halo"])
        prm_rows = [(conv_a_w, 0, 3, 512), (conv_b_w, 3, 31, 512), (conv_b_bias, 34, 1, 512),
                    (ln_b_g, 35, 1, 512), (ln_b_b, 36, 1, 512), (beta_a, 37, 1, 512),
                    (beta_b, 38, 1, 512), (norm_mix_g, 39, 1, D), (norm_ffn_g, 40, 1, D)]
        for (src, r0, nr, w_) in prm_rows:
            P.dma("sp", lambda e, src=src, r0=r0, nr=nr, w_=w_: e.dma_start(out=prm[r0:r0 + nr, 0:w_], in_=src),
                  s_prm, writes=[("prmrow", r0)], deps=[P.last_writer["prm"]])
        P.dma("sp", lambda e: e.dma_start(out=gF_row[0:1, :], in_=norm_final_g), s_gf, writes=["gF_row"])
        P.dma("sp", lambda e: e.dma_start(out=br[0:1, 0:4], in_=b_rg), s_br, writes=[("br", 0)])
        P.dma("sp", lambda e: e.dma_start(out=br[0:1, 4:36], in_=b_re), s_br, writes=[("br", 1)])
        P.dma("sp", lambda e: e.dma_start(out=wr[:, :, 0:4], in_=w_rg.rearrange("(k p) c -> p k c", p=128)),
              s_wr, writes=[("wr", 0)])
        P.dma("sp", lambda e: e.dma_start(out=wr[:, :, 4:36], in_=w_re.rearrange("(k p) c -> p k c", p=128)),
              s_wr, writes=[("wr", 1)])
        for k in range(8):
            P.op("pe", lambda e, k=k: e.transpose(out=banks[0][:, k * 41:(k + 1) * 41],
                                                 in_=prm[0:41, k * 128:(k + 1) * 128],
                                                 identity=identf[0:41, 0:41]),
                 reads=["prm", "identf"] + [("prmrow", r[1]) for r in prm_rows], writes=[("bank", 0)])
        P.op("dve", lambda e: e.tensor_copy(out=prmT[:], in_=banks[0][:, 0:8 * 41].rearrange("p (k c) -> p k c", c=41)),
             reads=[("bank", 0)], writes=["prmT"])
        for h in range(2):
            P.op("pe", lambda e, h=h: e.matmul(banks[1 + h][:, :], lhsT=ones_row[0:1, :],
                                              rhs=gF_row[0:1, h * 512:(h + 1) * 512], start=True, stop=True),
                 reads=["ones_row", "gF_row"], writes=[("bank", 1 + h)])
            P.op("dve", lambda e, h=h: e.tensor_copy(out=gF_bc[:, h * 512:(h + 1) * 512], in_=banks[1 + h][:, :]),
                 reads=[("bank", 1 + h)], writes=["gF_bc"])
        P.op("pe", lambda e: e.matmul(banks[3][:, 0:36], lhsT=ones_row[0:1, :], rhs=br[0:1, :], start=True, stop=True),
             reads=["ones_row", ("br", 0), ("br", 1)], writes=[("bank", 3)])
        P.op("dve", lambda e: e.tensor_copy(out=br_bc[:], in_=banks[3][:, 0:36]), reads=[("bank", 3)], writes=["br_bc"])
        P.op("dve", lambda e: e.tensor_tensor(out=wr[:], in0=wr[:],
                                              in1=prmT[:, :, 40:41].broadcast_to([128, 8, 36]), op=ALU.mult),
             reads=[("wr", 0), ("wr", 1), "prmT"], writes=["wr"])
        hT = ar.alloc([128, 8, S], BF16)
        winb = ar.alloc([128, 8, 2560], BF16)
        tmpA = [ar.alloc([128, QW], F32) for _ in range(2)]
        s_xb = [sem(f"s_xb{i}") for i in range(3)]
        zt = ar.alloc([128, 2048], BF16)
        bigt = ar.alloc([128, NE * CAP * 16 // 128], I32)
        s_init = sem("s_init")
        P.op("pool", lambda e: e.memset(zt[:], 0.0), writes=["zt"])
        P.op("pool", lambda e: e.memset(bigt[:], BIGI), writes=["bigt"])
        init_ops = []
        init_list = [("inv", 0)] + [("yt", r0) for r0 in range(0, 2 * S, 2048)]
        init_pos = [0]

        def init_next(dep):
            if init_pos[0] >= len(init_list):
                return
            kind, r0 = init_list[init_pos[0]]
            init_pos[0] += 1
            if kind == "inv":
                init_ops.append(P.dma("sp", lambda e: e.dma_start(
                    out=inv_d.rearrange("(p a) c -> p (a c)", p=128), in_=bigt[:]), s_init, reads=["bigt"], deps=[dep]))
                return
            dst = yt_d
            init_ops.append(P.dma("sp", lambda e, r0=r0, dst=dst: e.dma_start(
                out=dst[r0:r0 + 2048, :].rearrange("(p a2 a1) d -> p a2 (a1 d)", p=128, a2=8, a1=2),
                in_=zt[:, None, :].broadcast_to([128, 8, 2048])), s_init, reads=["zt"], deps=[dep]))
        s_pre = [sem(f"s_pre{i}") for i in range(NPRE)]
        pre_list = [(e_, m) for e_ in range(NPRE) for m in range(3)]
        pre_pos = [0]

        def precast_next(dep):
            if pre_pos[0] >= len(pre_list):
                return
            e_, m = pre_list[pre_pos[0]]
            pre_pos[0] += 1
            src = (w1, w3, w2)[m][e_].rearrange("(k p) f -> p k f", p=128)
            dst = (w1p, w3p, w2p)[m][e_].rearrange("p (k f) -> p k f", k=(8, 8, 4)[m])
            P.dma("pool", lambda e, src=src, dst=dst: e.dma_start(out=dst, in_=src), s_pre[e_],
                  writes=[("wp", e_, m)], deps=[dep], nofence=True)
        s_win = [sem(f"s_win{i}") for i in range(5)]
        def load_win(blk, deps=()):
            P.dma("pool", lambda e, blk=blk: e.dma_start(
                out=winb[:, :, blk * 512:(blk + 1) * 512],
                in_=w_in[:, blk * 512:(blk + 1) * 512].rearrange("(k p) c -> p k c", p=128)),
                s_win[blk], writes=[("winb", blk)], deps=deps)
        load_win(3)
        load_win(4)
        win_after = {7: 0, 11: 2, 15: 1}
        def ph0_prep(i):
            b = i % 3
            n = i % 2
            xl = P.dma("sp", lambda e, i=i, b=b: e.dma_start(out=xb[b][:], in_=x[i * 128:(i + 1) * 128, :]),
                       s_xb[b], writes=[("xb", b)])
            if i in win_after:
                load_win(win_after[i], deps=[xl])
            P.op("act", lambda e, i=i, b=b: e.activation(out=junkb[:], in_=xb[b][:], func=AF.Square,
                                                         accum_out=ss1[:, i:i + 1]),
                 reads=[("xb", b)], writes=["junkb", ("ss1", i)])
            P.op("act", lambda e, i=i: e.activation(out=rs1[:, i:i + 1], in_=ss1[:, i:i + 1], func=AF.Ln,
                                                    bias=epsc[:, 0:1], scale=1.0 / D),
                 reads=[("ss1", i), "epsc"], writes=[("rs1", i)])
            P.op("act", lambda e, i=i: e.activation(out=rs1[:, i:i + 1], in_=rs1[:, i:i + 1], func=AF.Exp,
                                                    scale=-0.5),
                 reads=[("rs1", i)], writes=[("rs1", i)])
            P.op("dve", lambda e, i=i, b=b, n=n: e.tensor_scalar(out=xnb[n][:], in0=xb[b][:],
                                                                scalar1=rs1[:, i:i + 1], scalar2=None, op0=ALU.mult),
                 reads=[("xb", b), ("rs1", i)], writes=[("xnb", n)])

        def ph0_tr(i):
            n = i % 2
            pb = banks[i % 2]
            pT = pb[:, :].bitcast(BF16).rearrange("p (k t) -> p k t", t=128)
            for k in range(8):
                P.op("pe", lambda e, k=k, n=n, pT=pT: e.transpose(out=pT[:, k, :], in_=xnb[n][:, k * 128:(k + 1) * 128],
                                                                 identity=identb[:]),
                     reads=[("xnb", n), "identb"], writes=[("bank", i % 2)])
            P.op("dve", lambda e, i=i, pT=pT: e.tensor_tensor(
                out=hT[:, :, i * 128:(i + 1) * 128], in0=pT,
                in1=prmT[:, :, 39:40].broadcast_to([128, 8, 128]), op=ALU.mult),
                reads=[("bank", i % 2), "prmT"], writes=[("hT", i)])

        def proj(bank, col0, q):
            for k in range(8):
                P.op("pe", lambda e, k=k, bank=bank, col0=col0, q=q: e.matmul(
                    banks[bank][:, :], lhsT=winb[:, k, col0:col0 + 128], rhs=hT[:, k, q * QW:(q + 1) * QW],
                    start=(k == 0), stop=(k == 7)),
                    reads=[("winb", col0 // 512)] + [("hT", 4 * q + t_) for t_ in range(4)], writes=[("bank", bank)])

        step_c = [0]

        def stepA(j, q):
            step = step_c[0]
            b0 = 2 + 2 * (step % 3)
            step += 1
            step_c[0] = step
            proj(b0, 1536 + j * 128, q)
            proj(b0 + 1, 2048 + j * 128, q)
            t = tmpA[step % 2]
            tk = ("tmpA", step % 2)
            P.op("act", lambda e, t=t, b0=b0: e.activation(out=t[:], in_=banks[b0 + 1][:, :], func=AF.Exp,
                                                           scale=-1.0),
                 reads=[("bank", b0 + 1)], writes=[tk])
            P.op("act", lambda e, t=t: e.activation(out=t[:], in_=t[:], func=AF.Ln, bias=ones_row[:, 0:1], scale=1.0),
                 reads=[tk, "ones_row"], writes=[tk])
            P.op("act", lambda e, t=t: e.activation(out=t[:], in_=t[:], func=AF.Exp, scale=-1.0),
                 reads=[tk], writes=[tk])
            P.op("dve", lambda e, t=t, b0=b0, j=j, q=q: e.tensor_tensor(
                out=u[:, j, 15 + q * QW:15 + (q + 1) * QW], in0=banks[b0][:, :], in1=t[:], op=ALU.mult),
                reads=[tk, ("bank", b0)], writes=[("u", j, q)])

        PRO = 5
        ph0_prep(0)
        ph0_prep(1)
        for i in range(PRO):
            ph0_tr(i)
            if i + 2 <= PRO:
                ph0_prep(i + 2)
        sA = 0
        for q in range(NQ):
            for j in range(4):
                t_ = PRO + sA
                sA += 1
                if t_ + 1 < NT:
                    ph0_prep(t_ + 1)
                stepA(j, q)
                if t_ < NT:
                    ph0_tr(t_)
        step = step_c[0]
        for j in range(4):
            for tp in range(34):
                eng = "pool" if (tp % 2 == 0) else "dve"
                P.op(eng, lambda e, j=j, tp=tp: e.tensor_scalar(
                    out=Dg[:, j * 34 + tp, :], in0=identf[:], scalar1=prmT[:, j, tp:tp + 1],
                    scalar2=1.0, op0=ALU.mult, op1=ALU.mult),
                    reads=["identf", "prmT"], writes=[("Dg", j, tp)])

        s_wout = [sem(f"s_wout{i}") for i in range(2)]
        for h in range(2):
            P.dma("pool", lambda e, h=h: e.dma_start(
                out=woutb[:, :, h * 512:(h + 1) * 512],
                in_=w_out[:, h * 512:(h + 1) * 512].rearrange("(k p) c -> p k c", p=128)),
                s_wout[h], writes=[("woutb", h)] + [("xb", b_) for b_ in range(3)] + [("xnb", n_) for n_ in range(2)])
        for j in range(4):
            for q in range(NQ):
                b0 = 2 + 2 * (step % 3)
                step += 1
                proj(b0, 0 + j * 128, q)
                proj(b0 + 1, 1024 + j * 128, q)
                t = tmpA[step % 2]
                P.op("act", lambda e, t=t, b0=b0: e.activation(out=t[:], in_=banks[b0][:, :], func=AF.Copy),
                     reads=[("bank", b0)], writes=[("tmpA", step % 2)])
                P.op("dve", lambda e, t=t, b0=b0, j=j, q=q: e.tensor_tensor(
                    out=v[:, j, 1 + q * QW:1 + (q + 1) * QW], in0=banks[b0 + 1][:, :], in1=t[:], op=ALU.mult),
                    reads=[("tmpA", step % 2), ("bank", b0 + 1)], writes=[("v", j, q)])
                if step % 4 == 0:
                    precast_next(P.ops[-1])
                if step % 3 == 1:
                    init_next(P.ops[-1])
        for j in range(4):
            for q in range(NQ):
                b0 = 2 + (step % 6)
                step += 1
                proj(b0, 512 + j * 128, q)
                P.op("act", lambda e, b0=b0, j=j, q=q: e.activation(
                    out=ab[:, j, q * QW:(q + 1) * QW], in_=banks[b0][:, :], func=AF.Copy),
                    reads=[("bank", b0)], writes=[("ab", j, q)])
                if step % 4 == 0:
                    precast_next(P.ops[-1])
                if step % 3 == 1:
                    init_next(P.ops[-1])

        while init_pos[0] < len(init_list):
            init_next(P.ops[-1])
        if debug:
            s_dbg = sem("s_dbg")
            dbg_ops = []
            dbg_ops.append(P.dma("sp", lambda e: e.dma_start(out=dbg["u"], in_=u[:]), s_dbg,
                                 reads=[("u", j, q) for j in range(4) for q in range(4)] + ["u_halo"]))
            dbg_ops.append(P.dma("sp", lambda e: e.dma_start(out=dbg["v"], in_=v[:]), s_dbg,
                                 reads=[("v", j, q) for j in range(4) for q in range(4)] + ["v_halo"]))
            dbg_ops.append(P.dma("sp", lambda e: e.dma_start(out=dbg["ab"], in_=ab[:]), s_dbg,
                                 reads=[("ab", j, q) for j in range(4) for q in range(4)]))

        P.set_fence()
        ar.reset(mU)
        ar.alloc([128, 8, D], BF16)
        cbuf = [ar.alloc([128, 4, QW], F32) for _ in range(2)]
        csq = [ar.alloc([128, 4, QW], BF16) for _ in range(2)]
        mean_sb = ar.alloc([128, QW], F32)
        var_sb = ar.alloc([128, QW], F32)
        t_sb = [ar.alloc([128, QW], F32) for _ in range(4)]
        yab = [ar.alloc([128, QW], F32) for _ in range(4)]
        ysq = [ar.alloc([128, QW], BF16) for _ in range(4)]
        rsH = [ar.alloc([128, QW], F32) for _ in range(2)] * 2
        yT = [ar.alloc([128, 8, QW], BF16) for _ in range(2)]
        xr = [ar.alloc([128, D], F32) for _ in range(2)]
        xsb = [ar.alloc([128, D], BF16) for _ in range(4)]
        lgs4 = ar.alloc([128, 4, 36], F32)
        rq = ar.alloc([128, 8, 4], F32)
        ohg4 = ar.alloc([128, 4, 4], F32)
        d4 = ar.alloc([128, 4, 4], F32)
        t48_4 = ar.alloc([128, 4, 4, 8], F32)
        lsel4 = ar.alloc([128, 4, 8], F32)
        oh1_4 = ar.alloc([128, 4, 8], F32)
        l2_4 = ar.alloc([128, 4, 8], F32)
        oh2_4 = ar.alloc([128, 4, 8], F32)
        E1_4 = ar.alloc([128, 4, 4, 8], F32)
        E2_4 = ar.alloc([128, 4, 4, 8], F32)
        pr4 = ar.alloc([128, 4, 32], F32)
        s4 = ar.alloc([128, 4, 2], F32)
        eid4 = ar.alloc([128, 4, 2], F32)
        rf4 = ar.alloc([128, 4, 2], F32)
        xsT = ar.alloc([128, 8, 128], F32)
        s_xr = [sem(f"s_xr{i}") for i in range(2)]
        s_x1 = [sem(f"s_x1{i}") for i in range(2)]
        s_sc = [sem(f"s_sc{i}") for i in range(4)]
        s_si = [sem(f"s_si{i}") for i in range(4)]
        invsrc = ar.alloc([128, NT * 2, 16], I32)
        P.op("pool", lambda e: e.iota(invsrc[:].rearrange("p (i k) c -> p i k c", k=2),
                                      pattern=[[256, NT], [1, 2], [0, 16]], base=0, channel_multiplier=2),
             writes=["invsrc"])
        P.op("pool", lambda e: e.iota(invsrc[:].rearrange("p (i k) c -> p i k c", k=2)[:, :, :, 8:16],
                                      pattern=[[128, NT], [0, 2], [0, 8]], base=0, channel_multiplier=1),
             writes=["invsrc"])
        scat_ops = []
        xs_ops = []
        x1_ops = {}

        def convB(q, j):
            bk = j % 2
            cb = cbuf[q % 2]
            cs = csq[q % 2]
            for tp in range(31):
                P.op("pe", lambda e, j=j, tp=tp, bk=bk, q=q: e.matmul(
                    banks[bk][:, :], lhsT=Dg[:, j * 34 + 3 + tp, :],
                    rhs=u[:, j, q * QW + tp:q * QW + tp + QW], start=(tp == 0), stop=(tp == 30)),
                    reads=[("Dg", j, 3 + tp)] + [("u", j, qq) for qq in (q - 1, q, q + 1) if 0 <= qq < NQ] + ["u_halo"],
                    writes=[("bank", bk)])
            P.op("act", lambda e, j=j, bk=bk, cb=cb: e.activation(
                out=cb[:, j, :], in_=banks[bk][:, :], func=AF.Identity, bias=prmT[:, j, 34:35], scale=1.0),
                reads=[("bank", bk), "prmT"], writes=[("cb", q % 2, j)])
            P.op("act", lambda e, j=j, bk=bk, cs=cs: e.activation(
                out=cs[:, j, :], in_=banks[bk][:, :], func=AF.Square, bias=prmT[:, j, 34:35], scale=1.0),
                reads=[("bank", bk), "prmT"], writes=[("cs", q % 2, j)])
            precast_next(P.ops[-1])

        def convA(q, j):
            bk = j % 2
            for tp in range(3):
                P.op("pe", lambda e, j=j, tp=tp, bk=bk, q=q: e.matmul(
                    banks[bk][:, :], lhsT=Dg[:, j * 34 + tp, :],
                    rhs=v[:, j, q * QW + tp:q * QW + tp + QW], start=(tp == 0), stop=(tp == 2)),
                    reads=[("Dg", j, tp)] + [("v", j, qq) for qq in (q - 1, q, q + 1) if 0 <= qq < NQ] + ["v_halo"],
                    writes=[("bank", bk)])
            P.op("dve", lambda e, j=j, bk=bk, q=q: e.tensor_tensor(
                out=yab[j][:], in0=banks[bk][:, :], in1=ab[:, j, q * QW:(q + 1) * QW], op=ALU.mult),
                reads=[("bank", bk), ("ab", j, q)], writes=[("yab", j)])
            P.op("pool", lambda e, j=j: e.tensor_tensor(out=ysq[j][:], in0=yab[j][:], in1=yab[j][:], op=ALU.mult),
                 reads=[("yab", j)], writes=[("ysq", j)])

        def head_stats(q, j, beta_col, ych):
            sbk = 2 + (j % 2)
            yq = yT[q % 2]
            P.op("pe", lambda e, j=j, sbk=sbk: e.matmul(banks[sbk][:, :], lhsT=B64[:], rhs=ysq[j][:],
                                                       start=True, stop=True),
                 reads=["B64", ("ysq", j)], writes=[("bank", sbk)])
            P.op("act", lambda e, j=j, sbk=sbk: e.activation(out=rsH[j][:], in_=banks[sbk][:, :], func=AF.Ln,
                                                             bias=epsc[:, 0:1], scale=1.0),
                 reads=[("bank", sbk), "epsc"], writes=[("rsH", j % 2)])
            P.op("act", lambda e, j=j: e.activation(out=rsH[j][:], in_=rsH[j][:], func=AF.Exp, scale=-0.5),
                 reads=[("rsH", j % 2)], writes=[("rsH", j % 2)])
            P.op("dve", lambda e, j=j, yq=yq, ych=ych, beta_col=beta_col: e.scalar_tensor_tensor(
                out=yq[:, ych, :], in0=yab[j][:], scalar=prmT[:, j, beta_col:beta_col + 1], in1=rsH[j][:],
                op0=ALU.mult, op1=ALU.mult),
                reads=[("yab", j), ("rsH", j % 2), "prmT"], writes=[("yT", q % 2, ych)])

        def S2(q):
            cb = cbuf[q % 2]
            cs = csq[q % 2]
            for j in range(4):
                P.op("pe", lambda e, j=j, cb=cb: e.matmul(banks[2][:, :], lhsT=O512f[:], rhs=cb[:, j, :],
                                                         start=(j == 0), stop=(j == 3)),
                     reads=["O512f", ("cb", q % 2, j)], writes=[("bank", 2)])
            for j in range(4):
                P.op("pe", lambda e, j=j, cs=cs: e.matmul(banks[3][:, :], lhsT=O512b[:], rhs=cs[:, j, :],
                                                         start=(j == 0), stop=(j == 3)),
                     reads=["O512b", ("cs", q % 2, j)], writes=[("bank", 3)])
            P.op("act", lambda e: e.activation(out=mean_sb[:], in_=banks[2][:, :], func=AF.Copy),
                 reads=[("bank", 2)], writes=["mean_sb"])
            P.op("dve", lambda e: e.tensor_tensor(out=var_sb[:], in0=mean_sb[:], in1=mean_sb[:], op=ALU.mult),
                 reads=["mean_sb"], writes=["var_sb"])
            P.op("dve", lambda e: e.tensor_tensor(out=var_sb[:], in0=banks[3][:, :], in1=var_sb[:], op=ALU.subtract),
                 reads=[("bank", 3), "var_sb"], writes=["var_sb"])
            P.op("act", lambda e: e.activation(out=var_sb[:], in_=var_sb[:], func=AF.Ln, bias=epsc[:, 0:1], scale=1.0),
                 reads=["var_sb", "epsc"], writes=["var_sb"])
            P.op("act", lambda e: e.activation(out=var_sb[:], in_=var_sb[:], func=AF.Exp, scale=-0.5),
                 reads=["var_sb"], writes=["var_sb"])
            for j in range(4):
                head_stats(q, j, 37, j)

        def S3(q):
            cb = cbuf[q % 2]
            for j in range(4):
                P.op("dve", lambda e, j=j, cb=cb: e.tensor_tensor(out=t_sb[j][:], in0=cb[:, j, :], in1=mean_sb[:],
                                                                 op=ALU.subtract),
                     reads=[("cb", q % 2, j), "mean_sb"], writes=[("t_sb", j)])
            for j in range(4):
                P.op("dve", lambda e, j=j: e.tensor_tensor(out=t_sb[j][:], in0=t_sb[j][:], in1=var_sb[:], op=ALU.mult),
                     reads=[("t_sb", j), "var_sb"], writes=[("t_sb", j)])
            for j in range(4):
                P.op("act", lambda e, j=j: e.activation(
                    out=yab[j][:], in_=t_sb[j][:], func=AF.Silu, bias=prmT[:, j, 36:37], scale=prmT[:, j, 35:36]),
                    reads=[("t_sb", j), "prmT"], writes=[("yab", j)])
            for j in range(4):
                P.op("pool", lambda e, j=j: e.tensor_tensor(out=ysq[j][:], in0=yab[j][:], in1=yab[j][:], op=ALU.mult),
                     reads=[("yab", j)], writes=[("ysq", j)])

        def S4(q):
            for j in range(4):
                head_stats(q, j, 38, 4 + j)

        wpre = []

        def S5a(q, tl):
            i = q * 4 + tl
            p2 = i % 2
            yq = yT[q % 2]
            P.dma("sp", lambda e, i=i, p2=p2: e.dma_start(out=xr[p2][:], in_=x[i * 128:(i + 1) * 128, :]),
                  s_xr[p2], writes=[("xr", p2, 0), ("xr", p2, 1)])
            if wpre:
                e0_, m0_ = wpre.pop(0)
                load_w(e0_, extra_writes=U_NAMES, parts=(m0_,))
            for h in range(2):
                for c in range(8):
                    P.op("pe", lambda e, h=h, c=c, tl=tl, yq=yq: e.matmul(
                        banks[4 + h][:, :], lhsT=yq[:, c, tl * 128:(tl + 1) * 128],
                        rhs=woutb[:, c, h * 512:(h + 1) * 512], start=(c == 0), stop=(c == 7)),
                        reads=[("yT", q % 2, c), ("woutb", h)], writes=[("bank", 4 + h)])
                P.op("dve", lambda e, h=h, p2=p2: e.tensor_tensor(
                    out=xr[p2][:, h * 512:(h + 1) * 512], in0=banks[4 + h][:, :],
                    in1=xr[p2][:, h * 512:(h + 1) * 512], op=ALU.add),
                    reads=[("bank", 4 + h), ("xr", p2, h)], writes=[("xr", p2, h)])
            xk = [("xr", p2, 0), ("xr", p2, 1)]
            x1_ops[i] = P.dma("act", lambda e, i=i, p2=p2: e.dma_start(out=x1_d[i * 128:(i + 1) * 128, :], in_=xr[p2][:]),
                              s_x1[p2], reads=xk, writes=[("x1_d", i)])
            P.op("act", lambda e, i=i, p2=p2: e.activation(out=junkb[:], in_=xr[p2][:], func=AF.Square,
                                                           accum_out=ss2[:, i:i + 1]),
                 reads=xk, writes=["junkb", ("ss2", i)])
            P.op("act", lambda e, i=i: e.activation(out=rs2[:, i:i + 1], in_=ss2[:, i:i + 1], func=AF.Ln,
                                                    bias=epsc[:, 0:1], scale=1.0 / D),
                 reads=[("ss2", i), "epsc"], writes=[("rs2", i)])
            P.op("act", lambda e, i=i: e.activation(out=rs2[:, i:i + 1], in_=rs2[:, i:i + 1], func=AF.Exp,
                                                    scale=-0.5),
                 reads=[("rs2", i)], writes=[("rs2", i)])
            P.op("pool", lambda e, i=i, p2=p2, tl=tl: e.tensor_scalar(out=xsb[tl][:], in0=xr[p2][:],
                                                                     scalar1=rs2[:, i:i + 1], scalar2=1.0,
                                                                     op0=ALU.mult, op1=ALU.mult),
                 reads=xk + [("rs2", i)], writes=[("xsb", tl)])
            xs_ops.append(P.dma("pool", lambda e, i=i, tl=tl: e.dma_start(out=xs_d[i * 128:(i + 1) * 128, :], in_=xsb[tl][:]),
                                s_sc[tl], reads=[("xsb", tl)], writes=[("xs_d", i)]))

        def S5b(q, tl):
            i = q * 4 + tl
            p2 = i % 2
            xk = [("xr", p2, 0), ("xr", p2, 1)]
            for hh in range(2):
                tbk = (6, 2, 3)[(2 * i + hh) % 3]
                pT6 = banks[tbk][:, :].rearrange("p (k t) -> p k t", t=128)
                for k4 in range(4):
                    k = hh * 4 + k4
                    P.op("pe", lambda e, k=k, k4=k4, p2=p2, pT6=pT6: e.transpose(
                        out=pT6[:, k4, :], in_=xr[p2][:, k * 128:(k + 1) * 128], identity=identf[:]),
                        reads=xk + ["identf"], writes=[("bank", tbk)])
                P.op("dve", lambda e, hh=hh, pT6=pT6: e.tensor_copy(out=xsT[:, hh * 4:(hh + 1) * 4, :], in_=pT6),
                     reads=[("bank", tbk)], writes=[("xsT", hh)])
            lgp = banks[7][:, tl * 36:(tl + 1) * 36]
            for k in range(8):
                P.op("pe", lambda e, k=k, lgp=lgp: e.matmul(lgp, lhsT=xsT[:, k, :], rhs=wr[:, k, :],
                                                           start=(k == 0), stop=(k == 7)),
                     reads=[("xsT", k // 4), "wr"], writes=[("b7lg", tl)])
            P.op("dve", lambda e, i=i, tl=tl, lgp=lgp: e.scalar_tensor_tensor(
                out=lgs4[:, tl, :], in0=lgp, scalar=rs2[:, i:i + 1], in1=br_bc[:], op0=ALU.mult, op1=ALU.add),
                reads=[("b7lg", tl), ("rs2", i), "br_bc"], writes=[("lgs4", tl)])
            if debug:
                dbg_ops.append(P.dma("sp", lambda e, i=i, tl=tl: e.dma_start(out=dbg["lg"][i], in_=lgs4[:, tl, :]), s_dbg,
                                     reads=[("lgs4", tl)]))

        def bc(ap2, shape):
            return ap2.broadcast_to(shape)

        def RT(q):
            R = "rt"
            T0 = q * 4
            lg_g = lgs4[:, :, 0:4]
            P.op("dve", lambda e: e.tensor_reduce(out=rq[:, 0, :], in_=lg_g, axis=AX.X, op=ALU.max),
                 reads=[("lgs4", t_) for t_ in range(4)], writes=[R])
            P.op("dve", lambda e: e.tensor_tensor(out=ohg4[:], in0=lg_g, in1=bc(rq[:, 0, :, None], [128, 4, 4]),
                                                  op=ALU.is_ge), reads=[R], writes=[R])
            P.op("dve", lambda e: e.tensor_tensor(out=d4[:], in0=lg_g, in1=bc(rq[:, 0, :, None], [128, 4, 4]),
                                                  op=ALU.subtract), reads=[R], writes=[R])
            P.op("act", lambda e: e.activation(out=d4[:], in_=d4[:], func=AF.Exp), reads=[R], writes=[R])
            P.op("dve", lambda e: e.tensor_reduce(out=rq[:, 1, :], in_=d4[:], axis=AX.X, op=ALU.add),
                 reads=[R], writes=[R])
            P.op("dve", lambda e: e.reciprocal(out=rq[:, 2, :], in_=rq[:, 1, :]), reads=[R], writes=[R])
            le4 = lgs4[:, :, 4:36].rearrange("p t (g x) -> p t g x", x=8)
            P.op("dve", lambda e: e.tensor_tensor(out=t48_4[:], in0=le4, in1=bc(ohg4[:, :, :, None], [128, 4, 4, 8]),
                                                  op=ALU.mult), reads=[R], writes=[R])
            P.op("dve", lambda e: e.tensor_reduce(out=lsel4[:], in_=t48_4[:].rearrange("p t g x -> p t x g"),
                                                  axis=AX.X, op=ALU.add), reads=[R], writes=[R])
            P.op("dve", lambda e: e.tensor_reduce(out=rq[:, 3, :], in_=lsel4[:], axis=AX.X, op=ALU.max),
                 reads=[R], writes=[R])
            P.op("dve", lambda e: e.tensor_tensor(out=oh1_4[:], in0=lsel4[:], in1=bc(rq[:, 3, :, None], [128, 4, 8]),
                                                  op=ALU.is_ge), reads=[R], writes=[R])
            fl = lambda t_: t_[:].rearrange("p t x -> p (t x)")
            P.op("dve", lambda e: e.scalar_tensor_tensor(out=fl(l2_4), in0=fl(oh1_4), scalar=-1e30, in1=fl(lsel4),
                                                         op0=ALU.mult, op1=ALU.add), reads=[R], writes=[R])
            P.op("dve", lambda e: e.tensor_reduce(out=rq[:, 4, :], in_=l2_4[:], axis=AX.X, op=ALU.max),
                 reads=[R], writes=[R])
            P.op("dve", lambda e: e.tensor_tensor(out=oh2_4[:], in0=l2_4[:], in1=bc(rq[:, 4, :, None], [128, 4, 8]),
                                                  op=ALU.is_ge), reads=[R], writes=[R])
            P.op("dve", lambda e: e.tensor_tensor(out=rq[:, 5, :], in0=rq[:, 4, :], in1=rq[:, 3, :],
                                                  op=ALU.subtract), reads=[R], writes=[R])
            P.op("act", lambda e: e.activation(out=rq[:, 5, :], in_=rq[:, 5, :], func=AF.Exp), reads=[R], writes=[R])
            P.op("dve", lambda e: e.tensor_scalar(out=rq[:, 5, :], in0=rq[:, 5, :], scalar1=1.0, scalar2=None,
                                                  op0=ALU.add), reads=[R], writes=[R])
            P.op("dve", lambda e: e.reciprocal(out=rq[:, 6, :], in_=rq[:, 5, :]), reads=[R], writes=[R])
            P.op("dve", lambda e: e.tensor_tensor(out=cw[:, T0:T0 + 4, 0], in0=rq[:, 6, :], in1=rq[:, 2, :],
                                                  op=ALU.mult), reads=[R], writes=[R, ("cw", q)])
            P.op("dve", lambda e: e.tensor_tensor(out=cw[:, T0:T0 + 4, 1], in0=rq[:, 2, :], in1=cw[:, T0:T0 + 4, 0],
                                                  op=ALU.subtract), reads=[R], writes=[R, ("cw", q)])
            P.op("dve", lambda e: e.tensor_tensor(out=E1_4[:], in0=bc(ohg4[:, :, :, None], [128, 4, 4, 8]),
                                                  in1=bc(oh1_4[:, :, None, :], [128, 4, 4, 8]), op=ALU.mult),
                 reads=[R], writes=[R, "E4"])
            P.op("dve", lambda e: e.tensor_tensor(out=E2_4[:], in0=bc(ohg4[:, :, :, None], [128, 4, 4, 8]),
                                                  in1=bc(oh2_4[:, :, None, :], [128, 4, 4, 8]), op=ALU.mult),
                 reads=[R], writes=[R, "E4"])
            P.op("dve", lambda e: e.tensor_tensor(
                out=Mb[:, T0:T0 + 4, :].rearrange("p t (g x) -> p t g x", x=8), in0=E1_4[:], in1=E2_4[:], op=ALU.add),
                reads=[R], writes=[("Mb", q)])

        def S6(q):
            T0 = q * 4
            posq = banks[7][:, 256:384].rearrange("p (t x) -> p t x", x=32)
            for tl in range(4):
                i = T0 + tl
                P.op("pe", lambda e, i=i, tl=tl: e.matmul(posq[:, tl, :], lhsT=Ltri[:], rhs=Mb[:, i, :], start=True,
                                                          stop=(i == 0)),
                     reads=["Ltri", ("Mb", q)], writes=["b7pos"])
                for jj in range(i):
                    P.op("pe", lambda e, jj=jj, i=i, tl=tl: e.matmul(posq[:, tl, :], lhsT=ones_b[:], rhs=Mb[:, jj, :],
                                                                    start=False, stop=(jj == i - 1)),
                         reads=["ones_b", ("Mb", jj // 4)], writes=["b7pos"])
            Ef = lambda E_: E_[:].rearrange("p t g x -> p t (g x)")
            S = "s6"
            for kk, E_ in enumerate((E1_4, E2_4)):
                P.op("dve", lambda e, E_=E_: e.tensor_tensor(out=pr4[:], in0=Ef(E_), in1=posq, op=ALU.mult),
                     reads=["E4", "b7pos"], writes=[S])
                P.op("dve", lambda e, kk=kk: e.tensor_reduce(out=s4[:, :, kk], in_=pr4[:], axis=AX.X, op=ALU.add),
                     reads=[S], writes=[S])
                P.op("dve", lambda e, E_=E_: e.tensor_tensor(out=pr4[:], in0=Ef(E_),
                                                             in1=bc(iota_e[:, None, :], [128, 4, 32]), op=ALU.mult),
                     reads=["E4", "iota_e", S], writes=[S])
                P.op("dve", lambda e, kk=kk: e.tensor_reduce(out=eid4[:, :, kk], in_=pr4[:], axis=AX.X, op=ALU.add),
                     reads=[S], writes=[S])
            f2 = lambda t_: t_[:].rearrange("p t k -> p (t k)")
            P.op("dve", lambda e: e.tensor_scalar(out=f2(s4), in0=f2(s4), scalar1=float(CAP - 1), scalar2=None,
                                                  op0=ALU.min), reads=[S], writes=[S])
            P.op("dve", lambda e: e.scalar_tensor_tensor(out=f2(rf4), in0=f2(eid4), scalar=float(CAP), in1=f2(s4),
                                                         op0=ALU.mult, op1=ALU.add), reads=[S], writes=[S])
            P.op("dve", lambda e: e.tensor_copy(out=ridx[:, T0:T0 + 4, :], in_=rf4[:]),
                 reads=[S], writes=[("ridx", q)])

        def S6b(q):
            T0 = q * 4
            for tl in range(4):
                i = T0 + tl
                for kk in range(2):
                    so = P.dma("pool", lambda e, i=i, kk=kk, tl=tl: e.indirect_dma_start(
                        out=inv_d[:, :], out_offset=bass.IndirectOffsetOnAxis(ap=ridx[:, i, kk:kk + 1], axis=0),
                        in_=invsrc[:, i * 2 + kk, :], in_offset=None),
                        s_si[tl], reads=[("ridx", q), "invsrc"], writes=[], deps=init_ops[0:1])
                    scat_ops.append(so)

        m2 = ar.mark()
        ar.reset(mF)
        NW = 3
        NXC0 = 5
        NXC = 13
        NYO = 6
        NY = 4
        NOT = 3
        yt = [ar.alloc([128, 2, D], BF16) for _ in range(NY)]
        ar.reset(mF)
        w1b = [None] * NW
        w3b = [None] * NW
        w2b = [None] * NW
        for sl_ in range(2):
            w1b[sl_] = ar.alloc([128, 8, 512], BF16)
            w3b[sl_] = ar.alloc([128, 8, 512], BF16)
            w2b[sl_] = ar.alloc([128, 4, D], BF16)
        assert ar.mark() <= mU, (ar.mark(), mU)
        ar.reset(mU)
        xc = [ar.alloc([128, D], F32) for _ in range(NXC0)]
        _save = ar.mark()
        ar.reset(mDg)
        xc += [ar.alloc([128, D], F32) for _ in range(NXC - NXC0)]
        assert ar.mark() <= mDg + 4 * 34 * 128
        ar.reset(_save)
        ot = [ar.alloc([128, D], F32) for _ in range(NOT)]
        w1b[2] = ar.alloc([128, 8, 512], BF16)
        w3b[2] = ar.alloc([128, 8, 512], BF16)
        w2b[2] = ar.alloc([128, 4, D], BF16)
        NXG = 3
        xg = [ar.alloc([128, NST, D], BF16) for _ in range(NXG)]
        xT = [ar.alloc([128, 8, CAP], BF16) for _ in range(2)]
        slt = [ar.alloc([128, CAP], F32) for _ in range(2)]
        actT = [ar.alloc([128, 4, CAP], BF16) for _ in range(2)]
        yo = [ar.alloc([128, D], BF16) for _ in range(NYO)]
        NINV = 6
        invs = [ar.alloc([128, NST, 16], I32) for _ in range(NINV)]
        m2end = ar.mark()
        ar.reset(m2)
        s_inv = [sem(f"s_inv{i}") for i in range(NINV)]
        s_w = [[sem(f"s_w{i}_{m}") for m in range(3)] for i in range(NW)]
        s_xg = [[sem(f"s_xg{i}_{j}") for j in range(NST)] for i in range(NXG)]
        s_yo = [sem(f"s_yo{i}") for i in range(NYO)]
        s_xc = [sem(f"s_xc{i}") for i in range(NXC)]
        s_yt = [sem(f"s_yt{i}") for i in range(NY)]
        s_ot = [sem(f"s_ot{i}") for i in range(NOT)]
        U_NAMES = [(nm, j_, q_) for nm in ("u", "v", "ab") for j_ in range(4) for q_ in range(NQ)] + ["u_halo", "v_halo"]

        s_wp = [[sem(f"s_wp{i}_{m}") for m in range(3)] for i in range(NW)]

        def load_w(e_, extra_writes=(), parts=(0, 1, 2)):
            sl = e_ % NW
            xw = list(extra_writes)
            if e_ < NPRE:
                rk = [("wp", e_, m) for m in range(3)]
                if 0 in parts:
                    P.dma("sp", lambda e, e_=e_, sl=sl: e.dma_start(
                        out=w1b[sl][:], in_=w1p[e_].rearrange("p (k f) -> p k f", k=8)), s_wp[sl][0],
                        reads=rk, writes=[("w1b", sl)] + xw)
                if 1 in parts:
                    P.dma("sp", lambda e, e_=e_, sl=sl: e.dma_start(
                        out=w3b[sl][:], in_=w3p[e_].rearrange("p (k f) -> p k f", k=8)), s_wp[sl][1],
                        reads=rk, writes=[("w3b", sl)] + xw)
                if 2 in parts:
                    P.dma("sp", lambda e, e_=e_, sl=sl: e.dma_start(
                        out=w2b[sl][:], in_=w2p[e_].rearrange("p (k f) -> p k f", k=4)), s_wp[sl][2],
                        reads=rk, writes=[("w2b", sl)] + xw)
                return
            assert tuple(parts) == (0, 1, 2)
            P.dma("pool", lambda e, e_=e_, sl=sl: e.dma_start(
                out=w1b[sl][:], in_=w1[e_].rearrange("(k p) f -> p k f", p=128)), s_w[sl][0], writes=[("w1b", sl)] + xw)
            P.dma("pool", lambda e, e_=e_, sl=sl: e.dma_start(
                out=w3b[sl][:], in_=w3[e_].rearrange("(k p) f -> p k f", p=128)), s_w[sl][1], writes=[("w3b", sl)] + xw)
            P.dma("pool", lambda e, e_=e_, sl=sl: e.dma_start(
                out=w2b[sl][:], in_=w2[e_].rearrange("(k p) f -> p k f", p=128)), s_w[sl][2], writes=[("w2b", sl)] + xw)

        for j in range(4):
            convB(0, j)
        for j in range(4):
            convA(0, j)
        def S5tile(q, tl):
            S5a(q, tl)
            if tl >= 1:
                S5b(q, tl - 1)
            if tl == 3:
                S5b(q, 3)

        for q in range(NQ):
            nxt = q + 1 < NQ
            last = q == NQ - 1
            if nxt:
                convB(q + 1, 0)
            S2(q)
            if 1 <= q < NQ - 1:
                S6(q - 1)
            if nxt:
                convB(q + 1, 1)
            if last:
                S5tile(q - 1, 0)
                S5tile(q - 1, 1)
            S3(q)
            if nxt:
                convB(q + 1, 2)
            if last:
                S5tile(q - 1, 2)
                S5tile(q - 1, 3)
            S4(q)
            if 1 <= q < NQ - 1:
                S6b(q - 1)
            if nxt:
                convB(q + 1, 3)
            if debug:
                dbg_ops.append(P.dma("sp", lambda e, q=q: e.dma_start(out=dbg["yT"][q], in_=yT[q % 2][:]), s_dbg,
                                     reads=[("yT", q % 2, c) for c in range(8)]))
            if nxt:
                for j in range(4):
                    convA(q + 1, j)
            if q == NQ - 2:
                wpre.extend([(0, 0), (0, 1), (0, 2), (1, 0), (1, 1), (1, 2)])
                continue
            if last:
                RT(q - 1)
                S5tile(q, 0)
                S6(q - 1)
                S5tile(q, 1)
                S6b(q - 1)
                S5tile(q, 2)
                S5tile(q, 3)
                RT(q)
            else:
                for tl in range(4):
                    S5tile(q, tl)
                RT(q)
        S6(NQ - 1)
        S6b(NQ - 1)
        while pre_pos[0] < len(pre_list):
            precast_next(P.ops[-1])
        if debug:
            dbg_ops.append(P.dma("sp", lambda e: e.dma_start(out=dbg["cw"], in_=cw[:]), s_dbg,
                                 reads=[("cw", q) for q in range(NQ)]))
            dbg_ops.append(P.dma("sp", lambda e: e.dma_start(out=dbg["ridx"], in_=ridx[:]), s_dbg,
                                 reads=[("ridx", q) for q in range(NQ)]))

        P.set_fence()
        ar.reset(m2end)
        g2T = prmT[:, :, 40:41]
        def load_xc(i):
            b = i % NXC
            P.dma("sp", lambda e, i=i, b=b: e.dma_start(out=xc[b][:], in_=x1_d[i * 128:(i + 1) * 128, :]),
                  s_xc[b], reads=[("x1_d", i)], writes=[("xc", b)])

        def load_inv(e_):
            sl = e_ % NINV
            P.dma("sp", lambda e, e_=e_, sl=sl: e.dma_start(
                out=invs[sl][:], in_=inv_d[e_ * CAP:(e_ + 1) * CAP, :].rearrange("(j p) c -> p j c", p=128)),
                s_inv[sl], writes=[("invs", sl)], deps=scat_ops)

        gbc_cache = []

        def gbc_reg(e):
            if not gbc_cache:
                gbc_cache.append(e.to_reg(S - 1))
            return gbc_cache[0]

        def gather_xg(e_):
            sl = e_ % NXG
            iv = e_ % NINV
            for j in range(NST):
                P.dma("pool", lambda e, sl=sl, iv=iv, j=j: e.indirect_dma_start(
                    out=xg[sl][:, j, :], out_offset=None, in_=xs_d[:, :],
                    in_offset=bass.IndirectOffsetOnAxis(ap=invs[iv][:, j, 8:9], axis=0),
                    bounds_check=gbc_reg(e), oob_is_err=False),
                    s_xg[sl][j], reads=[("invs", iv)], writes=[("xg", sl, j)], deps=xs_ops)

        for sl_ in range(NXG):
            P.op("dve", lambda e, sl_=sl_: e.memset(xg[sl_][:], 0.0), writes=[("xg", sl_, j_) for j_ in range(NST)])
        load_inv(0)
        load_inv(1)
        load_inv(2)
        gather_xg(0)
        gather_xg(1)
        for i in range(NXC0):
            load_xc(i)
        yg_ops = []
        yoc = [0]
        bc_cache = []

        def bc_reg(e):
            if not bc_cache:
                bc_cache.append(e.to_reg(2 * S - 1))
            return bc_cache[0]

        def ex_trans(e_, kp):
            p2 = e_ % 2
            gx = e_ % NXG
            tbk = kp % 2
            pTb = banks[tbk][:, :].bitcast(BF16)[:, 0:2 * CAP].rearrange("p (k t) -> p k t", t=CAP)
            for k2 in range(2):
                k = kp * 2 + k2
                for j in range(NST):
                    P.op("pe", lambda e, k=k, k2=k2, j=j, gx=gx, pTb=pTb: e.transpose(
                        out=pTb[:, k2, j * 128:(j + 1) * 128], in_=xg[gx][:, j, k * 128:(k + 1) * 128],
                        identity=identb[:]),
                        reads=[("xg", gx, j), "identb"], writes=[("bank", tbk)])
            P.op("dve", lambda e, kp=kp, p2=p2, pTb=pTb: e.tensor_tensor(
                out=xT[p2][:, kp * 2:(kp + 1) * 2, :], in0=pTb,
                in1=g2T[:, kp * 2:(kp + 1) * 2, :].broadcast_to([128, 2, CAP]), op=ALU.mult),
                reads=[("bank", tbk), "prmT"], writes=[("xT", p2, kp)])

        def ex_h13(e_):
            sl = e_ % NW
            p2 = e_ % 2
            for f in range(4):
                b1 = 2 + (f % 2)
                b3 = 4 + (f % 2)
                for k in range(8):
                    P.op("pe", lambda e, k=k, f=f, b1=b1, sl=sl, p2=p2: e.matmul(
                        banks[b1][:, 0:CAP], lhsT=w1b[sl][:, k, f * 128:(f + 1) * 128], rhs=xT[p2][:, k, :],
                        start=(k == 0), stop=(k == 7)),
                        reads=[("w1b", sl), ("xT", p2, k // 2)], writes=[("bank", b1)])
                for k in range(8):
                    P.op("pe", lambda e, k=k, f=f, b3=b3, sl=sl, p2=p2: e.matmul(
                        banks[b3][:, 0:CAP], lhsT=w3b[sl][:, k, f * 128:(f + 1) * 128], rhs=xT[p2][:, k, :],
                        start=(k == 0), stop=(k == 7)),
                        reads=[("w3b", sl), ("xT", p2, k // 2)], writes=[("bank", b3)])
                P.op("act", lambda e, f=f, b1=b1: e.activation(out=slt[f % 2][:], in_=banks[b1][:, 0:CAP], func=AF.Silu),
                     reads=[("bank", b1)], writes=[("slt", f % 2)])
                P.op("dve", lambda e, f=f, b3=b3, p2=p2: e.tensor_tensor(
                    out=actT[p2][:, f, :], in0=banks[b3][:, 0:CAP], in1=slt[f % 2][:], op=ALU.mult),
                    reads=[("bank", b3), ("slt", f % 2)], writes=[("actT", p2, f)])

        def ex_out(e_, j):
            sl = e_ % NW
            p2 = e_ % 2
            yb_ = yoc[0] % NYO
            yoc[0] += 1
            for h in range(2):
                ob = 6 + h
                for f in range(4):
                    P.op("pe", lambda e, f=f, j=j, h=h, ob=ob, sl=sl, p2=p2: e.matmul(
                        banks[ob][:, :], lhsT=actT[p2][:, f, j * 128:(j + 1) * 128],
                        rhs=w2b[sl][:, f, h * 512:(h + 1) * 512], start=(f == 0), stop=(f == 3)),
                        reads=[("w2b", sl), ("actT", p2, f)], writes=[("bank", ob)])
                if h == 0:
                    P.op("act", lambda e, h=h, ob=ob, yb_=yb_: e.activation(
                        out=yo[yb_][:, h * 512:(h + 1) * 512], in_=banks[ob][:, :], func=AF.Copy),
                        reads=[("bank", ob)], writes=[("yo", yb_, h)])
                else:
                    P.op("dve", lambda e, h=h, ob=ob, yb_=yb_: e.tensor_copy(
                        out=yo[yb_][:, h * 512:(h + 1) * 512], in_=banks[ob][:, :]),
                        reads=[("bank", ob)], writes=[("yo", yb_, h)])
            iv = e_ % NINV
            yg_ops.append(P.dma("pool", lambda e, j=j, iv=iv, yb_=yb_: e.indirect_dma_start(
                out=yt_d[:, :], out_offset=bass.IndirectOffsetOnAxis(ap=invs[iv][:, j, 0:1], axis=0),
                in_=yo[yb_][:], in_offset=None, bounds_check=bc_reg(e), oob_is_err=False),
                s_yo[yb_], reads=[("yo", yb_, 0), ("yo", yb_, 1), ("invs", iv)]))

        for kp in range(4):
            ex_trans(0, kp)
        for e_ in range(NE):
            if e_ + 2 < NE:
                gather_xg(e_ + 2)
                load_w(e_ + 2)
            if e_ + 3 < NE:
                load_inv(e_ + 3)
            if NXC0 + e_ < NXC:
                load_xc(NXC0 + e_)
            ex_h13(e_)
            nx = e_ + 1 < NE
            if nx:
                ex_trans(e_ + 1, 0)
                ex_trans(e_ + 1, 1)
            ex_out(e_, 0)
            if nx:
                ex_trans(e_ + 1, 2)
                ex_trans(e_ + 1, 3)
            ex_out(e_, 1)
            ex_out(e_, 2)

        out_ops = []

        def load_yt(i):
            yb6 = i % NY
            P.dma("sp", lambda e, i=i, yb6=yb6: e.dma_start(
                out=yt[yb6][:], in_=yt_d[i * 256:(i + 1) * 256, :].rearrange("(p k) d -> p k d", k=2)),
                s_yt[yb6], writes=[("yt", yb6)], deps=yg_ops)

        for i in range(NY):
            load_yt(i)

        def combine(i):
            yb6 = i % NY
            b = i % NXC
            P.op("dve", lambda e, i=i, yb6=yb6, b=b: e.scalar_tensor_tensor(
                out=xc[b][:], in0=yt[yb6][:, 0, :], scalar=cw[:, i, 0:1], in1=xc[b][:], op0=ALU.mult, op1=ALU.add),
                reads=[("yt", yb6), ("xc", b), ("cw", i // 4)], writes=[("xc", b)])
            P.op("dve", lambda e, i=i, yb6=yb6, b=b: e.scalar_tensor_tensor(
                out=xc[b][:], in0=yt[yb6][:, 1, :], scalar=cw[:, i, 1:2], in1=xc[b][:], op0=ALU.mult, op1=ALU.add),
                reads=[("yt", yb6), ("xc", b), ("cw", i // 4)], writes=[("xc", b)])

        combine(0)
        for i in range(NT):
            p2 = i % NOT
            b = i % NXC
            P.op("act", lambda e, i=i, b=b: e.activation(out=junkb[:], in_=xc[b][:], func=AF.Square,
                                                         accum_out=ssF[:, i:i + 1]),
                 reads=[("xc", b)], writes=["junkb", ("ssF", i)])
            P.op("act", lambda e, i=i: e.activation(out=rsF[:, i:i + 1], in_=ssF[:, i:i + 1], func=AF.Ln,
                                                    bias=epsc[:, 0:1], scale=1.0 / D),
                 reads=[("ssF", i), "epsc"], writes=[("rsF", i)])
            P.op("act", lambda e, i=i: e.activation(out=rsF[:, i:i + 1], in_=rsF[:, i:i + 1], func=AF.Exp, scale=-0.5),
                 reads=[("rsF", i)], writes=[("rsF", i)])
            if i + 1 < NT:
                combine(i + 1)
            P.op("dve", lambda e, i=i, p2=p2, b=b: e.scalar_tensor_tensor(
                out=ot[p2][:], in0=xc[b][:], scalar=rsF[:, i:i + 1], in1=gF_bc[:], op0=ALU.mult, op1=ALU.mult),
                reads=[("xc", b), ("rsF", i), "gF_bc"], writes=[("ot", p2)])
            out_ops.append(P.dma("sp", lambda e, i=i, p2=p2: e.dma_start(out=out[i * 128:(i + 1) * 128, :], in_=ot[p2][:]),
                                 s_ot[p2], reads=[("ot", p2)]))
            if i + NXC < NT:
                load_xc(i + NXC)
            if i + NY < NT:
                load_yt(i + NY)
        finals = out_ops[-NOT:] + (dbg_ops if debug else [])
        P.emit(st, final_waits=finals)
        if debug:
            print('arena mF', mF, 'mU', mU, 'peak', ar.peak, 'ARN', ARN, 'nops', len(P.ops), 'nsem', nsem[0])
    return nc


_IN_NAMES = ["norm_mix_g", "w_in", "conv_a_w", "conv_b_w", "conv_b_bias", "ln_b_g", "ln_b_b", "beta_a", "beta_b",
             "w_out", "norm_ffn_g", "w_route_group", "b_route_group", "w_route_expert", "b_route_expert",
             "w1", "w3", "w2"]


def make_in_maps(inputs):
    f = lambda a: np.ascontiguousarray(np.asarray(a, dtype=np.float32))
    shared = {}
    for n in _IN_NAMES:
        a = f(inputs[n])[0]
        if a.ndim == 1:
            a = a.reshape(1, -1)
        shared[n] = np.ascontiguousarray(a)
    shared["norm_final_g"] = f(inputs["norm_final_g"]).reshape(1, -1)
    x = f(inputs["x"])
    return [dict(shared, x=np.ascontiguousarray(x[c])) for c in range(8)]


def kernel(**inputs):
    nc = build_nc()
    in_maps = make_in_maps(inputs)
    res = run_bass_kernel_spmd(nc, in_maps, core_ids=list(range(8)))
    return np.stack([np.asarray(r["out"], dtype=np.float32) for r in res.results], axis=0)
```
